# Optimizing a Trainium2 kernel written in Bass

```python
import jax, jax.numpy as jnp
from jax import lax
import numpy as np

D_MODEL = 1024
BATCH = 8
SEQ = 4096
DEPTH = 4

GRID_W = 64
CTX_LEN = 256
ROPE_BASE = 10000.0
Q_BLOCK = 128
EPS = 1e-6

MLA_HEADS = D_MODEL // 128
MLA_NOPE = 64
MLA_ROPE = 32
MLA_QK = MLA_NOPE + MLA_ROPE
MLA_V = 64
Q_LORA = 3 * D_MODEL // 8
KV_LORA = D_MODEL // 4
MLA_IN = Q_LORA + KV_LORA + MLA_ROPE
CONV_CH = D_MODEL // 2
CONV_K = 31
EVEN_IN = MLA_IN + 2 * CONV_CH
EVEN_OUT = MLA_HEADS * MLA_V + CONV_CH

RW_N = 64
RW_HEADS = D_MODEL // 128
RW_DIM = RW_HEADS * RW_N
DECAY_LORA = 64
ICLR_LORA = 64
GATE_LORA = 128
SHIFT_K = 3
RW_IN = 3 * RW_DIM + 2 * DECAY_LORA + 2 * ICLR_LORA + GATE_LORA
GN_EPS = 64e-5

NA_HEADS = D_MODEL // 128
NA_DIM = 64
WIN_H = 8
WIN_W = 16
NA_IN = 3 * NA_HEADS * NA_DIM
ODD_IN = RW_IN + NA_IN
ODD_OUT = RW_DIM + NA_HEADS * NA_DIM

N_EXPERTS = 16
D_EXPERT = D_MODEL
EC_FACTOR = 2

kernel_name = 'hybrid_mla_conformer_rwkv7_natten_ecmoe_dit'


def rms_norm(x, g):
    xf = x.astype(jnp.float32)
    y = xf * lax.rsqrt(jnp.mean(jnp.square(xf), -1, keepdims=True) + EPS)
    return (y * g.astype(jnp.float32)).astype(x.dtype)


def layer_norm(x, g, b):
    xf = x.astype(jnp.float32)
    mu = jnp.mean(xf, -1, keepdims=True)
    var = jnp.mean(jnp.square(xf - mu), -1, keepdims=True)
    y = (xf - mu) * lax.rsqrt(var + EPS)
    return (y * g.astype(jnp.float32) + b.astype(jnp.float32)).astype(x.dtype)


def head_group_norm(y, g, b):
    B, T, H, N = y.shape
    mu = jnp.mean(y, -1, keepdims=True)
    var = jnp.mean(jnp.square(y - mu), -1, keepdims=True)
    yn = ((y - mu) * lax.rsqrt(var + GN_EPS)).reshape(B, T, H * N)
    return yn * g + b


def depthwise_conv(x, w):
    pad = (w.shape[0] - 1) // 2
    return lax.conv_general_dilated(x, w[:, None, :].astype(x.dtype), (1,), [(pad, pad)],
                                    dimension_numbers=('NWC', 'WIO', 'NWC'),
                                    feature_group_count=x.shape[-1])


def axial_rope_tables(n_tok, rot_dim):
    t = jnp.arange(n_tok, dtype=jnp.int32)
    row = (t // GRID_W).astype(jnp.float32)
    col = (t % GRID_W).astype(jnp.float32)
    half = rot_dim // 2
    inv = ROPE_BASE ** (-jnp.arange(0, half, 2, dtype=jnp.float32) / half)
    ar = row[:, None] * inv
    ac = col[:, None] * inv
    ang = jnp.concatenate([ar, ar, ac, ac], -1)
    return jnp.cos(ang), jnp.sin(ang)


def rotate_half(u):
    u1, u2 = jnp.split(u, 2, -1)
    return jnp.concatenate([-u2, u1], -1)


def apply_axial_rope(x, cos, sin):
    half = x.shape[-1] // 2
    rot = jnp.concatenate([rotate_half(x[..., :half]), rotate_half(x[..., half:])], -1)
    return (x * cos[:, None, :] + rot * sin[:, None, :]).astype(x.dtype)


def dense_attention(q, k, v):
    scale = q.shape[-1] ** -0.5
    s = jnp.einsum('bqhd,bkhd->bhqk', q, k).astype(jnp.float32) * scale
    p = jax.nn.softmax(s, -1).astype(v.dtype)
    return jnp.einsum('bhqk,bkhd->bqhd', p, v)


def blocked_attention(q, k, v):
    B, T, H, D = q.shape
    nb = T // Q_BLOCK
    qb = jnp.moveaxis(q.reshape(B, nb, Q_BLOCK, H, D), 1, 0)
    out = lax.map(lambda qq: dense_attention(qq, k, v), qb)
    return jnp.moveaxis(out, 0, 1).reshape(B, T, H, v.shape[-1])


def mla_qkv(u, q_norm, w_uq, kv_norm, w_ukv, q_g, k_g, rope_cs=None):
    B, T, _ = u.shape
    cq = u[..., :Q_LORA]
    ckv = u[..., Q_LORA:Q_LORA + KV_LORA]
    kr = u[..., Q_LORA + KV_LORA:MLA_IN]
    q = (rms_norm(cq, q_norm) @ w_uq).reshape(B, T, MLA_HEADS, MLA_QK)
    kv = (rms_norm(ckv, kv_norm) @ w_ukv).reshape(B, T, MLA_HEADS, MLA_NOPE + MLA_V)
    k_nope, v = kv[..., :MLA_NOPE], kv[..., MLA_NOPE:]
    k = jnp.concatenate([k_nope, jnp.broadcast_to(kr[:, :, None, :], (B, T, MLA_HEADS, MLA_ROPE))], -1)
    q = rms_norm(q, q_g)
    k = rms_norm(k, k_g)
    if rope_cs is not None:
        cos, sin = rope_cs
        q = jnp.concatenate([q[..., :MLA_NOPE], apply_axial_rope(q[..., MLA_NOPE:], cos, sin)], -1)
        k = jnp.concatenate([k[..., :MLA_NOPE], apply_axial_rope(k[..., MLA_NOPE:], cos, sin)], -1)
    return q, k, v


def conformer_conv(u, dw_w, dw_b, ln_g, ln_b):
    a, gate = jnp.split(u, 2, -1)
    h = a * jax.nn.sigmoid(gate)
    h = depthwise_conv(h, dw_w) + dw_b
    return jax.nn.silu(layer_norm(h, ln_g, ln_b))


def even_mixer(h, hc, rope_cs, w_in, w_out, q_norm, w_uq, kv_norm, w_ukv, q_g, k_g,
               dw_w, dw_b, ln_g, ln_b, with_ctx_out):
    B, T, _ = h.shape
    u = h @ w_in
    uc = hc @ w_in
    mla_p = (q_norm, w_uq, kv_norm, w_ukv, q_g, k_g)
    q, k, v = mla_qkv(u[..., :MLA_IN], *mla_p, rope_cs=rope_cs)
    qc, kc, vc = mla_qkv(uc[..., :MLA_IN], *mla_p)
    o_att = blocked_attention(q, jnp.concatenate([k, kc], 1), jnp.concatenate([v, vc], 1))
    o_conv = conformer_conv(u[..., MLA_IN:], dw_w, dw_b, ln_g, ln_b)
    y = jnp.concatenate([o_att.reshape(B, T, -1), o_conv], -1) @ w_out
    if not with_ctx_out:
        return y, None
    oc_att = dense_attention(qc, kc, vc)
    oc_conv = conformer_conv(uc[..., MLA_IN:], dw_w, dw_b, ln_g, ln_b)
    yc = jnp.concatenate([oc_att.reshape(B, hc.shape[1], -1), oc_conv], -1) @ w_out
    return y, yc


def wkv7_step(S, inp):
    r, w, k, v, a, b = inp
    sa = jnp.einsum('bhvk,bhk->bhv', S, a)
    S = S * w[:, :, None, :] + sa[..., None] * b[:, :, None, :] + v[..., None] * k[:, :, None, :]
    return S, jnp.einsum('bhvk,bhk->bhv', S, r)


def wkv7_scan(S0, r, w, k, v, a, b, reverse):
    xs = tuple(jnp.moveaxis(t.astype(jnp.float32), 1, 0) for t in (r, w, k, v, a, b))
    S, ys = lax.scan(wkv7_step, S0, xs, reverse=reverse)
    return S, jnp.moveaxis(ys, 0, 1)


def rwkv_inputs(u, shift_w, w0, w2, a0, a2, g2, k_k, k_a):
    u = depthwise_conv(u, shift_w)
    B, T, _ = u.shape
    o1 = 3 * RW_DIM
    o2 = o1 + 2 * DECAY_LORA
    o3 = o2 + 2 * ICLR_LORA
    r, k, v = jnp.split(u[..., :o1], 3, axis=-1)
    lw = u[..., o1:o2].reshape(B, T, 2, DECAY_LORA)
    la = u[..., o2:o3].reshape(B, T, 2, ICLR_LORA)
    lg = u[..., o3:]
    logw = (w0 + jnp.einsum('btdr,drc->btdc', jnp.tanh(lw), w2)).astype(jnp.float32)
    w = jnp.exp(-jnp.exp(-jax.nn.softplus(-logw) - 0.5))
    a = jax.nn.sigmoid((a0 + jnp.einsum('btdr,drc->btdc', la, a2)).astype(jnp.float32))
    kk = (k * k_k).reshape(B, T, RW_HEADS, RW_N).astype(jnp.float32)
    kk = kk * lax.rsqrt(jnp.sum(kk * kk, -1, keepdims=True) + 1e-12)
    k_eff = k[:, :, None, :].astype(jnp.float32) * (1.0 + (a - 1.0) * k_a)
    g = jax.nn.sigmoid(lg) @ g2
    hd = lambda t: t.reshape(B, T, RW_HEADS, RW_N).astype(jnp.float32)
    dir_hd = lambda t: t.reshape(B, T, 2, RW_HEADS, RW_N)
    return hd(r), hd(v), kk, dir_hd(w), dir_hd(a), dir_hd(k_eff), g


def rwkv_mixer(u, uc, shift_w, w0, w2, a0, a2, g2, k_k, k_a, r_k, gn_g, gn_b, with_ctx_out):
    p = (shift_w, w0, w2, a0, a2, g2, k_k, k_a)
    lat = rwkv_inputs(u, *p)
    cx = rwkv_inputs(uc, *p)
    rk = r_k.reshape(RW_HEADS, RW_N).astype(jnp.float32)
    B = u.shape[0]

    def direction(inp, d, S0):
        r, v, kk, w, a, k_eff, _ = inp
        a_d, k_d = a[:, :, d], k_eff[:, :, d]
        S, y = wkv7_scan(S0, r, w[:, :, d], k_d, v, -kk, kk * a_d, reverse=(d == 1))
        return S, y + jnp.sum(r * k_d * rk, -1, keepdims=True) * v

    y_lat, y_ctx = [], []
    for d in range(2):
        S0 = jnp.zeros((B, RW_HEADS, RW_N, RW_N), jnp.float32)
        S_c, yc_d = direction(cx, d, S0)
        _, yl_d = direction(lat, d, S_c)
        y_lat.append(yl_d)
        y_ctx.append(yc_d)
    y = head_group_norm(y_lat[0] + y_lat[1], gn_g, gn_b) * lat[6]
    if not with_ctx_out:
        return y, None
    yc = head_group_norm(y_ctx[0] + y_ctx[1], gn_g, gn_b) * cx[6]
    return y, yc


def natten_qkv(u, q_g, k_g):
    B, T, _ = u.shape
    q, k, v = [t.reshape(B, T, NA_HEADS, NA_DIM) for t in jnp.split(u, 3, -1)]
    return rms_norm(q, q_g), rms_norm(k, k_g), v


def neighbourhood_attention(q, k, v, k_ctx, v_ctx, rpb):
    B, T, H, Dh = q.shape
    rows = T // GRID_W
    kh = min(WIN_H, rows)
    kw = min(WIN_W, GRID_W)
    n_loc = kh * kw
    scale = Dh ** -0.5
    qg = q.reshape(B, rows, GRID_W, H, Dh)
    kg = k.reshape(B, rows, GRID_W, H, Dh)
    vg = v.reshape(B, rows, GRID_W, H, Dh)
    cols = jnp.arange(GRID_W, dtype=jnp.int32)
    col_start = jnp.clip(cols - kw // 2, 0, GRID_W - kw)
    col_idx = col_start[:, None] + jnp.arange(kw, dtype=jnp.int32)[None, :]
    dcol = col_idx - cols[:, None] + (WIN_W - 1)

    def one_row(r):
        rs = jnp.clip(r - kh // 2, 0, rows - kh)
        k_band = lax.dynamic_slice_in_dim(kg, rs, kh, axis=1)
        v_band = lax.dynamic_slice_in_dim(vg, rs, kh, axis=1)
        k_win = k_band[:, :, col_idx]
        v_win = v_band[:, :, col_idx]
        q_row = lax.dynamic_index_in_dim(qg, r, axis=1, keepdims=False)
        drow = rs + jnp.arange(kh, dtype=jnp.int32) - r + (WIN_H - 1)
        bias = jnp.transpose(rpb[:, drow][:, :, dcol], (0, 2, 1, 3))
        s_loc = jnp.einsum('bchd,bicjhd->bhcij', q_row, k_win).astype(jnp.float32) * scale + bias.astype(jnp.float32)
        s_ctx = jnp.einsum('bchd,bnhd->bhcn', q_row, k_ctx).astype(jnp.float32) * scale
        s = jnp.concatenate([s_loc.reshape(B, H, GRID_W, n_loc), s_ctx], -1)
        p = jax.nn.softmax(s, -1).astype(v.dtype)
        o = jnp.einsum('bhcij,bicjhd->bchd', p[..., :n_loc].reshape(B, H, GRID_W, kh, kw), v_win)
        return o + jnp.einsum('bhcn,bnhd->bchd', p[..., n_loc:], v_ctx)

    out = lax.map(one_row, jnp.arange(rows, dtype=jnp.int32))
    return jnp.moveaxis(out, 0, 1).reshape(B, T, H, Dh)


def odd_mixer(h, hc, w_in, w_out, shift_w, w0, w2, a0, a2, g2, k_k, k_a, r_k, gn_g, gn_b,
              q_g, k_g, rpb, with_ctx_out):
    B, T, _ = h.shape
    u = h @ w_in
    uc = hc @ w_in
    y_rw, yc_rw = rwkv_mixer(u[..., :RW_IN], uc[..., :RW_IN], shift_w, w0, w2, a0, a2, g2,
                             k_k, k_a, r_k, gn_g, gn_b, with_ctx_out)
    q, k, v = natten_qkv(u[..., RW_IN:], q_g, k_g)
    qc, kc, vc = natten_qkv(uc[..., RW_IN:], q_g, k_g)
    y_na = neighbourhood_attention(q, k, v, kc, vc, rpb)
    y = jnp.concatenate([y_rw, y_na.reshape(B, T, -1)], -1) @ w_out
    if not with_ctx_out:
        return y, None
    yc_na = dense_attention(qc, kc, vc)
    yc = jnp.concatenate([yc_rw, yc_na.reshape(B, hc.shape[1], -1)], -1) @ w_out
    return y, yc


def expert_choice_ffn(h, router, w1, w3, w2):
    B, T, D = h.shape
    cap = max(1, EC_FACTOR * T // N_EXPERTS)
    aff = jax.nn.softmax(jnp.einsum('btd,de->bte', h, router).astype(jnp.float32), -1)
    gate, idx = lax.top_k(jnp.swapaxes(aff, 1, 2), cap)
    xs = jax.vmap(lambda hb, ib: hb[ib])(h, idx)
    hid = jax.nn.silu(jnp.einsum('becd,edf->becf', xs, w1)) * jnp.einsum('becd,edf->becf', xs, w3)
    ys = jnp.einsum('becf,efd->becd', hid, w2) * gate[..., None].astype(h.dtype)
    return jax.vmap(lambda yb, ib: jnp.zeros((T, D), yb.dtype).at[ib.reshape(-1)].add(yb.reshape(-1, D)))(ys, idx)


def setup_inputs(seed: int = 0) -> dict:
    key = jax.random.key(seed)
    keys = iter(jax.random.split(key, 64))

    def nrm(shape, scale):
        return jax.random.normal(next(keys), shape, jnp.float32) * scale

    def gain(shape):
        return 1.0 + nrm(shape, 0.05)

    ne, no = (DEPTH + 1) // 2, DEPTH // 2
    d = D_MODEL
    shift_base = jnp.array([0.25, 0.5, 0.25], jnp.float32)[None, :, None]
    return {
        'x': nrm((BATCH, SEQ, d), 1.0),
        'c': nrm((BATCH, d), 1.0),
        'ctx': nrm((BATCH, CTX_LEN, d), 1.0),
        'c_ctx': nrm((d,), 1.0),
        'ada_w': nrm((DEPTH, d, 6 * d), 0.5 * d ** -0.5),
        'ada_b': nrm((DEPTH, 6 * d), 0.02),
        'norm1_g': gain((DEPTH, d)),
        'norm2_g': gain((DEPTH, d)),
        'ev_w_in': nrm((ne, d, EVEN_IN), d ** -0.5),
        'ev_w_out': nrm((ne, EVEN_OUT, d), EVEN_OUT ** -0.5),
        'mla_q_norm': gain((ne, Q_LORA)),
        'mla_w_uq': nrm((ne, Q_LORA, MLA_HEADS * MLA_QK), Q_LORA ** -0.5),
        'mla_kv_norm': gain((ne, KV_LORA)),
        'mla_w_ukv': nrm((ne, KV_LORA, MLA_HEADS * (MLA_NOPE + MLA_V)), KV_LORA ** -0.5),
        'mla_q_g': gain((ne, MLA_QK)),
        'mla_k_g': gain((ne, MLA_QK)),
        'cv_dw_w': nrm((ne, CONV_K, CONV_CH), CONV_K ** -0.5),
        'cv_dw_b': nrm((ne, CONV_CH), 0.02),
        'cv_ln_g': gain((ne, CONV_CH)),
        'cv_ln_b': nrm((ne, CONV_CH), 0.02),
        'od_w_in': nrm((no, d, ODD_IN), d ** -0.5),
        'od_w_out': nrm((no, ODD_OUT, d), ODD_OUT ** -0.5),
        'rw_shift_w': shift_base + nrm((no, SHIFT_K, RW_IN), 0.1),
        'rw_w0': jax.random.uniform(next(keys), (no, 2, RW_DIM), jnp.float32, -4.0, 1.0),
        'rw_w2': nrm((no, 2, DECAY_LORA, RW_DIM), 0.5 * DECAY_LORA ** -0.5),
        'rw_a0': nrm((no, 2, RW_DIM), 0.5),
        'rw_a2': nrm((no, 2, ICLR_LORA, RW_DIM), 0.5 * ICLR_LORA ** -0.5),
        'rw_g2': nrm((no, GATE_LORA, RW_DIM), GATE_LORA ** -0.5),
        'rw_k_k': 0.85 + nrm((no, RW_DIM), 0.05),
        'rw_k_a': gain((no, RW_DIM)),
        'rw_r_k': nrm((no, RW_DIM), 0.3),
        'rw_gn_g': gain((no, RW_DIM)),
        'rw_gn_b': nrm((no, RW_DIM), 0.02),
        'na_q_g': gain((no, NA_DIM)),
        'na_k_g': gain((no, NA_DIM)),
        'na_rpb': nrm((no, NA_HEADS, 2 * WIN_H - 1, 2 * WIN_W - 1), 0.5),
        'moe_router': nrm((DEPTH, d, N_EXPERTS), d ** -0.5),
        'moe_w1': nrm((DEPTH, N_EXPERTS, d, D_EXPERT), d ** -0.5),
        'moe_w3': nrm((DEPTH, N_EXPERTS, d, D_EXPERT), d ** -0.5),
        'moe_w2': nrm((DEPTH, N_EXPERTS, D_EXPERT, d), D_EXPERT ** -0.5),
    }


def reference(x, c, ctx, c_ctx, ada_w, ada_b, norm1_g, norm2_g,
              ev_w_in, ev_w_out, mla_q_norm, mla_w_uq, mla_kv_norm, mla_w_ukv, mla_q_g, mla_k_g,
              cv_dw_w, cv_dw_b, cv_ln_g, cv_ln_b,
              od_w_in, od_w_out, rw_shift_w, rw_w0, rw_w2, rw_a0, rw_a2, rw_g2, rw_k_k, rw_k_a, rw_r_k,
              rw_gn_g, rw_gn_b, na_q_g, na_k_g, na_rpb,
              moe_router, moe_w1, moe_w3, moe_w2):
    rope_cs = axial_rope_tables(x.shape[1], MLA_ROPE)
    for i in range(DEPTH):
        last = i == DEPTH - 1
        j = i // 2
        mod = jnp.einsum('bd,de->be', jax.nn.silu(c), ada_w[i]) + ada_b[i]
        mod_c = jax.nn.silu(c_ctx) @ ada_w[i] + ada_b[i]
        sh1, sc1, g1, sh2, sc2, g2 = jnp.split(mod[:, None, :], 6, -1)
        csh1, csc1, cg1, csh2, csc2, cg2 = jnp.split(mod_c[None, None, :], 6, -1)
        h = rms_norm(x, norm1_g[i]) * (1.0 + sc1) + sh1
        hc = rms_norm(ctx, norm1_g[i]) * (1.0 + csc1) + csh1
        if i % 2 == 0:
            y, yc = even_mixer(h, hc, rope_cs, ev_w_in[j], ev_w_out[j], mla_q_norm[j], mla_w_uq[j],
                               mla_kv_norm[j], mla_w_ukv[j], mla_q_g[j], mla_k_g[j],
                               cv_dw_w[j], cv_dw_b[j], cv_ln_g[j], cv_ln_b[j], not last)
        else:
            y, yc = odd_mixer(h, hc, od_w_in[j], od_w_out[j], rw_shift_w[j], rw_w0[j], rw_w2[j],
                              rw_a0[j], rw_a2[j], rw_g2[j], rw_k_k[j], rw_k_a[j], rw_r_k[j],
                              rw_gn_g[j], rw_gn_b[j], na_q_g[j], na_k_g[j], na_rpb[j], not last)
        x = x + g1 * y
        h = rms_norm(x, norm2_g[i]) * (1.0 + sc2) + sh2
        x = x + g2 * expert_choice_ffn(h, moe_router[i], moe_w1[i], moe_w3[i], moe_w2[i])
        if not last:
            ctx = ctx + cg1 * yc
            hc = rms_norm(ctx, norm2_g[i]) * (1.0 + csc2) + csh2
            ctx = ctx + cg2 * expert_choice_ffn(hc, moe_router[i], moe_w1[i], moe_w3[i], moe_w2[i])
    return x
```

```python
import numpy as np
from contextlib import ExitStack
import concourse.bass as bass
import concourse.mybir as mybir
from concourse.bass_utils import run_bass_kernel_spmd

F32 = mybir.dt.float32
BF16 = mybir.dt.bfloat16
I32 = mybir.dt.int32
U32 = mybir.dt.uint32
ALU = mybir.AluOpType
AF = mybir.ActivationFunctionType
AX = mybir.AxisListType

ENGS = ('pe', 'dve', 'act', 'pool', 'sp')
DMA_RING = 6

D = 1024
T = 4096
L = 256
NT = T + L
NTILE = NT // 128
NLT = T // 128
EPS = 1e-6
E_EV = 1696
E_OD = 3456
RW_IN = 1920
CHUNKED = True


class Res:
    __slots__ = ('lw', 'rd', 'name')

    def __init__(self, name=''):
        self.lw = None
        self.rd = {}
        self.name = name


class Sched:
    def __init__(self, nc, stack):
        self.nc = nc
        self.th = {e: [] for e in ENGS}
        self.cnt = {e: 0 for e in ENGS}
        self.sem = {e: stack.enter_context(nc.semaphore('s_' + e)) for e in ENGS}
        self.ring = {e: [stack.enter_context(nc.semaphore('d_%s%d' % (e, i))) for i in range(DMA_RING)]
                     for e in ('sp', 'pool', 'act')}
        self.dn = {e: 0 for e in ('sp', 'pool', 'act')}
        self.seen = {e: {} for e in ENGS}

    def res(self, name=''):
        return Res(name)

    def _deps(self, reads, writes):
        evs = []
        for r in reads:
            if r.lw is not None:
                evs.append(r.lw)
        for w in writes:
            if w.lw is not None:
                evs.append(w.lw)
            evs.extend(w.rd.items())
        return evs

    def _waits(self, eng, evs):
        seen = self.seen[eng]
        need = {}
        pes = self.sem['pe']
        for s, v in evs:
            if eng == 'pe' and s is pes:
                continue
            if seen.get(s, 0) < v and need.get(s, 0) < v:
                need[s] = v
        for s, v in need.items():
            seen[s] = v
            self.th[eng].append(lambda E, s=s, v=v: E.wait_ge(s, v))

    def _commit(self, ev, reads, writes):
        s, v = ev
        for r in reads:
            if r.rd.get(s, 0) < v:
                r.rd[s] = v
        for w in writes:
            w.lw = ev
            w.rd = {}

    def op(self, eng, fn, reads=(), writes=()):
        self._waits(eng, self._deps(reads, writes))
        self.cnt[eng] += 1
        v = self.cnt[eng]
        s = self.sem[eng]
        self.th[eng].append(lambda E, fn=fn, s=s: fn(E).then_inc(s, 1))
        self._commit((s, v), reads, writes)

    def dma(self, eng, fn, reads=(), writes=()):
        j = self.dn[eng]
        self.dn[eng] += 1
        s = self.ring[eng][j % DMA_RING]
        evs = self._deps(reads, writes)
        if j >= DMA_RING:
            evs.append((s, 16 * (j // DMA_RING)))
        self._waits(eng, evs)
        self.th[eng].append(lambda E, fn=fn, s=s: fn(E).then_inc(s, 16))
        self._commit((s, 16 * (j // DMA_RING + 1)), reads, writes)

    def all_events(self):
        evs = [(self.sem[e], self.cnt[e]) for e in ENGS if self.cnt[e] > 0]
        for q, ring in self.ring.items():
            n = self.dn[q]
            for k, s in enumerate(ring):
                if n > k:
                    evs.append((s, 16 * ((n - k + DMA_RING - 1) // DMA_RING)))
        return evs

    def barrier(self):
        evs = self.all_events()
        for e in ENGS:
            self._waits(e, evs)

    def emit(self):
        nc = self.nc
        th = self.th
        with nc.Block() as block:
            @block.tensor
            def _(e):
                for t in th['pe']:
                    t(e)

            @block.vector
            def _(e):
                for t in th['dve']:
                    t(e)

            @block.scalar
            def _(e):
                for t in th['act']:
                    t(e)

            @block.gpsimd
            def _(e):
                for t in th['pool']:
                    t(e)

            @block.sync
            def _(e):
                for t in th['sp']:
                    t(e)


class Buf:
    __slots__ = ('ap', 'r')

    def __init__(self, ap, r):
        self.ap = ap
        self.r = r

    def __getitem__(self, k):
        return self.ap[k]


def _dsz(dt):
    return 2 if dt == BF16 else 4


class Arena:
    def __init__(self, nc, S, nwords):
        self.t = nc.alloc_sbuf_tensor('arena', [128, nwords], F32)
        self.S = S
        self.n = nwords
        self.top = 0

    def alloc(self, shape, dt=F32, name=''):
        free = int(np.prod(shape[1:]))
        nw = (free * _dsz(dt) + 31) // 32 * 8
        off = self.top
        self.top += nw
        assert self.top <= self.n, ('arena overflow', name, self.top, self.n)
        v = self.t[:, off:off + nw]
        if dt != F32:
            v = v.bitcast(dt)
        v = v[0:shape[0], 0:free]
        if len(shape) == 3:
            v = v.rearrange('p (a b) -> p a b', a=shape[1])
        elif len(shape) == 4:
            v = v.rearrange('p (a b c) -> p a b c', a=shape[1], b=shape[2])
        elif len(shape) == 5:
            v = v.rearrange('p (a b c d) -> p a b c d', a=shape[1], b=shape[2], c=shape[3])
        return Buf(v, Res(name))

    def mark(self):
        return self.top

    def release(self, m):
        self.S.barrier()
        self.top = m


def build(nlayers=4, dbg=()):
    nc = bass.Bass("TRN2", target_bir_lowering=False)
    st = ExitStack()
    S = Sched(nc, st)
    A = Arena(nc, S, 51200)

    def din(name, shape, dt=F32):
        return nc.dram_tensor(name, list(shape), dt, kind="ExternalInput")

    def dscr(name, shape, dt=F32):
        return nc.dram_tensor(name, list(shape), dt, kind=("ExternalOutput" if name in dbg else "Internal"))

    x_d = din('x', [T, D])
    ctx_d = din('ctx', [L, D])
    cc_d = din('cc', [2, D])
    ada_w = din('ada_w', [4, D, 6 * D])
    ada_b = din('ada_b', [4, 6 * D])
    norm1_g = din('norm1_g', [4, D])
    norm2_g = din('norm2_g', [4, D])
    ev_w_in = din('ev_w_in', [2, D, E_EV])
    ev_w_out = din('ev_w_out', [2, D, D])
    mla_q_norm = din('mla_q_norm', [2, 384])
    mla_w_uq = din('mla_w_uq', [2, 384, 768])
    mla_kv_norm = din('mla_kv_norm', [2, 256])
    mla_w_ukv = din('mla_w_ukv', [2, 256, 1024])
    mla_q_g = din('mla_q_g', [2, 96])
    mla_k_g = din('mla_k_g', [2, 96])
    cv_dw_w = din('cv_dw_w', [2, 31, 512])
    cv_dw_b = din('cv_dw_b', [2, 512])
    cv_ln_g = din('cv_ln_g', [2, 512])
    cv_ln_b = din('cv_ln_b', [2, 512])
    od_w_in = din('od_w_in', [2, D, E_OD])
    od_w_out = din('od_w_out', [2, D, D])
    rw_shift_w = din('rw_shift_w', [2, 3, RW_IN])
    rw_w0 = din('rw_w0', [2, 2, 512])
    rw_w2 = din('rw_w2', [2, 2, 64, 512])
    rw_a0 = din('rw_a0', [2, 2, 512])
    rw_a2 = din('rw_a2', [2, 2, 64, 512])
    rw_g2 = din('rw_g2', [2, 128, 512])
    rw_k_k = din('rw_k_k', [2, 512])
    rw_k_a = din('rw_k_a', [2, 512])
    rw_r_k = din('rw_r_k', [2, 512])
    rw_gn_g = din('rw_gn_g', [2, 512])
    rw_gn_b = din('rw_gn_b', [2, 512])
    na_q_g = din('na_q_g', [2, 64])
    na_k_g = din('na_k_g', [2, 64])
    nab_d = din('nab', [2, 8, 128, 8, 256])
    moe_router = din('moe_router', [4, D, 16])
    moe_w1 = din('moe_w1', [4, 16, D, D])
    moe_w3 = din('moe_w3', [4, 16, D, D])
    moe_w2 = din('moe_w2', [4, 16, D, D])
    ident_d = din('ident', [128, 128])
    flip_d = din('flip', [128, 128])
    rope_d = din('rope', [T, 64])
    iota_d = din('iota', [128, 256])
    tri_d = din('tri', [128, 128])
    blk_d = din('blk', [128, 128])
    msk_d = din('msk', [128, 6, 128])

    out_d = nc.dram_tensor('out', [T, D], F32, kind="ExternalOutput")
    xc_d = dscr('xc', [L, D])
    U_d = dscr('U', [NT, E_OD])
    CAT_d = dscr('CAT', [NT, D], BF16)
    H2_d = dscr('H2', [NT, D], BF16)
    QT_d = dscr('QT', [8, 96, NT], BF16)
    KT_d = dscr('KT', [8, 96, NT], BF16)
    VA_d = dscr('VA', [NT, 8 * 65], BF16)
    HGT_d = dscr('HGT', [512, NT])
    HCT_d = dscr('HCT', [512, NT])
    AFF_d = dscr('AFF', [16, NT])
    OPS_d = dscr('OPS', [2, NT, 2560])
    TM_d = dscr('TM', [2, 4, NT, 512], BF16)
    PLB_d = dscr('PLB', [2, NT, 512])
    FT_d = dscr('FT', [2, 4, 64, 8, NT], BF16)
    YS_d = dscr('YS', [2, NT, 512])
    VT_d = dscr('VT', [2, 512, NT])
    YT_d = dscr('YT', [2, 512, NT])
    BON_d = dscr('BON', [NT, 512])
    GG_d = dscr('GG', [NT, 512])

    def AP(t, off, pat):
        return bass.AP(t, off, [list(p) for p in pat])

    def I(eng, method, R=(), W=(), **kw):
        S.op(eng, lambda E, m=method, kw=kw: getattr(E, m)(**kw), R, W)

    def dma(eng, out, in_, R=(), W=(), **kw):
        S.dma(eng, lambda E, out=out, in_=in_, kw=kw: E.dma_start(out=out, in_=in_, **kw), R, W)

    def mm(out, lhsT, rhs, start, stop, R, W):
        S.op('pe', lambda E: E.matmul(out, lhsT, rhs, start=start, stop=stop), R, W)

    def tr(out, in_, ident, R, W):
        S.op('pe', lambda E: E.transpose(out, in_, ident), R, W)

    def xrows(tt):
        if tt < NLT:
            return out_d[tt * 128:(tt + 1) * 128, :]
        return xc_d[(tt - NLT) * 128:(tt - NLT + 1) * 128, :]

    def psum(name, shape, dt=F32):
        return Buf(nc.alloc_psum_tensor(name, list(shape), dt)[:], Res(name))

    PBALL = nc.alloc_psum_tensor('pball', [128, 4096], F32)
    PB = [Buf(PBALL[:, i * 512:(i + 1) * 512], Res('pb%d' % i)) for i in range(8)]
    PT = [Buf(PBALL[:, (6 + i) * 512:(7 + i) * 512].bitcast(BF16), PB[6 + i].r) for i in range(2)]

    XR = [Res('x%d' % t) for t in range(NTILE)]
    UR = [Res('u%d' % t) for t in range(NTILE)]
    CATR = [Res('cat%d' % t) for t in range(NTILE)]
    H2R = [Res('h2%d' % t) for t in range(NTILE)]
    QTR, KTR, VAR, HGTR, HCTR, AFFR = Res('qt'), Res('kt'), Res('va'), Res('hgt'), Res('hct'), Res('aff')
    OPSR, VTR, YTR, BONR, GGR = Res('ops'), Res('vt'), Res('yt'), Res('bon'), Res('gg')
    TMR, PLBR, FTR, YSR = Res('tm'), Res('plb'), Res('ft'), Res('ys')

    ident_f = A.alloc([128, 128], F32, 'ident_f')
    ident_b = A.alloc([128, 128], BF16, 'ident_b')
    flip_f = A.alloc([128, 128], F32, 'flip_f')
    SCB = A.alloc([128, 2, 8, 128], F32, 'scb')
    MODB = A.alloc([128, 8, 1024], F32, 'modb')
    dma('sp', ident_f[:, :], ident_d[:, :], W=[ident_f.r])
    dma('sp', flip_f[:, :], flip_d[:, :], W=[flip_f.r])
    I('dve', 'tensor_copy', R=[ident_f.r], W=[ident_b.r], out=ident_b[:, :], in_=ident_f[:, :])

    for tt in range(NTILE):
        src = x_d[tt * 128:(tt + 1) * 128, :] if tt < NLT else ctx_d[(tt - NLT) * 128:(tt - NLT + 1) * 128, :]
        dma('sp', xrows(tt), src, W=[XR[tt]])

    m0 = A.mark()
    ccol = A.alloc([128, 2, 8], F32, 'ccol')
    dma('sp', ccol[:, :, :], AP(cc_d, 0, [[1, 128], [D, 2], [128, 8]]), W=[ccol.r], allow_slow_non_contiguous=True)
    I('act', 'activation', R=[ccol.r], W=[ccol.r], out=ccol[:, :, :], in_=ccol[:, :, :], func=AF.Silu)
    for w in range(2):
        for kc in range(8):
            I('dve', 'tensor_copy', R=[ccol.r], W=[SCB.r], out=SCB[:, w, kc, :],
              in_=ccol[:, w, kc:kc + 1].to_broadcast([128, 128]))
    A.release(m0)

    def bc_load(eng, dst, t, off, n, R=()):
        dma(eng, dst, AP(t, off, [[0, 128], [1, n]]), R=R, W=[])

    def mod_pass(i, jobs):
        m = A.mark()
        biasb = A.alloc([128, 1024], F32, 'biasb')
        wst = [A.alloc([128, 8, 256], F32, 'wst%d' % k) for k in range(2)]
        k = 0
        for (j, sl, sc_) in jobs:
            dma('sp', biasb[:, :], AP(ada_b, i * 6 * D + j * D, [[0, 128], [1, D]]), W=[biasb.r])
            for n in range(4):
                wb = wst[k % 2]
                dma('sp', wb[:, :, :], AP(ada_w, i * D * 6 * D + j * D + n * 256, [[6 * D, 128], [128 * 6 * D, 8], [1, 256]]),
                    W=[wb.r])
                for w, slot in ((0, sl), (1, sc_)):
                    pb = PB[k % 2 * 2 + w]
                    for kc in range(8):
                        mm(pb[:, 0:256], SCB[:, w, kc, :], wb[:, kc, :], kc == 0, kc == 7, [SCB.r, wb.r], [pb.r])
                    I('dve', 'tensor_tensor', R=[pb.r, biasb.r], W=[MODB.r], out=MODB[:, slot, n * 256:(n + 1) * 256],
                      in0=pb[:, 0:256], in1=biasb[:, n * 256:(n + 1) * 256], op=ALU.add)
                k += 1
        A.release(m)

    def fold_gain(gain_t, i, slots):
        m = A.mark()
        gb = A.alloc([128, 1024], F32, 'gainb')
        dma('sp', gb[:, :], AP(gain_t, i * D, [[0, 128], [1, D]]), W=[gb.r])
        for s in slots:
            I('dve', 'scalar_tensor_tensor', R=[MODB.r, gb.r], W=[MODB.r], out=MODB[:, s, :], in0=MODB[:, s, :],
              scalar=1.0, in1=gb[:, :], op0=ALU.add, op1=ALU.mult)
        A.release(m)

    def rms_rstd(ss_ap, n, rs_ap, R, W, ecol=0):
        I('act', 'activation', R=R, W=R, out=ss_ap, in_=ss_ap, func=AF.Sqrt, scale=1.0 / n, bias=epsb[:, ecol:ecol + 1])
        I('dve', 'reciprocal', R=R, W=W, out=rs_ap, in_=ss_ap)

    epsb = A.alloc([128, 4], F32, 'epsb')
    I('dve', 'memset', W=[epsb.r], ap=epsb[:, 0:1], constant=EPS)
    I('dve', 'memset', W=[epsb.r], ap=epsb[:, 1:2], constant=64e-5)
    I('dve', 'memset', W=[epsb.r], ap=epsb[:, 2:3], constant=1e-12)

    def load_w_bf16(dst, wt, base, K, N, R=()):
        nch = (N + 1023) // 1024
        cw = N // nch
        assert cw * nch == N
        for kc in range(K // 128):
            for c in range(nch):
                dma('pool', dst[:, kc, c * cw:(c + 1) * cw], AP(wt, base + kc * 128 * N + c * cw, [[N, 128], [1, cw]]), R=R, W=[dst.r])

    def norm_mod_tile(xt, G_ap, SH_ap, hf, hout, ssb, R_mod):
        I('act', 'activation', R=[xt.r], W=[hf.r, ssb.r], out=hf[:, :], in_=xt[:, :], func=AF.Square, accum_out=ssb[:, 0:1])
        rms_rstd(ssb[:, 0:1], D, ssb[:, 1:2], [ssb.r], [ssb.r])
        I('dve', 'scalar_tensor_tensor', R=[xt.r, ssb.r] + R_mod, W=[hf.r], out=hf[:, :], in0=xt[:, :],
          scalar=ssb[:, 1:2], in1=G_ap, op0=ALU.mult, op1=ALU.mult)
        I('pool', 'tensor_tensor', R=[hf.r] + R_mod, W=[hout.r], out=hout[:, :], in0=hf[:, :], in1=SH_ap, op=ALU.add)

    def transpose8(src, dstT, pt, ident, n=8):
        for kc in range(n):
            tr(pt[:, kc * 128:(kc + 1) * 128], src[:, kc * 128:(kc + 1) * 128], ident[:, :], [src.r, ident.r], [pt.r])
        I('act', 'copy', R=[pt.r], W=[dstT.r], out=dstT[:, 0:n * 128], in_=pt[:, 0:n * 128])

    def phase_inproj(i, w_in_t, j, E):
        mod_pass(i, [(0, 0, 2), (1, 1, 3)])
        fold_gain(norm1_g, i, [1, 3])
        m = A.mark()
        WIN = A.alloc([128, 8, E], BF16, 'win')
        load_w_bf16(WIN, w_in_t, j * D * E, D, E)
        xts = [A.alloc([128, 1024], F32, 'xt%d' % k) for k in range(2)]
        hfs = [A.alloc([128, 1024], F32, 'hf%d' % k) for k in range(2)]
        hbs = [A.alloc([128, 1024], BF16, 'hb%d' % k) for k in range(2)]
        hTs = [A.alloc([128, 1024], BF16, 'hT%d' % k) for k in range(2)]
        ssbs = [A.alloc([128, 2], F32, 'ssb%d' % k) for k in range(2)]
        uts = [A.alloc([128, E], F32, 'ut%d' % k) for k in range(2)]
        chunks = [(n0, min(E, n0 + 512)) for n0 in range(0, E, 512)]
        for tt in range(NTILE):
            xt = xts[tt % 2]
            ut = uts[tt % 2]
            hf, hb, hT, ssb = hfs[tt % 2], hbs[tt % 2], hTs[tt % 2], ssbs[tt % 2]
            lat = tt < NLT
            dma('sp', xt[:, :], xrows(tt), R=[XR[tt]], W=[xt.r])
            norm_mod_tile(xt, MODB[:, 1 if lat else 3, :], MODB[:, 0 if lat else 2, :], hf, hb, ssb, [MODB.r])
            transpose8(hb, hT, PT[tt % 2], ident_b)
            for ci, (n0, n1) in enumerate(chunks):
                pb = PB[ci % 4]
                for kc in range(8):
                    mm(pb[:, 0:n1 - n0], hT[:, kc * 128:(kc + 1) * 128], WIN[:, kc, n0:n1], kc == 0, kc == 7,
                       [hT.r, WIN.r], [pb.r])
                I('act' if ci % 2 else 'dve', 'tensor_copy' if ci % 2 == 0 else 'copy', R=[pb.r], W=[ut.r],
                  out=ut[:, n0:n1], in_=pb[:, 0:n1 - n0])
            dma('sp', U_d[tt * 128:(tt + 1) * 128, 0:E], ut[:, :], R=[ut.r], W=[UR[tt]])
        A.release(m)

    def phase_mla_prep(j):
        m = A.mark()
        WUQ = A.alloc([128, 3, 768], BF16, 'wuq')
        WUKV = A.alloc([128, 2, 1024], BF16, 'wukv')
        load_w_bf16(WUQ, mla_w_uq, j * 384 * 768, 384, 768)
        load_w_bf16(WUKV, mla_w_ukv, j * 256 * 1024, 256, 1024)
        QNB = A.alloc([128, 384], F32, 'qnb')
        KVNB = A.alloc([128, 256], F32, 'kvnb')
        QGB = A.alloc([128, 96], F32, 'qgb')
        KGB = A.alloc([128, 96], F32, 'kgb')
        cst = Res('mlaconst')
        for dst, t, off, n in ((QNB, mla_q_norm, j * 384, 384), (KVNB, mla_kv_norm, j * 256, 256),
                               (QGB, mla_q_g, j * 96, 96), (KGB, mla_k_g, j * 96, 96)):
            dma('sp', dst[:, :], AP(t, off, [[0, 128], [1, n]]), W=[cst])
        uqs = [A.alloc([128, 672], F32, 'uq%d' % k) for k in range(2)]
        rps = [A.alloc([128, 64], F32, 'rp%d' % k) for k in range(2)]
        junk = A.alloc([128, 768], F32, 'junk')
        st_ = A.alloc([128, 32], F32, 'stats')
        cn = A.alloc([128, 640], BF16, 'cn')
        cT = A.alloc([128, 640], BF16, 'cT')
        qf = A.alloc([128, 8, 96], F32, 'qf')
        kvf = A.alloc([128, 8, 128], F32, 'kvf')
        kn = A.alloc([128, 8, 96], F32, 'kn')
        t1 = A.alloc([128, 8, 32], F32, 't1')
        t2 = A.alloc([128, 8, 32], F32, 't2')
        qkb = [A.alloc([128, 8, 96], BF16, 'qkb%d' % k) for k in range(2)]
        qkT = [A.alloc([96, 8, 128], BF16, 'qkT%d' % k) for k in range(2)]
        vas = [A.alloc([128, 8, 65], BF16, 'va%d' % k) for k in range(2)]
        for k in range(2):
            I('dve', 'memset', W=[vas[k].r], ap=vas[k][:, :, :], constant=1.0)

        def head_norm(src3, n, ssq, rq):
            I('dve', 'tensor_tensor', R=[qf.r, kvf.r], W=[junk.r], out=junk[:, 0:8 * n].rearrange('p (a b) -> p a b', a=8),
              in0=src3, in1=src3, op=ALU.mult)
            I('dve', 'tensor_reduce', R=[junk.r], W=[st_.r], out=ssq,
              in_=junk[:, 0:8 * n].rearrange('p (a b) -> p a b', a=8), axis=AX.X, op=ALU.add)

        def rope(x3, rp, xr):
            cosb = rp[:, 0:32].unsqueeze(1).to_broadcast([128, 8, 32])
            I('dve', 'tensor_tensor', R=[xr, rp.r], W=[t1.r], out=t1[:, :, :], in0=x3, in1=cosb, op=ALU.mult)
            x4 = x3.rearrange('p h (a b) -> p h a b', a=4)
            t4 = t2[:, :, :].rearrange('p h (a b) -> p h a b', a=4)
            s4 = rp[:, 32:64].rearrange('p (a b) -> p a b', a=4)
            for a_dst, a_src in ((0, 1), (1, 0), (2, 3), (3, 2)):
                I('dve', 'tensor_tensor', R=[xr, rp.r], W=[t2.r], out=t4[:, :, a_dst, :], in0=x4[:, :, a_src, :],
                  in1=s4[:, a_dst, :].unsqueeze(1).to_broadcast([128, 8, 8]), op=ALU.mult)
            I('dve', 'tensor_tensor', R=[t1.r, t2.r], W=[xr], out=x3, in0=t1[:, :, :], in1=t2[:, :, :], op=ALU.add)

        for tt in range(NTILE):
            lat = tt < NLT
            uq = uqs[tt % 2]
            rp = rps[tt % 2]
            va = vas[tt % 2]
            dma('sp', uq[:, :], U_d[tt * 128:(tt + 1) * 128, 0:672], R=[UR[tt]], W=[uq.r])
            if lat:
                dma('sp', rp[:, :], rope_d[tt * 128:(tt + 1) * 128, :], W=[rp.r])
            I('act', 'activation', R=[uq.r], W=[junk.r, st_.r], out=junk[:, 0:384], in_=uq[:, 0:384], func=AF.Square,
              accum_out=st_[:, 0:1])
            I('act', 'activation', R=[uq.r], W=[junk.r, st_.r], out=junk[:, 0:256], in_=uq[:, 384:640], func=AF.Square,
              accum_out=st_[:, 1:2])
            I('act', 'activation', R=[uq.r], W=[junk.r, st_.r], out=junk[:, 0:32], in_=uq[:, 640:672], func=AF.Square,
              accum_out=st_[:, 2:3])
            rms_rstd(st_[:, 0:1], 384, st_[:, 4:5], [st_.r], [st_.r])
            rms_rstd(st_[:, 1:2], 256, st_[:, 5:6], [st_.r], [st_.r])
            I('dve', 'scalar_tensor_tensor', R=[uq.r, st_.r, cst], W=[cn.r], out=cn[:, 0:384], in0=uq[:, 0:384],
              scalar=st_[:, 4:5], in1=QNB[:, :], op0=ALU.mult, op1=ALU.mult)
            I('dve', 'scalar_tensor_tensor', R=[uq.r, st_.r, cst], W=[cn.r], out=cn[:, 384:640], in0=uq[:, 384:640],
              scalar=st_[:, 5:6], in1=KVNB[:, :], op0=ALU.mult, op1=ALU.mult)
            transpose8(cn, cT, PT[tt % 2], ident_b, n=5)
            for ci, (n0, n1) in enumerate(((0, 512), (512, 768))):
                pb = PB[ci]
                for kc in range(3):
                    mm(pb[:, 0:n1 - n0], cT[:, kc * 128:(kc + 1) * 128], WUQ[:, kc, n0:n1], kc == 0, kc == 2, [cT.r, WUQ.r], [pb.r])
                I('act', 'copy', R=[pb.r], W=[qf.r], out=qf[:, :, :].rearrange('p a b -> p (a b)')[:, n0:n1], in_=pb[:, 0:n1 - n0])
            for ci in range(2):
                pb = PB[2 + ci]
                for kc in range(2):
                    mm(pb[:, :], cT[:, (3 + kc) * 128:(4 + kc) * 128], WUKV[:, kc, ci * 512:(ci + 1) * 512], kc == 0, kc == 1,
                       [cT.r, WUKV.r], [pb.r])
                I('act', 'copy', R=[pb.r], W=[kvf.r], out=kvf[:, :, :].rearrange('p a b -> p (a b)')[:, ci * 512:(ci + 1) * 512],
                  in_=pb[:, :])
            head_norm(qf[:, :, :], 96, st_[:, 8:16], None)
            rms_rstd(st_[:, 8:16], 96, st_[:, 16:24], [st_.r], [st_.r])
            I('dve', 'tensor_tensor', R=[qf.r, st_.r], W=[qf.r], out=qf[:, :, :], in0=qf[:, :, :],
              in1=st_[:, 16:24].unsqueeze(2).to_broadcast([128, 8, 96]), op=ALU.mult)
            I('dve', 'tensor_tensor', R=[qf.r, cst], W=[qf.r], out=qf[:, :, :], in0=qf[:, :, :],
              in1=QGB[:, :].unsqueeze(1).to_broadcast([128, 8, 96]), op=ALU.mult)
            head_norm(kvf[:, :, 0:64], 64, st_[:, 8:16], None)
            I('dve', 'tensor_scalar', R=[st_.r], W=[st_.r], out=st_[:, 8:16], in0=st_[:, 8:16], scalar1=st_[:, 2:3], scalar2=None,
              op0=ALU.add)
            rms_rstd(st_[:, 8:16], 96, st_[:, 24:32], [st_.r], [st_.r])
            I('dve', 'tensor_tensor', R=[kvf.r, st_.r], W=[kn.r], out=kn[:, :, 0:64], in0=kvf[:, :, 0:64],
              in1=st_[:, 24:32].unsqueeze(2).to_broadcast([128, 8, 64]), op=ALU.mult)
            I('dve', 'tensor_tensor', R=[uq.r, st_.r], W=[kn.r], out=kn[:, :, 64:96],
              in0=uq[:, 640:672].unsqueeze(1).to_broadcast([128, 8, 32]),
              in1=st_[:, 24:32].unsqueeze(2).to_broadcast([128, 8, 32]), op=ALU.mult)
            I('dve', 'tensor_tensor', R=[kn.r, cst], W=[kn.r], out=kn[:, :, :], in0=kn[:, :, :],
              in1=KGB[:, :].unsqueeze(1).to_broadcast([128, 8, 96]), op=ALU.mult)
            if lat:
                rope(qf[:, :, 64:96], rp, qf.r)
                rope(kn[:, :, 64:96], rp, kn.r)
            I('pool', 'tensor_copy', R=[kvf.r], W=[va.r], out=va[:, :, 0:64], in_=kvf[:, :, 64:128])
            dma('sp', VA_d[tt * 128:(tt + 1) * 128, :], va[:, :, :].rearrange('p a b -> p (a b)'), R=[va.r], W=[VAR])
            for which, (src, dst_d, dres) in enumerate(((qf, QT_d, QTR), (kn, KT_d, KTR))):
                qb = qkb[which]
                qT = qkT[which]
                pt = PT[which]
                I('pool', 'tensor_copy', R=[src.r], W=[qb.r], out=qb[:, :, :], in_=src[:, :, :])
                for h in range(8):
                    tr(pt[0:96, h * 128:(h + 1) * 128], qb[:, h, :], ident_b[:, :], [qb.r, ident_b.r], [pt.r])
                I('act', 'copy', R=[pt.r], W=[qT.r], out=qT[:, :, :].rearrange('p a b -> p (a b)'), in_=pt[0:96, :])
                dma('sp', AP(dst_d, tt * 128, [[NT, 96], [96 * NT, 8], [1, 128]]), qT[:, :, :], R=[qT.r], W=[dres])
        A.release(m)

    def phase_attention(dh, scale):
        m = A.mark()
        kTs = [A.alloc([dh, NT], BF16, 'kT%d' % k) for k in range(2)]
        qTs = [A.alloc([dh, NT], BF16, 'qT%d' % k) for k in range(2)]
        vhs = [A.alloc([128, NTILE, 65], BF16, 'vh%d' % k) for k in range(2)]
        pts = [A.alloc([128, 512], BF16, 'pT%d' % k) for k in range(3)]
        oat = [A.alloc([128, NTILE, 64], BF16, 'oat%d' % k) for k in range(2)]
        rc = A.alloc([128, 4], F32, 'rc')
        PO = [PB[2], PB[3], PB[4], PB[5]]
        ei = 0
        for h in range(8):
            kT, qT, vh, oa = kTs[h % 2], qTs[h % 2], vhs[h % 2], oat[h % 2]
            dma('sp', kT[:, :], KT_d[h, 0:dh, :], R=[KTR], W=[kT.r])
            dma('sp', qT[:, :], QT_d[h, 0:dh, :], R=[QTR], W=[qT.r])
            dma('sp', vh[:, :, :], AP(VA_d, h * 65, [[520, 128], [128 * 520, NTILE], [1, 65]]), R=[VAR], W=[vh.r])
            blocks = [(qb * 512, 512, list(range(NTILE))) for qb in range(8)] + [(T, 256, [NLT, NLT + 1])]
            for (q0, nq, kts) in blocks:
                nqs = nq // 128
                def pv(pT, kt, ki):
                    for qs in range(nqs):
                        mm(PO[qs][:, 0:65], pT[:, qs * 128:(qs + 1) * 128], vh[:, kt, :], ki == 0, ki == len(kts) - 1,
                           [pT.r, vh.r], [PO[qs].r])
                prev = None
                for ki, kt in enumerate(kts):
                    pb = PB[ei % 2]
                    pT = pts[ei % 3]
                    ei += 1
                    mm(pb[:, 0:nq], kT[:, kt * 128:(kt + 1) * 128], qT[:, q0:q0 + nq], True, True, [kT.r, qT.r], [pb.r])
                    I('act', 'activation', R=[pb.r], W=[pT.r], out=pT[:, 0:nq], in_=pb[:, 0:nq], func=AF.Exp, scale=scale)
                    if prev is not None:
                        pv(*prev)
                    prev = (pT, kt, ki)
                pv(*prev)
                for qs in range(nqs):
                    I('dve', 'reciprocal', R=[PO[qs].r], W=[rc.r], out=rc[:, qs:qs + 1], in_=PO[qs][:, 64:65])
                    I('dve', 'tensor_scalar', R=[PO[qs].r, rc.r], W=[oa.r], out=oa[:, q0 // 128 + qs, :], in0=PO[qs][:, 0:64],
                      scalar1=rc[:, qs:qs + 1], scalar2=None, op0=ALU.mult)
            for tt in range(NTILE):
                pass
            dma('sp', AP(CAT_d, h * 64, [[D, 128], [128 * D, NTILE], [1, 64]]), oa[:, :, :], R=[oa.r], W=CATR)
        A.release(m)

    def phase_conv(j):
        m = A.mark()
        wraw = A.alloc([32, 512], F32, 'wraw')
        DW = A.alloc([128, 4, 32], F32, 'dw')
        I('dve', 'memset', W=[wraw.r], ap=wraw[:, :], constant=0.0)
        dma('sp', wraw[0:31, :], cv_dw_w[j, :, :], W=[wraw.r])
        dma('sp', wraw[31:32, :], cv_dw_b[j:j + 1, :], W=[wraw.r])
        for ct in range(4):
            tr(PB[0][:, ct * 32:(ct + 1) * 32], wraw[:, ct * 128:(ct + 1) * 128], ident_f[0:32, 0:32], [wraw.r, ident_f.r], [PB[0].r])
        I('act', 'copy', R=[PB[0].r], W=[DW.r], out=DW[:, :, :].rearrange('p a b -> p (a b)'), in_=PB[0][:, 0:128])
        LNG = A.alloc([128, 512], F32, 'lng')
        LNB = A.alloc([128, 512], F32, 'lnb')
        cst = Res('cvconst')
        dma('sp', LNG[:, :], AP(cv_ln_g, j * 512, [[0, 128], [1, 512]]), W=[cst])
        dma('sp', LNB[:, :], AP(cv_ln_b, j * 512, [[0, 128], [1, 512]]), W=[cst])
        m2 = A.mark()
        ugs = [A.alloc([128, 1024], F32, 'ug%d' % k) for k in range(2)]
        sg = A.alloc([128, 512], F32, 'sg')
        hgs = [A.alloc([128, 4, 128], F32, 'hg%d' % k) for k in range(2)]
        for tt in range(NTILE):
            ug = ugs[tt % 2]
            hg = hgs[tt % 2]
            pb = PB[1 + tt % 2]
            dma('sp', ug[:, :], U_d[tt * 128:(tt + 1) * 128, 672:1696], R=[UR[tt]], W=[ug.r])
            I('act', 'activation', R=[ug.r], W=[sg.r], out=sg[:, :], in_=ug[:, 512:1024], func=AF.Sigmoid)
            I('dve', 'tensor_tensor', R=[ug.r, sg.r], W=[sg.r], out=sg[:, :], in0=ug[:, 0:512], in1=sg[:, :], op=ALU.mult)
            for ct in range(4):
                tr(pb[:, ct * 128:(ct + 1) * 128], sg[:, ct * 128:(ct + 1) * 128], ident_f[:, :], [sg.r, ident_f.r], [pb.r])
            I('act', 'copy', R=[pb.r], W=[hg.r], out=hg[:, :, :].rearrange('p a b -> p (a b)'), in_=pb[:, :])
            dma('sp', AP(HGT_d, tt * 128, [[NT, 128], [128 * NT, 4], [1, 128]]), hg[:, :, :], R=[hg.r], W=[HGTR])
        A.release(m2)
        m2 = A.mark()
        cv = A.alloc([128, T + 32], F32, 'cv')
        acc = A.alloc([128, T], F32, 'acc')
        for ct in range(4):
            for (t0, n) in ((0, T), (T, L)):
                I('pool', 'memset', W=[cv.r], ap=cv[:, 0:16], constant=0.0)
                I('pool', 'memset', W=[cv.r], ap=cv[:, 15 + n:15 + n + 16], constant=0.0)
                dma('sp', cv[:, 15:15 + n], HGT_d[ct * 128:(ct + 1) * 128, t0:t0 + n], R=[HGTR], W=[cv.r])
                I('dve', 'tensor_scalar', R=[cv.r, DW.r], W=[acc.r], out=acc[:, 0:n], in0=cv[:, 0:n], scalar1=DW[:, ct, 0:1],
                  scalar2=DW[:, ct, 31:32], op0=ALU.mult, op1=ALU.add)
                for k in range(1, 31):
                    I('dve', 'scalar_tensor_tensor', R=[cv.r, DW.r, acc.r], W=[acc.r], out=acc[:, 0:n], in0=cv[:, k:k + n],
                      scalar=DW[:, ct, k:k + 1], in1=acc[:, 0:n], op0=ALU.mult, op1=ALU.add)
                dma('sp', HCT_d[ct * 128:(ct + 1) * 128, t0:t0 + n], acc[:, 0:n], R=[acc.r], W=[HCTR])
        A.release(m2)
        m2 = A.mark()
        hcs = [A.alloc([128, 4, 128], F32, 'hc%d' % k) for k in range(2)]
        xc = A.alloc([128, 512], F32, 'xc_')
        jk = A.alloc([128, 512], F32, 'jk')
        stt = A.alloc([128, 4], F32, 'stt')
        ocs = [A.alloc([128, 512], BF16, 'oc%d' % k) for k in range(2)]
        for tt in range(NTILE):
            hc = hcs[tt % 2]
            oc = ocs[tt % 2]
            pb = PB[1 + tt % 2]
            dma('sp', hc[:, :, :], AP(HCT_d, tt * 128, [[NT, 128], [128 * NT, 4], [1, 128]]), R=[HCTR], W=[hc.r])
            for ct in range(4):
                tr(pb[:, ct * 128:(ct + 1) * 128], hc[:, ct, :], ident_f[:, :], [hc.r, ident_f.r], [pb.r])
            I('dve', 'tensor_reduce', R=[pb.r], W=[stt.r], out=stt[:, 0:1], in_=pb[:, :], axis=AX.X, op=ALU.add)
            I('dve', 'tensor_scalar', R=[stt.r], W=[stt.r], out=stt[:, 1:2], in0=stt[:, 0:1], scalar1=-1.0 / 512, scalar2=None,
              op0=ALU.mult)
            I('dve', 'tensor_scalar', R=[pb.r, stt.r], W=[xc.r], out=xc[:, :], in0=pb[:, :], scalar1=stt[:, 1:2], scalar2=None,
              op0=ALU.add)
            I('act', 'activation', R=[xc.r], W=[jk.r, stt.r], out=jk[:, :], in_=xc[:, :], func=AF.Square, accum_out=stt[:, 2:3])
            rms_rstd(stt[:, 2:3], 512, stt[:, 3:4], [stt.r], [stt.r])
            I('dve', 'scalar_tensor_tensor', R=[xc.r, stt.r, cst], W=[xc.r], out=xc[:, :], in0=xc[:, :], scalar=stt[:, 3:4],
              in1=LNG[:, :], op0=ALU.mult, op1=ALU.mult)
            I('pool', 'tensor_tensor', R=[xc.r, cst], W=[xc.r], out=xc[:, :], in0=xc[:, :], in1=LNB[:, :], op=ALU.add)
            I('act', 'activation', R=[xc.r], W=[oc.r], out=oc[:, :], in_=xc[:, :], func=AF.Silu)
            dma('sp', CAT_d[tt * 128:(tt + 1) * 128, 512:1024], oc[:, :], R=[oc.r], W=[CATR[tt]])
        A.release(m2)
        A.release(m)

    def phase_outproj(i, w_out_t, j, AFFT):
        mod_pass(i, [(2, 0, 1), (3, 2, 3), (4, 4, 5), (5, 6, 7)])
        fold_gain(norm2_g, i, [4, 5])
        m = A.mark()
        WOUT = A.alloc([128, 8, D], BF16, 'wout')
        load_w_bf16(WOUT, w_out_t, j * D * D, D, D)
        ROUT = A.alloc([128, 8, 16], F32, 'rout')
        dma('sp', ROUT[:, :, :], AP(moe_router, i * D * 16, [[16, 128], [128 * 16, 8], [1, 16]]), W=[ROUT.r])
        cbs = [A.alloc([128, 1024], BF16, 'cb%d' % k) for k in range(2)]
        cTs = [A.alloc([128, 1024], BF16, 'cT5%d' % k) for k in range(2)]
        xts = [A.alloc([128, 1024], F32, 'xt5%d' % k) for k in range(2)]
        hfs = [A.alloc([128, 1024], F32, 'hf5%d' % k) for k in range(2)]
        h2fs = [A.alloc([128, 1024], F32, 'h2f%d' % k) for k in range(2)]
        h2bs = [A.alloc([128, 1024], BF16, 'h2b%d' % k) for k in range(2)]
        h2Ts = [A.alloc([128, 1024], F32, 'h2T%d' % k) for k in range(2)]
        ssbs = [A.alloc([128, 2], F32, 'ssb5%d' % k) for k in range(2)]
        sms = [A.alloc([128, 40], F32, 'sm%d' % k) for k in range(2)]
        for tt in range(NTILE):
            lat = tt < NLT
            w = 0 if lat else 1
            cb, xt, h2b = cbs[tt % 2], xts[tt % 2], h2bs[tt % 2]
            cT, hf, h2f, h2T, ssb, sm = cTs[tt % 2], hfs[tt % 2], h2fs[tt % 2], h2Ts[tt % 2], ssbs[tt % 2], sms[tt % 2]
            dma('sp', cb[:, :], CAT_d[tt * 128:(tt + 1) * 128, :], R=[CATR[tt]], W=[cb.r])
            dma('sp', xt[:, :], xrows(tt), R=[XR[tt]], W=[xt.r])
            transpose8(cb, cT, PT[tt % 2], ident_b)
            for ci in range(2):
                pb = PB[ci]
                for kc in range(8):
                    mm(pb[:, :], cT[:, kc * 128:(kc + 1) * 128], WOUT[:, kc, ci * 512:(ci + 1) * 512], kc == 0, kc == 7,
                       [cT.r, WOUT.r], [pb.r])
                I('dve', 'tensor_tensor', R=[pb.r, MODB.r], W=[hf.r], out=hf[:, ci * 512:(ci + 1) * 512], in0=pb[:, :],
                  in1=MODB[:, 0 + w, ci * 512:(ci + 1) * 512], op=ALU.mult)
            I('pool', 'tensor_tensor', R=[hf.r, xt.r], W=[xt.r], out=xt[:, :], in0=xt[:, :], in1=hf[:, :], op=ALU.add)
            dma('sp', xrows(tt), xt[:, :], R=[xt.r], W=[XR[tt]])
            norm_mod_tile(xt, MODB[:, 4 + w, :], MODB[:, 2 + w, :], hf, h2f, ssb, [MODB.r])
            I('act', 'copy', R=[h2f.r], W=[h2b.r], out=h2b[:, :], in_=h2f[:, :])
            dma('sp', H2_d[tt * 128:(tt + 1) * 128, :], h2b[:, :], R=[h2b.r], W=[H2R[tt]])
            for half in range(2):
                pb = PB[2 + half]
                for kc in range(4):
                    tr(pb[:, kc * 128:(kc + 1) * 128], h2f[:, (half * 4 + kc) * 128:(half * 4 + kc + 1) * 128], ident_f[:, :],
                       [h2f.r, ident_f.r], [pb.r])
                I('act' if half else 'dve', 'copy' if half else 'tensor_copy', R=[pb.r], W=[h2T.r],
                  out=h2T[:, half * 512:(half + 1) * 512], in_=pb[:, :])
            pl = PB[4]
            for kc in range(8):
                mm(pl[:, 0:16], h2T[:, kc * 128:(kc + 1) * 128], ROUT[:, kc, :], kc == 0, kc == 7, [h2T.r, ROUT.r], [pl.r])
            I('dve', 'tensor_reduce', R=[pl.r], W=[sm.r], out=sm[:, 0:1], in_=pl[:, 0:16], axis=AX.X, op=ALU.max)
            I('dve', 'tensor_scalar', R=[sm.r], W=[sm.r], out=sm[:, 1:2], in0=sm[:, 0:1], scalar1=-1.0, scalar2=None, op0=ALU.mult)
            I('act', 'activation', R=[pl.r, sm.r], W=[sm.r], out=sm[:, 8:24], in_=pl[:, 0:16], func=AF.Exp, bias=sm[:, 1:2],
              accum_out=sm[:, 2:3])
            I('dve', 'reciprocal', R=[sm.r], W=[sm.r], out=sm[:, 3:4], in_=sm[:, 2:3])
            I('dve', 'tensor_scalar', R=[sm.r], W=[sm.r], out=sm[:, 24:40], in0=sm[:, 8:24], scalar1=sm[:, 3:4], scalar2=None,
              op0=ALU.mult)
            pa = PB[5]
            tr(pa[0:16, 0:128], sm[:, 24:40], ident_f[:, :], [sm.r, ident_f.r], [pa.r])
            I('act', 'copy', R=[pa.r], W=[AFFT.r], out=AFFT[:, tt * 128:(tt + 1) * 128], in_=pa[0:16, 0:128])
        if 'AFF' in dbg:
            dma('sp', AFF_d[:, :], AFFT[:, :], R=[AFFT.r], W=[AFFR])
        A.release(m)

    def phase_topk(AFFT, GT, IXG, IXS, with_ctx):
        m = A.mark()
        NR = 64
        MX = A.alloc([16, 512 + 32], F32, 'mx')
        IDX = A.alloc([16, 512 + 32], U32, 'idx')
        IDF = A.alloc([16, 512 + 32], F32, 'idf')
        WK = A.alloc([16, T], F32, 'wk')
        I('dve', 'tensor_copy', R=[AFFT.r], W=[WK.r], out=WK[:, :], in_=AFFT[:, 0:T])
        for r in range(NR):
            sl = slice(r * 8, (r + 1) * 8)
            I('dve', 'max', R=[WK.r], W=[MX.r], out=MX[:, sl], in_=WK[:, :])
            I('dve', 'max_index', R=[WK.r, MX.r], W=[IDX.r], out=IDX[:, sl], in_max=MX[:, sl], in_values=WK[:, :])
            if r < NR - 1:
                I('dve', 'match_replace', R=[WK.r, MX.r], W=[WK.r], out=WK[:, :], in_to_replace=MX[:, sl], in_values=WK[:, :],
                  imm_value=-1.0)
        nslot = 4
        if with_ctx:
            nslot = 5
            I('dve', 'tensor_copy', R=[AFFT.r], W=[WK.r], out=WK[:, 0:L], in_=AFFT[:, T:NT])
            for r in range(4):
                sl = slice(512 + r * 8, 512 + (r + 1) * 8)
                I('dve', 'max', R=[WK.r], W=[MX.r], out=MX[:, sl], in_=WK[:, 0:L])
                I('dve', 'max_index', R=[WK.r, MX.r], W=[IDX.r], out=IDX[:, sl], in_max=MX[:, sl], in_values=WK[:, 0:L])
                if r < 3:
                    I('dve', 'match_replace', R=[WK.r, MX.r], W=[WK.r], out=WK[:, 0:L], in_to_replace=MX[:, sl],
                      in_values=WK[:, 0:L], imm_value=-1.0)
        I('dve', 'tensor_copy', R=[IDX.r], W=[IDF.r], out=IDF[:, :], in_=IDX[:, :])
        IXF = A.alloc([128, 5, 16], F32, 'ixf')
        I('dve', 'memset', W=[GT.r], ap=GT[:, :, :], constant=0.0)
        I('dve', 'memset', W=[IXF.r], ap=IXF[:, :, :], constant=0.0)
        for s in range(nslot):
            n = 128 if s < 4 else 32
            pa = PB[s % 2]
            tr(pa[0:n, 0:16], MX[:, s * 128:s * 128 + n], ident_f[0:16, 0:16], [MX.r, ident_f.r], [pa.r])
            tr(pa[0:n, 16:32], IDF[:, s * 128:s * 128 + n], ident_f[0:16, 0:16], [IDF.r, ident_f.r], [pa.r])
            I('act', 'copy', R=[pa.r], W=[GT.r], out=GT[0:n, s, :], in_=pa[0:n, 0:16])
            I('act', 'copy', R=[pa.r], W=[IXF.r], out=IXF[0:n, s, :], in_=pa[0:n, 16:32])
        I('dve', 'tensor_copy', R=[IXF.r], W=[IXS.r], out=IXS[:, :, :], in_=IXF[:, :, :])
        if with_ctx:
            I('dve', 'tensor_scalar', R=[IXF.r], W=[IXF.r], out=IXF[:, 4, :], in0=IXF[:, 4, :], scalar1=float(T), scalar2=None,
              op0=ALU.add)
        I('dve', 'tensor_copy', R=[IXF.r], W=[IXG.r], out=IXG[:, :, :], in_=IXF[:, :, :])
        A.release(m)

    def phase_moe(i, GT, IXG, IXS, with_ctx):
        m = A.mark()
        nslot = 5 if with_ctx else 4
        NC_ = 512 + (32 if with_ctx else 0)
        WBs = [[A.alloc([128, 8, D], BF16, 'w%d_%d' % (k, q)) for q in range(3)] for k in range(2)]
        xss = [[A.alloc([128, 1024], BF16, 'xs%d_%d' % (k, s_)) for s_ in range(nslot)] for k in range(2)]
        XST = A.alloc([128, 8, 544], BF16, 'xst')
        HIDT = A.alloc([128, 8, 544], BF16, 'hidt')
        s1 = [A.alloc([128, 544], F32, 's1_%d' % k) for k in range(2)]
        yss = [A.alloc([128, 1024], F32, 'ys%d' % k) for k in range(2)]
        gi = 0

        wstg = [A.alloc([128, D], F32, 'wstg%d' % k) for k in range(2)]
        wsi = [0]

        def load_weights(e):
            W1, W3, W2 = WBs[e % 2]
            for wt, dst in ((moe_w1, W1), (moe_w3, W3)):
                load_w_bf16(dst, wt, (i * 16 + e) * D * D, D, D)
            for kc in range(8):
                stg = wstg[wsi[0] % 2]
                wsi[0] += 1
                dma('sp', stg[:, :], AP(moe_w2, (i * 16 + e) * D * D + kc * 128 * D, [[D, 128], [1, D]]), W=[stg.r])
                I('act' if kc % 2 else 'pool', 'copy' if kc % 2 else 'tensor_copy', R=[stg.r], W=[W2.r], out=W2[:, kc, :], in_=stg[:, :])

        def issue_gathers(e):
            for s in range(nslot):
                n = 128 if s < 4 else 32
                xs = xss[e % 2][s]
                S.dma('pool', lambda E, xs=xs, s=s, e=e, n=n: E.indirect_dma_start(
                    out=xs[0:n, :], out_offset=None, in_=H2_d[:, :],
                    in_offset=bass.IndirectOffsetOnAxis(ap=IXG[0:n, s, e:e + 1], axis=0)),
                    reads=H2R + [IXG.r], writes=[xs.r])

        load_weights(0)
        issue_gathers(0)
        for e in range(16):
            W1, W3, W2 = WBs[e % 2]
            for s in range(nslot):
                n = 128 if s < 4 else 32
                xs = xss[e % 2][s]
                pt = PT[gi % 2]
                gi += 1
                for kc in range(8):
                    tr(pt[:, kc * 128:kc * 128 + n], xs[0:n, kc * 128:(kc + 1) * 128], ident_b[0:n, 0:n], [xs.r, ident_b.r], [pt.r])
                I('act', 'copy', R=[pt.r], W=[XST.r], out=XST[:, :, s * 128:s * 128 + n],
                  in_=pt[:, :].rearrange('p (a b) -> p a b', a=8)[:, :, 0:n])
            if e + 1 < 16:
                load_weights(e + 1)
                issue_gathers(e + 1)
            for fc in range(8):
                cols = [(0, 512)] + ([(512, 544)] if with_ctx else [])
                pbs = {}
                for wi, Wm in enumerate((W1, W3)):
                    for ci, (c0, c1) in enumerate(cols):
                        pb = PB[wi * 2 + ci]
                        pbs[(wi, ci)] = pb
                        for kc in range(8):
                            mm(pb[:, 0:c1 - c0], Wm[:, kc, fc * 128:(fc + 1) * 128], XST[:, kc, c0:c1], kc == 0, kc == 7,
                               [Wm.r, XST.r], [pb.r])
                sb = s1[fc % 2]
                for ci, (c0, c1) in enumerate(cols):
                    I('act', 'activation', R=[pbs[(0, ci)].r], W=[sb.r], out=sb[:, c0:c1], in_=pbs[(0, ci)][:, 0:c1 - c0], func=AF.Silu)
                    I('dve', 'tensor_tensor', R=[sb.r, pbs[(1, ci)].r], W=[HIDT.r], out=HIDT[:, fc, c0:c1], in0=sb[:, c0:c1],
                      in1=pbs[(1, ci)][:, 0:c1 - c0], op=ALU.mult)
            for s in range(nslot):
                n = 128 if s < 4 else 32
                ys = yss[s % 2]
                for ci in range(2):
                    pb = PB[4 + ci]
                    for fc in range(8):
                        mm(pb[0:n, :], HIDT[:, fc, s * 128:s * 128 + n], W2[:, fc, ci * 512:(ci + 1) * 512], fc == 0, fc == 7,
                           [HIDT.r, W2.r], [pb.r])
                    I('dve', 'scalar_tensor_tensor', R=[pb.r, GT.r, MODB.r], W=[ys.r], out=ys[0:n, ci * 512:(ci + 1) * 512],
                      in0=pb[0:n, :], scalar=GT[0:n, s, e:e + 1], in1=MODB[0:n, 6 + (0 if s < 4 else 1), ci * 512:(ci + 1) * 512],
                      op0=ALU.mult, op1=ALU.mult)
                tgt = out_d if s < 4 else xc_d
                xres = XR[0:NLT] if s < 4 else XR[NLT:NTILE]
                S.dma('pool', lambda E, ys=ys, s=s, e=e, n=n, tgt=tgt: E.indirect_dma_start(
                    out=tgt[:, :], out_offset=bass.IndirectOffsetOnAxis(ap=IXS[0:n, s, e:e + 1], axis=0),
                    in_=ys[0:n, :], in_offset=None, compute_op=ALU.add),
                    reads=[ys.r, IXS.r], writes=xres)
        A.release(m)


    def fstep(tt):
        return L + tt * 128 if tt < NLT else (tt - NLT) * 128

    def bstep(tt):
        return L + (NLT - 1 - tt) * 128 if tt < NLT else (1 - (tt - NLT)) * 128

    def phase_rwkv_prep(j):
        m = A.mark()
        SW = A.alloc([128, 3, RW_IN], F32, 'sw')
        cst = Res('rwconst')
        for k in range(3):
            dma('sp', SW[:, k, :], AP(rw_shift_w, (j * 3 + k) * RW_IN, [[0, 128], [1, RW_IN]]), W=[cst])
        W0B = A.alloc([128, 2, 512], F32, 'w0b')
        A0B = A.alloc([128, 2, 512], F32, 'a0b')
        KKB = A.alloc([128, 512], F32, 'kkb')
        KAB = A.alloc([128, 512], F32, 'kab')
        RKB = A.alloc([128, 512], F32, 'rkb')
        dma('sp', W0B[:, :, :].rearrange('p a b -> p (a b)'), AP(rw_w0, j * 1024, [[0, 128], [1, 1024]]), W=[cst])
        dma('sp', A0B[:, :, :].rearrange('p a b -> p (a b)'), AP(rw_a0, j * 1024, [[0, 128], [1, 1024]]), W=[cst])
        dma('sp', KKB[:, :], AP(rw_k_k, j * 512, [[0, 128], [1, 512]]), W=[cst])
        dma('sp', KAB[:, :], AP(rw_k_a, j * 512, [[0, 128], [1, 512]]), W=[cst])
        dma('sp', RKB[:, :], AP(rw_r_k, j * 512, [[0, 128], [1, 512]]), W=[cst])
        W2 = A.alloc([128, 512], BF16, 'w2')
        A2 = A.alloc([128, 512], BF16, 'a2')
        G2 = A.alloc([128, 512], BF16, 'g2')
        dma('pool', W2[:, :], AP(rw_w2, j * 128 * 512, [[512, 128], [1, 512]]), W=[W2.r])
        dma('pool', A2[:, :], AP(rw_a2, j * 128 * 512, [[512, 128], [1, 512]]), W=[A2.r])
        dma('pool', G2[:, :], AP(rw_g2, j * 128 * 512, [[512, 128], [1, 512]]), W=[G2.r])
        um = A.alloc([128, RW_IN], F32, 'um')
        u0 = A.alloc([128, RW_IN], F32, 'u0')
        up = A.alloc([128, RW_IN], F32, 'up')
        L3 = A.alloc([128, 384], BF16, 'l3')
        L3T = A.alloc([128, 384], BF16, 'l3t')
        OPST = A.alloc([128, 2, 5, 512], F32, 'opst')
        OPF = A.alloc([128, 5, 512], F32, 'opf')
        kk = A.alloc([128, 8, 64], F32, 'kk')
        tA = A.alloc([128, 512], F32, 'tA')
        tB = A.alloc([128, 512], F32, 'tB')
        av = [A.alloc([128, 512], F32, 'av%d' % d) for d in range(2)]
        rr = A.alloc([128, 512], F32, 'rr')
        st_ = A.alloc([128, 32], F32, 'rst')
        gg = A.alloc([128, 512], F32, 'ggt')
        bon = A.alloc([128, 8, 64], F32, 'bon')
        vf = A.alloc([128, 512], F32, 'vf')
        vts = [A.alloc([128, 4, 128], F32, 'vts%d' % d) for d in range(2)]
        for tt in range(NTILE):
            first = tt in (0, NLT)
            lastt = tt in (NLT - 1, NTILE - 1)
            r0 = tt * 128
            dma('sp', u0[:, :], U_d[r0:r0 + 128, 0:RW_IN], R=[UR[tt]], W=[u0.r])
            if first:
                I('pool', 'memset', W=[um.r], ap=um[:, :], constant=0.0)
                dma('sp', um[1:128, :], U_d[r0:r0 + 127, 0:RW_IN], R=[UR[tt]], W=[um.r])
            else:
                dma('sp', um[:, :], U_d[r0 - 1:r0 + 127, 0:RW_IN], R=[UR[tt], UR[tt - 1]], W=[um.r])
            if lastt:
                I('pool', 'memset', W=[up.r], ap=up[:, :], constant=0.0)
                dma('sp', up[0:127, :], U_d[r0 + 1:r0 + 128, 0:RW_IN], R=[UR[tt]], W=[up.r])
            else:
                dma('sp', up[:, :], U_d[r0 + 1:r0 + 129, 0:RW_IN], R=[UR[tt], UR[tt + 1]], W=[up.r])
            I('dve', 'tensor_tensor', R=[u0.r, cst], W=[u0.r], out=u0[:, :], in0=u0[:, :], in1=SW[:, 1, :], op=ALU.mult)
            I('pool', 'tensor_tensor', R=[um.r, cst], W=[um.r], out=um[:, :], in0=um[:, :], in1=SW[:, 0, :], op=ALU.mult)
            I('pool', 'tensor_tensor', R=[up.r, cst], W=[up.r], out=up[:, :], in0=up[:, :], in1=SW[:, 2, :], op=ALU.mult)
            I('dve', 'tensor_tensor', R=[u0.r, um.r], W=[u0.r], out=u0[:, :], in0=u0[:, :], in1=um[:, :], op=ALU.add)
            I('dve', 'tensor_tensor', R=[u0.r, up.r], W=[u0.r], out=u0[:, :], in0=u0[:, :], in1=up[:, :], op=ALU.add)
            r_ap, k_ap, v_ap = u0[:, 0:512], u0[:, 512:1024], u0[:, 1024:1536]
            I('act', 'activation', R=[u0.r], W=[L3.r], out=L3[:, 0:128], in_=u0[:, 1536:1664], func=AF.Tanh)
            I('act', 'activation', R=[u0.r], W=[L3.r], out=L3[:, 256:384], in_=u0[:, 1792:1920], func=AF.Sigmoid)
            I('act', 'copy', R=[u0.r], W=[L3.r], out=L3[:, 128:256], in_=u0[:, 1664:1792])
            transpose8(L3, L3T, PT[tt % 2], ident_b, n=3)
            I('pool', 'tensor_copy', R=[u0.r], W=[OPST.r], out=OPST[:, 0, 0, :], in_=r_ap)
            I('pool', 'tensor_copy', R=[u0.r], W=[OPF.r], out=OPF[:, 0, :], in_=r_ap)
            I('dve', 'tensor_tensor', R=[u0.r, cst], W=[kk.r], out=kk[:, :, :].rearrange('p a b -> p (a b)'), in0=k_ap, in1=KKB[:, :],
              op=ALU.mult)
            I('dve', 'tensor_tensor', R=[kk.r], W=[tA.r], out=tA[:, :].rearrange('p (a b) -> p a b', a=8), in0=kk[:, :, :],
              in1=kk[:, :, :], op=ALU.mult)
            I('dve', 'tensor_reduce', R=[tA.r], W=[st_.r], out=st_[:, 0:8], in_=tA[:, :].rearrange('p (a b) -> p a b', a=8),
              axis=AX.X, op=ALU.add)
            rms_rstd(st_[:, 0:8], 1.0, st_[:, 8:16], [st_.r], [st_.r], ecol=2)
            I('dve', 'tensor_tensor', R=[kk.r, st_.r], W=[kk.r], out=kk[:, :, :], in0=kk[:, :, :],
              in1=st_[:, 8:16].unsqueeze(2).to_broadcast([128, 8, 64]), op=ALU.mult)
            kkf = kk[:, :, :].rearrange('p a b -> p (a b)')
            I('pool', 'tensor_scalar', R=[kk.r], W=[OPST.r], out=OPST[:, 0, 3, :], in0=kkf, scalar1=-1.0, scalar2=None, op0=ALU.mult)
            I('pool', 'tensor_scalar', R=[kk.r], W=[OPF.r], out=OPF[:, 3, :], in0=kkf, scalar1=-1.0, scalar2=None, op0=ALU.mult)
            I('pool', 'tensor_tensor', R=[u0.r, cst], W=[rr.r], out=rr[:, :], in0=r_ap, in1=RKB[:, :], op=ALU.mult)
            for d in range(2):
                dst = OPST[:, 0, :, :] if d == 0 else OPF[:, :, :]
                dres = OPST.r if d == 0 else OPF.r
                pw, pa_ = PB[0 + d], PB[2 + d]
                mm(pw[:, :], L3T[d * 64:(d + 1) * 64, 0:128], W2[d * 64:(d + 1) * 64, :], True, True, [L3T.r, W2.r], [pw.r])
                mm(pa_[:, :], L3T[d * 64:(d + 1) * 64, 128:256], A2[d * 64:(d + 1) * 64, :], True, True, [L3T.r, A2.r], [pa_.r])
                I('dve', 'tensor_tensor', R=[pw.r, cst], W=[tA.r], out=tA[:, :], in0=pw[:, :], in1=W0B[:, d, :], op=ALU.add)
                I('act', 'activation', R=[tA.r], W=[tA.r], out=tA[:, :], in_=tA[:, :], func=AF.Sigmoid)
                I('act', 'activation', R=[tA.r], W=[dres], out=dst[:, 1, :], in_=tA[:, :], func=AF.Exp, scale=-float(np.exp(-0.5)))
                a_t = av[d]
                I('dve', 'tensor_tensor', R=[pa_.r, cst], W=[a_t.r], out=a_t[:, :], in0=pa_[:, :], in1=A0B[:, d, :], op=ALU.add)
                I('act', 'activation', R=[a_t.r], W=[a_t.r], out=a_t[:, :], in_=a_t[:, :], func=AF.Sigmoid)
                I('dve', 'scalar_tensor_tensor', R=[a_t.r, cst], W=[tB.r], out=tB[:, :], in0=a_t[:, :], scalar=-1.0, in1=KAB[:, :],
                  op0=ALU.add, op1=ALU.mult)
                I('dve', 'scalar_tensor_tensor', R=[tB.r, u0.r], W=[dres], out=dst[:, 2, :], in0=tB[:, :], scalar=1.0, in1=k_ap,
                  op0=ALU.add, op1=ALU.mult)
                I('pool', 'tensor_tensor', R=[kk.r, a_t.r], W=[dres], out=dst[:, 4, :], in0=kkf, in1=a_t[:, :], op=ALU.mult)
                I('dve', 'tensor_tensor', R=[rr.r, dres], W=[tB.r], out=tB[:, :], in0=rr[:, :], in1=dst[:, 2, :], op=ALU.mult)
                I('dve', 'tensor_reduce', R=[tB.r], W=[st_.r], out=st_[:, 16 + d * 8:24 + d * 8],
                  in_=tB[:, :].rearrange('p (a b) -> p a b', a=8), axis=AX.X, op=ALU.add)
            I('dve', 'tensor_tensor', R=[st_.r], W=[st_.r], out=st_[:, 16:24], in0=st_[:, 16:24], in1=st_[:, 24:32], op=ALU.add)
            I('dve', 'tensor_tensor', R=[st_.r, u0.r], W=[bon.r], out=bon[:, :, :], in0=v_ap.rearrange('p (a b) -> p a b', a=8),
              in1=st_[:, 16:24].unsqueeze(2).to_broadcast([128, 8, 64]), op=ALU.mult)
            dma('sp', BON_d[r0:r0 + 128, :], bon[:, :, :].rearrange('p a b -> p (a b)'), R=[bon.r], W=[BONR])
            pg = PB[4]
            mm(pg[:, :], L3T[:, 256:384], G2[:, :], True, True, [L3T.r, G2.r], [pg.r])
            I('act', 'copy', R=[pg.r], W=[gg.r], out=gg[:, :], in_=pg[:, :])
            dma('sp', GG_d[r0:r0 + 128, :], gg[:, :], R=[gg.r], W=[GGR])
            for q in range(5):
                pf = PB[q % 2]
                mm(pf[:, :], flip_f[:, :], OPF[:, q, :], True, True, [flip_f.r, OPF.r], [pf.r])
                I('act' if q % 2 else 'dve', 'copy' if q % 2 else 'tensor_copy', R=[pf.r], W=[OPST.r], out=OPST[:, 1, q, :], in_=pf[:, :])
            I('pool', 'tensor_copy', R=[u0.r], W=[vf.r], out=vf[:, :], in_=v_ap)
            for d in range(2):
                pv = PB[2 + d]
                for g in range(4):
                    mm(pv[:, g * 128:(g + 1) * 128], vf[:, g * 128:(g + 1) * 128], (ident_f if d == 0 else flip_f)[:, :], True, True,
                       [vf.r, ident_f.r, flip_f.r], [pv.r])
                I('act', 'copy', R=[pv.r], W=[vts[d].r], out=vts[d][:, :, :].rearrange('p a b -> p (a b)'), in_=pv[:, :])
                s0 = fstep(tt) if d == 0 else bstep(tt)
                dma('sp', AP(VT_d, d * 512 * NT + s0, [[NT, 128], [128 * NT, 4], [1, 128]]), vts[d][:, :, :], R=[vts[d].r], W=[VTR])
                dma('sp', OPS_d[d, s0:s0 + 128, :], OPST[:, d, :, :].rearrange('p a b -> p (a b)'), R=[OPST.r], W=[OPSR])
        A.release(m)

    def phase_rwkv_scan():
        m = A.mark()
        NS = 4
        St = A.alloc([128, 2, 4, 64], F32, 'state')
        tmp = A.alloc([128, 2, 4, 64], F32, 'stmp')
        tk = [A.alloc([128, 2, 4, 64], F32, 'stk%d' % k) for k in range(2)]
        sa = A.alloc([128, 2, 4], F32, 'sa')
        BCT = [A.alloc([128, 2, NS, 20, 64], F32, 'bct%d' % k) for k in range(2)]
        VB = [A.alloc([128, 2, 4, 128], F32, 'vb%d' % k) for k in range(2)]
        YB = [A.alloc([128, 2, 4, 128], F32, 'yb%d' % k) for k in range(2)]
        I('dve', 'memset', W=[St.r], ap=St[:, :, :, :], constant=0.0)
        bc3 = lambda ap: ap.unsqueeze(3).to_broadcast([128, 2, 4, 64])
        for s in range(NT):
            blk = s // 128
            vb, yb = VB[blk % 2], YB[blk % 2]
            if s % 128 == 0:
                for d in range(2):
                    dma('sp', vb[:, d, :, :], AP(VT_d, d * 512 * NT + s, [[NT, 128], [128 * NT, 4], [1, 128]]), R=[VTR], W=[vb.r])
            bct = BCT[(s // NS) % 2]
            if s % NS == 0:
                for d in range(2):
                    for h2 in range(2):
                        dma('sp', bct[h2 * 64:(h2 + 1) * 64, d, :, :, :],
                            AP(OPS_d, (d * NT + s) * 2560 + h2 * 64, [[0, 64], [2560, NS], [128, 20], [1, 64]]), R=[OPSR], W=[bct.r])
            sl = s % NS
            op = lambda q: bct[:, :, sl, q * 4:(q + 1) * 4, :]
            c = s % 128
            t2 = tk[s % 2]
            I('pool', 'tensor_tensor', R=[bct.r, vb.r], W=[t2.r], out=t2[:, :, :, :], in0=op(2), in1=bc3(vb[:, :, :, c]), op=ALU.mult)
            I('dve', 'tensor_tensor', R=[St.r, bct.r], W=[tmp.r], out=tmp[:, :, :, :], in0=St[:, :, :, :], in1=op(3), op=ALU.mult)
            I('dve', 'tensor_reduce', R=[tmp.r], W=[sa.r], out=sa[:, :, :], in_=tmp[:, :, :, :], axis=AX.X, op=ALU.add)
            I('dve', 'tensor_tensor', R=[St.r, bct.r], W=[St.r], out=St[:, :, :, :], in0=St[:, :, :, :], in1=op(1), op=ALU.mult)
            I('dve', 'tensor_tensor', R=[sa.r, bct.r], W=[tmp.r], out=tmp[:, :, :, :], in0=op(4), in1=bc3(sa[:, :, :]), op=ALU.mult)
            I('dve', 'tensor_tensor', R=[St.r, tmp.r], W=[St.r], out=St[:, :, :, :], in0=St[:, :, :, :], in1=tmp[:, :, :, :], op=ALU.add)
            I('dve', 'tensor_tensor', R=[St.r, t2.r], W=[St.r], out=St[:, :, :, :], in0=St[:, :, :, :], in1=t2[:, :, :, :], op=ALU.add)
            I('dve', 'tensor_tensor', R=[St.r, bct.r], W=[tmp.r], out=tmp[:, :, :, :], in0=St[:, :, :, :], in1=op(0), op=ALU.mult)
            I('dve', 'tensor_reduce', R=[tmp.r], W=[yb.r], out=yb[:, :, :, c], in_=tmp[:, :, :, :], axis=AX.X, op=ALU.add)
            if c == 127:
                for d in range(2):
                    dma('sp', AP(YT_d, d * 512 * NT + s - 127, [[NT, 128], [128 * NT, 4], [1, 128]]), yb[:, d, :, :], R=[yb.r], W=[YTR])
        A.release(m)

    def phase_rwkv_post(j):
        m = A.mark()
        GNG = A.alloc([128, 512], F32, 'gng')
        GNB = A.alloc([128, 512], F32, 'gnb')
        cst = Res('gnconst')
        dma('sp', GNG[:, :], AP(rw_gn_g, j * 512, [[0, 128], [1, 512]]), W=[cst])
        dma('sp', GNB[:, :], AP(rw_gn_b, j * 512, [[0, 128], [1, 512]]), W=[cst])
        yfs = [A.alloc([128, 4, 128], F32, 'yf%d' % k) for k in range(2)]
        ybs = [A.alloc([128, 4, 128], F32, 'yb_%d' % k) for k in range(2)]
        zb = A.alloc([128, 512], F32, 'zb')
        bons = [A.alloc([128, 512], F32, 'bon%d' % k) for k in range(2)]
        ggs = [A.alloc([128, 512], F32, 'gg%d' % k) for k in range(2)]
        y = A.alloc([128, 8, 64], F32, 'ysum')
        sq = A.alloc([128, 8, 64], F32, 'ysq')
        st_ = A.alloc([128, 32], F32, 'gst')
        ocs = [A.alloc([128, 512], BF16, 'orw%d' % k) for k in range(2)]
        for tt in range(NTILE):
            yf, yb, bo, gt, oc = yfs[tt % 2], ybs[tt % 2], bons[tt % 2], ggs[tt % 2], ocs[tt % 2]
            r0 = tt * 128
            dma('sp', yf[:, :, :], AP(YT_d, fstep(tt), [[NT, 128], [128 * NT, 4], [1, 128]]), R=[YTR], W=[yf.r])
            dma('sp', yb[:, :, :], AP(YT_d, 512 * NT + bstep(tt), [[NT, 128], [128 * NT, 4], [1, 128]]), R=[YTR], W=[yb.r])
            dma('sp', bo[:, :], BON_d[r0:r0 + 128, :], R=[BONR], W=[bo.r])
            dma('sp', gt[:, :], GG_d[r0:r0 + 128, :], R=[GGR], W=[gt.r])
            pz, py = PB[tt % 2], PB[2 + tt % 2]
            for g in range(4):
                mm(pz[:, g * 128:(g + 1) * 128], yb[:, g, :], ident_f[:, :], True, True, [yb.r, ident_f.r], [pz.r])
            I('act', 'copy', R=[pz.r], W=[zb.r], out=zb[:, :], in_=pz[:, :])
            mm(py[:, :], flip_f[:, :], zb[:, :], True, False, [flip_f.r, zb.r], [py.r])
            for g in range(4):
                mm(py[:, g * 128:(g + 1) * 128], yf[:, g, :], ident_f[:, :], False, g == 3, [yf.r, ident_f.r], [py.r])
            yfl = y[:, :, :].rearrange('p a b -> p (a b)')
            I('dve', 'tensor_tensor', R=[py.r, bo.r], W=[y.r], out=yfl, in0=py[:, :], in1=bo[:, :], op=ALU.add)
            I('dve', 'tensor_reduce', R=[y.r], W=[st_.r], out=st_[:, 0:8], in_=y[:, :, :], axis=AX.X, op=ALU.add)
            I('dve', 'tensor_scalar', R=[st_.r], W=[st_.r], out=st_[:, 8:16], in0=st_[:, 0:8], scalar1=-1.0 / 64, scalar2=None, op0=ALU.mult)
            I('dve', 'tensor_tensor', R=[y.r, st_.r], W=[y.r], out=y[:, :, :], in0=y[:, :, :],
              in1=st_[:, 8:16].unsqueeze(2).to_broadcast([128, 8, 64]), op=ALU.add)
            I('pool', 'tensor_tensor', R=[y.r], W=[sq.r], out=sq[:, :, :], in0=y[:, :, :], in1=y[:, :, :], op=ALU.mult)
            I('dve', 'tensor_reduce', R=[sq.r], W=[st_.r], out=st_[:, 16:24], in_=sq[:, :, :], axis=AX.X, op=ALU.add)
            rms_rstd(st_[:, 16:24], 64, st_[:, 24:32], [st_.r], [st_.r], ecol=1)
            I('dve', 'tensor_tensor', R=[y.r, st_.r], W=[y.r], out=y[:, :, :], in0=y[:, :, :],
              in1=st_[:, 24:32].unsqueeze(2).to_broadcast([128, 8, 64]), op=ALU.mult)
            I('dve', 'tensor_tensor', R=[y.r, cst], W=[y.r], out=yfl, in0=yfl, in1=GNG[:, :], op=ALU.mult)
            I('pool', 'tensor_tensor', R=[y.r, cst], W=[y.r], out=yfl, in0=yfl, in1=GNB[:, :], op=ALU.add)
            I('dve', 'tensor_tensor', R=[y.r, gt.r], W=[oc.r], out=oc[:, :], in0=yfl, in1=gt[:, :], op=ALU.mult)
            dma('sp', CAT_d[r0:r0 + 128, 0:512], oc[:, :], R=[oc.r], W=[CATR[tt]])
        A.release(m)


    def phase_rwkv_prep2(j):
        m = A.mark()
        SW = A.alloc([128, 3, RW_IN], F32, 'sw')
        cst = Res('rwconst')
        for k in range(3):
            dma('sp', SW[:, k, :], AP(rw_shift_w, (j * 3 + k) * RW_IN, [[0, 128], [1, RW_IN]]), W=[cst])
        W0B = A.alloc([128, 2, 512], F32, 'w0b')
        A0B = A.alloc([128, 2, 512], F32, 'a0b')
        KKB = A.alloc([128, 512], F32, 'kkb')
        KAB = A.alloc([128, 512], F32, 'kab')
        RKB = A.alloc([128, 512], F32, 'rkb')
        TRI = A.alloc([128, 128], F32, 'tri')
        BLK = A.alloc([128, 128], F32, 'blk')
        dma('sp', W0B[:, :, :].rearrange('p a b -> p (a b)'), AP(rw_w0, j * 1024, [[0, 128], [1, 1024]]), W=[cst])
        dma('sp', A0B[:, :, :].rearrange('p a b -> p (a b)'), AP(rw_a0, j * 1024, [[0, 128], [1, 1024]]), W=[cst])
        dma('sp', KKB[:, :], AP(rw_k_k, j * 512, [[0, 128], [1, 512]]), W=[cst])
        dma('sp', KAB[:, :], AP(rw_k_a, j * 512, [[0, 128], [1, 512]]), W=[cst])
        dma('sp', RKB[:, :], AP(rw_r_k, j * 512, [[0, 128], [1, 512]]), W=[cst])
        dma('sp', TRI[:, :], tri_d[:, :], W=[cst])
        dma('sp', BLK[:, :], blk_d[:, :], W=[cst])
        W2 = A.alloc([128, 512], BF16, 'w2')
        A2 = A.alloc([128, 512], BF16, 'a2')
        G2 = A.alloc([128, 512], BF16, 'g2')
        dma('pool', W2[:, :], AP(rw_w2, j * 128 * 512, [[512, 128], [1, 512]]), W=[W2.r])
        dma('pool', A2[:, :], AP(rw_a2, j * 128 * 512, [[512, 128], [1, 512]]), W=[A2.r])
        dma('pool', G2[:, :], AP(rw_g2, j * 128 * 512, [[512, 128], [1, 512]]), W=[G2.r])
        um = A.alloc([128, RW_IN], F32, 'um')
        u0 = A.alloc([128, RW_IN], F32, 'u0')
        up = A.alloc([128, RW_IN], F32, 'up')
        L3 = A.alloc([128, 384], BF16, 'l3')
        L3T = A.alloc([128, 384], BF16, 'l3t')
        OPST = A.alloc([128, 2, 6, 512], F32, 'opst')
        OPF = A.alloc([128, 6, 512], F32, 'opf')
        kk = A.alloc([128, 8, 64], F32, 'kk')
        tA = A.alloc([128, 512], F32, 'tA')
        tB = A.alloc([128, 512], F32, 'tB')
        av = [A.alloc([128, 512], F32, 'av%d' % d) for d in range(2)]
        rr = A.alloc([128, 512], F32, 'rr')
        st_ = A.alloc([128, 32], F32, 'rst')
        gg = A.alloc([128, 512], F32, 'ggt')
        bon = A.alloc([128, 8, 64], F32, 'bon')
        CUM = A.alloc([128, 512], F32, 'cum')
        EE = [A.alloc([128, 512], F32, 'ee%d' % k) for k in range(4)]
        PLBt = A.alloc([128, 512], F32, 'plbt')
        TMb = [A.alloc([128, 512], BF16, 'tmb%d' % k) for k in range(7)]
        FTs = [A.alloc([64, 8, 128], BF16, 'fts%d' % k) for k in range(2)]
        fti = 0
        for tt in range(NTILE):
            first = tt in (0, NLT)
            lastt = tt in (NLT - 1, NTILE - 1)
            r0 = tt * 128
            dma('sp', u0[:, :], U_d[r0:r0 + 128, 0:RW_IN], R=[UR[tt]], W=[u0.r])
            if first:
                I('pool', 'memset', W=[um.r], ap=um[:, :], constant=0.0)
                dma('sp', um[1:128, :], U_d[r0:r0 + 127, 0:RW_IN], R=[UR[tt]], W=[um.r])
            else:
                dma('sp', um[:, :], U_d[r0 - 1:r0 + 127, 0:RW_IN], R=[UR[tt], UR[tt - 1]], W=[um.r])
            if lastt:
                I('pool', 'memset', W=[up.r], ap=up[:, :], constant=0.0)
                dma('sp', up[0:127, :], U_d[r0 + 1:r0 + 128, 0:RW_IN], R=[UR[tt]], W=[up.r])
            else:
                dma('sp', up[:, :], U_d[r0 + 1:r0 + 129, 0:RW_IN], R=[UR[tt], UR[tt + 1]], W=[up.r])
            I('dve', 'tensor_tensor', R=[u0.r, cst], W=[u0.r], out=u0[:, :], in0=u0[:, :], in1=SW[:, 1, :], op=ALU.mult)
            I('pool', 'tensor_tensor', R=[um.r, cst], W=[um.r], out=um[:, :], in0=um[:, :], in1=SW[:, 0, :], op=ALU.mult)
            I('pool', 'tensor_tensor', R=[up.r, cst], W=[up.r], out=up[:, :], in0=up[:, :], in1=SW[:, 2, :], op=ALU.mult)
            I('dve', 'tensor_tensor', R=[u0.r, um.r], W=[u0.r], out=u0[:, :], in0=u0[:, :], in1=um[:, :], op=ALU.add)
            I('dve', 'tensor_tensor', R=[u0.r, up.r], W=[u0.r], out=u0[:, :], in0=u0[:, :], in1=up[:, :], op=ALU.add)
            r_ap, k_ap, v_ap = u0[:, 0:512], u0[:, 512:1024], u0[:, 1024:1536]
            I('act', 'activation', R=[u0.r], W=[L3.r], out=L3[:, 0:128], in_=u0[:, 1536:1664], func=AF.Tanh)
            I('act', 'activation', R=[u0.r], W=[L3.r], out=L3[:, 256:384], in_=u0[:, 1792:1920], func=AF.Sigmoid)
            I('act', 'copy', R=[u0.r], W=[L3.r], out=L3[:, 128:256], in_=u0[:, 1664:1792])
            transpose8(L3, L3T, PT[tt % 2], ident_b, n=3)
            I('act', 'copy', R=[u0.r], W=[OPST.r], out=OPST[:, 0, 0, :], in_=r_ap)
            I('act', 'copy', R=[u0.r], W=[OPF.r], out=OPF[:, 0, :], in_=r_ap)
            I('act', 'copy', R=[u0.r], W=[OPST.r], out=OPST[:, 0, 5, :], in_=v_ap)
            I('dve', 'tensor_copy', R=[u0.r], W=[OPF.r], out=OPF[:, 5, :], in_=v_ap)
            I('dve', 'tensor_tensor', R=[u0.r, cst], W=[kk.r], out=kk[:, :, :].rearrange('p a b -> p (a b)'), in0=k_ap, in1=KKB[:, :],
              op=ALU.mult)
            I('dve', 'tensor_tensor', R=[kk.r], W=[tA.r], out=tA[:, :].rearrange('p (a b) -> p a b', a=8), in0=kk[:, :, :],
              in1=kk[:, :, :], op=ALU.mult)
            I('dve', 'tensor_reduce', R=[tA.r], W=[st_.r], out=st_[:, 0:8], in_=tA[:, :].rearrange('p (a b) -> p a b', a=8),
              axis=AX.X, op=ALU.add)
            rms_rstd(st_[:, 0:8], 1.0, st_[:, 8:16], [st_.r], [st_.r], ecol=2)
            I('dve', 'tensor_tensor', R=[kk.r, st_.r], W=[kk.r], out=kk[:, :, :], in0=kk[:, :, :],
              in1=st_[:, 8:16].unsqueeze(2).to_broadcast([128, 8, 64]), op=ALU.mult)
            kkf = kk[:, :, :].rearrange('p a b -> p (a b)')
            I('act', 'mul', R=[kk.r], W=[OPST.r], out=OPST[:, 0, 3, :], in_=kkf, mul=-1.0)
            I('act', 'mul', R=[kk.r], W=[OPF.r], out=OPF[:, 3, :], in_=kkf, mul=-1.0)
            I('pool', 'tensor_tensor', R=[u0.r, cst], W=[rr.r], out=rr[:, :], in0=r_ap, in1=RKB[:, :], op=ALU.mult)
            for d in range(2):
                dst = OPST[:, 0, :, :] if d == 0 else OPF[:, :, :]
                dres = OPST.r if d == 0 else OPF.r
                pw, pa_ = PB[0 + d], PB[2 + d]
                mm(pw[:, :], L3T[d * 64:(d + 1) * 64, 0:128], W2[d * 64:(d + 1) * 64, :], True, True, [L3T.r, W2.r], [pw.r])
                mm(pa_[:, :], L3T[d * 64:(d + 1) * 64, 128:256], A2[d * 64:(d + 1) * 64, :], True, True, [L3T.r, A2.r], [pa_.r])
                I('dve', 'tensor_tensor', R=[pw.r, cst], W=[tA.r], out=tA[:, :], in0=pw[:, :], in1=W0B[:, d, :], op=ALU.add)
                I('act', 'activation', R=[tA.r], W=[tA.r], out=tA[:, :], in_=tA[:, :], func=AF.Sigmoid)
                I('act', 'mul', R=[tA.r], W=[dres], out=dst[:, 1, :], in_=tA[:, :], mul=-float(np.exp(-0.5)))
                a_t = av[d]
                I('dve', 'tensor_tensor', R=[pa_.r, cst], W=[a_t.r], out=a_t[:, :], in0=pa_[:, :], in1=A0B[:, d, :], op=ALU.add)
                I('act', 'activation', R=[a_t.r], W=[a_t.r], out=a_t[:, :], in_=a_t[:, :], func=AF.Sigmoid)
                I('dve', 'scalar_tensor_tensor', R=[a_t.r, cst], W=[tB.r], out=tB[:, :], in0=a_t[:, :], scalar=-1.0, in1=KAB[:, :],
                  op0=ALU.add, op1=ALU.mult)
                I('dve', 'scalar_tensor_tensor', R=[tB.r, u0.r], W=[dres], out=dst[:, 2, :], in0=tB[:, :], scalar=1.0, in1=k_ap,
                  op0=ALU.add, op1=ALU.mult)
                I('pool', 'tensor_tensor', R=[kk.r, a_t.r], W=[dres], out=dst[:, 4, :], in0=kkf, in1=a_t[:, :], op=ALU.mult)
                I('dve', 'tensor_tensor', R=[rr.r, dres], W=[tB.r], out=tB[:, :], in0=rr[:, :], in1=dst[:, 2, :], op=ALU.mult)
                I('dve', 'tensor_reduce', R=[tB.r], W=[st_.r], out=st_[:, 16 + d * 8:24 + d * 8],
                  in_=tB[:, :].rearrange('p (a b) -> p a b', a=8), axis=AX.X, op=ALU.add)
            I('dve', 'tensor_tensor', R=[st_.r], W=[st_.r], out=st_[:, 16:24], in0=st_[:, 16:24], in1=st_[:, 24:32], op=ALU.add)
            I('dve', 'tensor_tensor', R=[st_.r, u0.r], W=[bon.r], out=bon[:, :, :], in0=v_ap.rearrange('p (a b) -> p a b', a=8),
              in1=st_[:, 16:24].unsqueeze(2).to_broadcast([128, 8, 64]), op=ALU.mult)
            dma('sp', BON_d[r0:r0 + 128, :], bon[:, :, :].rearrange('p a b -> p (a b)'), R=[bon.r], W=[BONR])
            pg = PB[4]
            mm(pg[:, :], L3T[:, 256:384], G2[:, :], True, True, [L3T.r, G2.r], [pg.r])
            I('act', 'copy', R=[pg.r], W=[gg.r], out=gg[:, :], in_=pg[:, :])
            dma('sp', GG_d[r0:r0 + 128, :], gg[:, :], R=[gg.r], W=[GGR])
            for q in range(6):
                pf = PB[q % 2]
                mm(pf[:, :], flip_f[:, :], OPF[:, q, :], True, True, [flip_f.r, OPF.r], [pf.r])
                I('act' if q % 2 else 'dve', 'copy' if q % 2 else 'tensor_copy', R=[pf.r], W=[OPST.r], out=OPST[:, 1, q, :], in_=pf[:, :])
            for d in range(2):
                s0 = fstep(tt) if d == 0 else bstep(tt)
                X = lambda q: OPST[:, d, q, :]
                pc, pl = PB[2 + d], PB[4 + d]
                mm(pc[:, :], TRI[:, :], X(1), True, True, [cst, OPST.r], [pc.r])
                mm(pl[:, :], BLK[:, :], X(1), True, True, [cst, OPST.r], [pl.r])
                I('act', 'copy', R=[pc.r], W=[CUM.r], out=CUM[:, :], in_=pc[:, :])
                I('act', 'activation', R=[CUM.r], W=[EE[0].r], out=EE[0][:, :], in_=CUM[:, :], func=AF.Exp)
                I('act', 'activation', R=[CUM.r], W=[EE[1].r], out=EE[1][:, :], in_=CUM[:, :], func=AF.Exp, scale=-1.0)
                I('pool', 'tensor_tensor', R=[CUM.r, OPST.r], W=[EE[2].r], out=EE[2][:, :], in0=CUM[:, :], in1=X(1), op=ALU.subtract)
                I('act', 'activation', R=[EE[2].r], W=[EE[2].r], out=EE[2][:, :], in_=EE[2][:, :], func=AF.Exp)
                I('dve', 'tensor_tensor', R=[pl.r, CUM.r], W=[EE[3].r], out=EE[3][:, :], in0=pl[:, :], in1=CUM[:, :], op=ALU.subtract)
                I('act', 'activation', R=[EE[3].r], W=[EE[3].r], out=EE[3][:, :], in_=EE[3][:, :], func=AF.Exp)
                I('act', 'activation', R=[pl.r], W=[PLBt.r], out=PLBt[:, :], in_=pl[:, :], func=AF.Exp)
                prods = ((0, 3, 2, 'dve'), (1, 0, 0, 'pool'), (2, 4, 1, 'dve'), (3, 2, 1, 'pool'), (4, 4, 3, 'dve'), (5, 2, 3, 'pool'))
                for (ti, q, e, eng) in prods:
                    I(eng, 'tensor_tensor', R=[OPST.r, EE[e].r], W=[TMb[ti].r], out=TMb[ti][:, :], in0=X(q), in1=EE[e][:, :], op=ALU.mult)
                I('act', 'copy', R=[OPST.r], W=[TMb[6].r], out=TMb[6][:, :], in_=X(5))
                for oi, ti in enumerate((0, 4, 5, 6)):
                    dma('sp', TM_d[d, oi, s0:s0 + 128, :], TMb[ti][:, :], R=[TMb[ti].r], W=[TMR])
                dma('sp', PLB_d[d, s0:s0 + 128, :], PLBt[:, :], R=[PLBt.r], W=[PLBR])
                for oi, ti in enumerate((0, 2, 3, 1)):
                    ft = FTs[fti % 2]
                    pt = PT[fti % 2]
                    fti += 1
                    for h in range(8):
                        tr(pt[0:64, h * 128:(h + 1) * 128], TMb[ti][:, h * 64:(h + 1) * 64], ident_b[:, :], [TMb[ti].r, ident_b.r], [pt.r])
                    I('act' if oi % 2 else 'dve', 'copy' if oi % 2 else 'tensor_copy', R=[pt.r], W=[ft.r],
                      out=ft[:, :, :].rearrange('p a b -> p (a b)'), in_=pt[0:64, :])
                    dma('sp', AP(FT_d, ((d * 4 + oi) * 64) * 8 * NT + s0, [[8 * NT, 64], [NT, 8], [1, 128]]), ft[:, :, :], R=[ft.r], W=[FTR])
        A.release(m)

    def phase_rwkv_chunk():
        m = A.mark()
        MSK = A.alloc([128, 6, 128], F32, 'msk')
        dma('sp', MSK[:, :, :], msk_d[:, :, :], W=[MSK.r])
        H = A.alloc([64, 2, 8, 64], F32, 'hstate')
        I('dve', 'memset', W=[H.r], ap=H[:, :, :, :], constant=0.0)
        HR = [Res('h0'), Res('h1')]
        NL = 7
        bufs = {}
        for d in range(2):
            bufs[d] = dict(
                tm=[A.alloc([128, 4, 8, 64], BF16, 'ctm%d_%d' % (d, k)) for k in range(2)],
                pl=[A.alloc([64, 512], F32, 'cpl%d_%d' % (d, k)) for k in range(2)],
                ft=[A.alloc([64, 4, 8, 128], BF16, 'cft%d_%d' % (d, k)) for k in range(2)],
                Z=[A.alloc([128, 8, 128], BF16, 'cz%d_%d' % (d, k)) for k in range(2)],
                N=[A.alloc([128, 8, 128], BF16, 'cn%d_%d' % (d, k)) for k in range(2)],
                P=[A.alloc([128, 8, 128], BF16, 'cp%d_%d' % (d, k)) for k in range(2)],
                PT=[A.alloc([128, 8, 128], BF16, 'cpt%d_%d' % (d, k)) for k in range(2)],
                Zp=A.alloc([128, 8, 128], BF16, 'czp%d' % d),
                MT=A.alloc([128, 8, 128], BF16, 'cmt%d' % d),
                QbT=A.alloc([128, 8, 128], BF16, 'cqb%d' % d),
                QkT=A.alloc([128, 8, 128], BF16, 'cqk%d' % d),
                Xf=A.alloc([128, 8, 128], F32, 'cxf%d' % d),
                Xb=A.alloc([128, 8, 128], BF16, 'cxb%d' % d),
                FT=A.alloc([64, 8, 64], F32, 'cF%d' % d),
                CP=A.alloc([64, 8, 64], F32, 'cC%d' % d),
                RfT=A.alloc([64, 8, 128], F32, 'cR%d' % d),
                Y0=A.alloc([128, 8, 64], F32, 'cy0%d' % d),
                Yo=[A.alloc([128, 8, 64], F32, 'cyo%d_%d' % (d, k)) for k in range(2)],
                tmpd=A.alloc([64, 8, 64], F32, 'ctd%d' % d),
            )
        idb = ident_f[0:64, 0:64].unsqueeze(1).to_broadcast([64, 8, 64])

        def stages(d, u):
            B = bufs[d]
            tm, plk, ft = B['tm'][u % 2], B['pl'][u % 2], B['ft'][u % 2]
            s0 = u * 128
            PXa, PXb, PGa, PGb = PB[4 * d], PB[4 * d + 1], PB[4 * d + 2], PB[4 * d + 3]
            PXr, PGr = [PXa.r, PXb.r], [PGa.r, PGb.r]
            PX = PBALL[:, (4 * d) * 512:(4 * d + 2) * 512].rearrange('p (a b) -> p a b', a=8)
            PG = PBALL[:, (4 * d + 2) * 512:(4 * d + 4) * 512].rearrange('p (a b) -> p a b', a=8)
            PGs = PBALL[0:64, (4 * d + 2) * 512:(4 * d + 3) * 512].rearrange('p (a b) -> p a b', a=8)
            PGr1 = [PGa.r]
            AtK, BpK, KpK, VmK = (tm[:, q, :, :] for q in range(4))
            AtT, BtT, KtT, RtT = (ft[:, q, :, :] for q in range(4))
            Z, N_ = B['Z'], B['N']
            MT, QbT, QkT, Xf, Xb = B['MT'], B['QbT'], B['QkT'], B['Xf'], B['Xb']
            FT, CP, RfT, Y0, tmpd = B['FT'], B['CP'], B['RfT'], B['Y0'], B['tmpd']
            Yo = B['Yo'][u % 2]
            out = []

            def load():
                dma('sp', tm[:, :, :, :].rearrange('p q a b -> p q (a b)'),
                    AP(TM_d, d * 4 * NT * 512 + s0 * 512, [[512, 128], [NT * 512, 4], [1, 512]]), R=[TMR], W=[tm.r])
                dma('sp', plk[:, :], PLB_d[d, s0:s0 + 64, :], R=[PLBR], W=[plk.r])
                for q in range(4):
                    dma('sp', ft[:, q, :, :], AP(FT_d, ((d * 4 + q) * 64) * 8 * NT + s0, [[8 * NT, 64], [NT, 8], [1, 128]]),
                        R=[FTR], W=[ft.r])
            out.append(load)

            def gram(lhs, rhs, mi, dst, eng):
                def f():
                    for h in range(8):
                        mm(PG[:, h, :], lhs[:, h, :], rhs[:, h, :], True, True, [ft.r], [PGa.r if h < 4 else PGb.r])
                    I(eng, 'tensor_tensor', R=PGr + [MSK.r], W=[dst.r], out=dst[:, :, :], in0=PG,
                      in1=MSK[:, mi, :].unsqueeze(1).to_broadcast([128, 8, 128]), op=ALU.mult)
                return f
            P_, PTt, Zp = B['P'], B['PT'], B['Zp']

            def gram2(lhs, rhs, m1, d1, m2, d2):
                def f():
                    for h in range(8):
                        mm(PG[:, h, :], lhs[:, h, :], rhs[:, h, :], True, True, [ft.r], [PGa.r if h < 4 else PGb.r])
                    I('dve', 'tensor_tensor', R=PGr + [MSK.r], W=[d1.r], out=d1[:, :, :], in0=PG,
                      in1=MSK[:, m1, :].unsqueeze(1).to_broadcast([128, 8, 128]), op=ALU.mult)
                    I('dve', 'tensor_tensor', R=PGr + [MSK.r], W=[d2.r], out=d2[:, :, :], in0=PG,
                      in1=MSK[:, m2, :].unsqueeze(1).to_broadcast([128, 8, 128]), op=ALU.mult)
                return f
            out.append(gram2(BtT, AtT, 0, Z[0], 3, PTt[0]))
            out.append(gram2(AtT, BtT, 1, N_[0], 4, P_[0]))
            out.append(gram(KtT, AtT, 5, MT, 'dve'))
            out.append(gram(BtT, RtT, 2, QbT, 'dve'))
            out.append(gram(KtT, RtT, 2, QkT, 'dve'))

            def mv():
                for h in range(8):
                    mm(PX[:, h, 0:64], MT[:, h, :], VmK[:, h, :], True, True, [MT.r, tm.r], [PXa.r if h < 4 else PXb.r])
                I('dve', 'tensor_copy', R=PXr, W=[Xf.r], out=Xf[:, :, 64:128], in_=PX[:, :, 0:64])
                I('act', 'copy', R=PXr, W=[Xb.r], out=Xb[:, :, 64:128], in_=PX[:, :, 0:64])
                I('pool', 'tensor_copy', R=[tm.r], W=[Xf.r], out=Xf[:, :, 0:64], in_=AtK)
                I('pool', 'tensor_copy', R=[tm.r], W=[Xb.r], out=Xb[:, :, 0:64], in_=AtK)
            out.append(mv)
            idb128 = ident_f[:, :].unsqueeze(1).to_broadcast([128, 8, 128])

            def xapply(L_):
                def f():
                    for h in range(8):
                        mm(PX[:, h, :], L_[:, h, :], Xb[:, h, :], True, True, [L_.r, Xb.r], [PXa.r if h < 4 else PXb.r])
                    I('dve', 'tensor_tensor', R=PXr + [Xf.r], W=[Xf.r], out=Xf[:, :, :], in0=PX, in1=Xf[:, :, :], op=ALU.add)
                    I('act', 'copy', R=[Xf.r], W=[Xb.r], out=Xb[:, :, :], in_=Xf[:, :, :])
                return f

            def mmset(ps, psr, lhs, rhs, dst, eng):
                def f():
                    for h in range(8):
                        mm(ps[:, h, :], lhs[:, h, :], rhs[:, h, :], True, True, [lhs.r, rhs.r], [psr[0] if h < 4 else psr[1]])
                    I(eng, 'copy' if eng == 'act' else 'tensor_copy', R=psr, W=[dst.r], out=dst[:, :, :], in_=ps)
                return f

            def mkzp(Zc):
                def f():
                    I('pool', 'tensor_tensor', R=[Zc.r, ident_f.r], W=[Zp.r], out=Zp[:, :, :], in0=Zc[:, :, :], in1=idb128, op=ALU.add)
                return f
            ND_LEV, NP_LEV = 4, 3
            for i in range(ND_LEV):
                Zc, Nc, Pc, PTc = Z[i % 2], N_[i % 2], P_[i % 2], PTt[i % 2]
                Zn, Nn, Pn, PTn = Z[(i + 1) % 2], N_[(i + 1) % 2], P_[(i + 1) % 2], PTt[(i + 1) % 2]
                out.append(mkzp(Zc))
                out.append(xapply(Zc))
                out.append(mmset(PG, PGr, Zp, Pc, Pn, 'act'))
                out.append(mmset(PX, PXr, Pc, Zp, PTn, 'dve'))
                if i < ND_LEV - 1:
                    out.append(mmset(PG, PGr, Nc, Zc, Zn, 'act'))
                    out.append(mmset(PX, PXr, Zc, Nc, Nn, 'dve'))
            for jl in range(NP_LEV):
                k0 = ND_LEV + jl
                Pc, PTc = P_[k0 % 2], PTt[k0 % 2]
                Pn, PTn = P_[(k0 + 1) % 2], PTt[(k0 + 1) % 2]
                out.append(xapply(PTc))
                if jl < NP_LEV - 1:
                    out.append(mmset(PG, PGr, Pc, PTc, PTn, 'act'))
                    out.append(mmset(PX, PXr, PTc, Pc, Pn, 'dve'))

            def trans():
                for h in range(8):
                    mm(PGs[:, h, :], Xb[:, h, 0:64], BpK[:, h, :], True, True, [Xb.r, tm.r], PGr1)
                I('pool', 'tensor_tensor', R=[plk.r, ident_f.r], W=[tmpd.r], out=tmpd[:, :, :],
                  in0=plk[:, :].rearrange('p (a b) -> p a b', a=8), in1=idb, op=ALU.mult)
                I('dve', 'tensor_tensor', R=PGr1 + [tmpd.r], W=[FT.r], out=FT[:, :, :], in0=PGs, in1=tmpd[:, :, :], op=ALU.add)
            out.append(trans)

            def cprime():
                for h in range(8):
                    mm(PGs[:, h, :], BpK[:, h, :], Xb[:, h, 64:128], True, False, [Xb.r, tm.r], PGr1)
                    mm(PGs[:, h, :], KpK[:, h, :], VmK[:, h, :], False, True, [tm.r], PGr1)
                I('act', 'copy', R=PGr1, W=[CP.r], out=CP[:, :, :], in_=PGs)
            out.append(cprime)

            def w1t():
                PGw = PBALL[0:64, (4 * d + 2) * 512:(4 * d + 4) * 512].rearrange('p (a b) -> p a b', a=8)
                for h in range(8):
                    mm(PGw[:, h, :], Xb[:, h, 0:64], QbT[:, h, :], True, True, [Xb.r, QbT.r], [PGa.r if h < 4 else PGb.r])
                I('dve', 'tensor_tensor', R=PGr + [ft.r], W=[RfT.r], out=RfT[:, :, :], in0=PGw, in1=RtT, op=ALU.add)
            out.append(w1t)

            def y0():
                PGy = PBALL[:, (4 * d + 2) * 512:(4 * d + 3) * 512].rearrange('p (a b) -> p a b', a=8)
                for h in range(8):
                    mm(PGy[:, h, :], QbT[:, h, :], Xb[:, h, 64:128], True, False, [Xb.r, QbT.r], PGr1)
                    mm(PGy[:, h, :], QkT[:, h, :], VmK[:, h, :], False, True, [QkT.r, tm.r], PGr1)
                I('act', 'copy', R=PGr1, W=[Y0.r], out=Y0[:, :, :], in_=PGy)
            out.append(y0)

            def final():
                PGy = PBALL[:, (4 * d + 2) * 512:(4 * d + 3) * 512].rearrange('p (a b) -> p a b', a=8)
                for h in range(8):
                    mm(PGy[:, h, :], RfT[:, h, :], H[:, d, h, :], True, True, [RfT.r, HR[d]], PGr1)
                I('dve', 'tensor_tensor', R=PGr1 + [Y0.r], W=[Yo.r], out=Yo[:, :, :], in0=PGy, in1=Y0[:, :, :], op=ALU.add)
                dma('sp', YS_d[d, s0:s0 + 128, :], Yo[:, :, :].rearrange('p a b -> p (a b)'), R=[Yo.r], W=[YSR])
            out.append(final)

            def chain():
                PGc = PBALL[0:64, (4 * d + 3) * 512:(4 * d + 4) * 512].rearrange('p (a b) -> p a b', a=8)
                for h in range(8):
                    mm(PGc[:, h, :], FT[:, h, :], H[:, d, h, :], True, True, [FT.r, HR[d]], [PGb.r])
                I('dve', 'tensor_tensor', R=[PGb.r, CP.r], W=[HR[d]], out=H[:, d, :, :], in0=PGc, in1=CP[:, :, :], op=ALU.add)
            out.append(chain)
            return out

        for u in range(NTILE):
            sts = [stages(d, u) for d in range(2)]
            for k in range(len(sts[0])):
                for d in range(2):
                    sts[d][k]()
        A.release(m)

    def phase_rwkv_post2(j):
        m = A.mark()
        GNG = A.alloc([128, 512], F32, 'gng')
        GNB = A.alloc([128, 512], F32, 'gnb')
        cst = Res('gnconst')
        dma('sp', GNG[:, :], AP(rw_gn_g, j * 512, [[0, 128], [1, 512]]), W=[cst])
        dma('sp', GNB[:, :], AP(rw_gn_b, j * 512, [[0, 128], [1, 512]]), W=[cst])
        yfs = [A.alloc([128, 512], F32, 'yf%d' % k) for k in range(2)]
        ybs = [A.alloc([128, 512], F32, 'yb_%d' % k) for k in range(2)]
        bons = [A.alloc([128, 512], F32, 'bon%d' % k) for k in range(2)]
        ggs = [A.alloc([128, 512], F32, 'gg%d' % k) for k in range(2)]
        y = A.alloc([128, 8, 64], F32, 'ysum')
        sq = A.alloc([128, 8, 64], F32, 'ysq')
        st_ = A.alloc([128, 32], F32, 'gst')
        ocs = [A.alloc([128, 512], BF16, 'orw%d' % k) for k in range(2)]
        for tt in range(NTILE):
            yf, yb, bo, gt, oc = yfs[tt % 2], ybs[tt % 2], bons[tt % 2], ggs[tt % 2], ocs[tt % 2]
            r0 = tt * 128
            dma('sp', yf[:, :], YS_d[0, fstep(tt):fstep(tt) + 128, :], R=[YSR], W=[yf.r])
            dma('sp', yb[:, :], YS_d[1, bstep(tt):bstep(tt) + 128, :], R=[YSR], W=[yb.r])
            dma('sp', bo[:, :], BON_d[r0:r0 + 128, :], R=[BONR], W=[bo.r])
            dma('sp', gt[:, :], GG_d[r0:r0 + 128, :], R=[GGR], W=[gt.r])
            py = PB[tt % 2]
            mm(py[:, :], flip_f[:, :], yb[:, :], True, True, [flip_f.r, yb.r], [py.r])
            yfl = y[:, :, :].rearrange('p a b -> p (a b)')
            I('pool', 'tensor_tensor', R=[yf.r, bo.r], W=[yf.r], out=yf[:, :], in0=yf[:, :], in1=bo[:, :], op=ALU.add)
            I('dve', 'tensor_tensor', R=[py.r, yf.r], W=[y.r], out=yfl, in0=py[:, :], in1=yf[:, :], op=ALU.add)
            I('dve', 'tensor_reduce', R=[y.r], W=[st_.r], out=st_[:, 0:8], in_=y[:, :, :], axis=AX.X, op=ALU.add)
            I('dve', 'tensor_scalar', R=[st_.r], W=[st_.r], out=st_[:, 8:16], in0=st_[:, 0:8], scalar1=-1.0 / 64, scalar2=None, op0=ALU.mult)
            I('dve', 'tensor_tensor', R=[y.r, st_.r], W=[y.r], out=y[:, :, :], in0=y[:, :, :],
              in1=st_[:, 8:16].unsqueeze(2).to_broadcast([128, 8, 64]), op=ALU.add)
            I('pool', 'tensor_tensor', R=[y.r], W=[sq.r], out=sq[:, :, :], in0=y[:, :, :], in1=y[:, :, :], op=ALU.mult)
            I('dve', 'tensor_reduce', R=[sq.r], W=[st_.r], out=st_[:, 16:24], in_=sq[:, :, :], axis=AX.X, op=ALU.add)
            rms_rstd(st_[:, 16:24], 64, st_[:, 24:32], [st_.r], [st_.r], ecol=1)
            I('dve', 'tensor_tensor', R=[y.r, st_.r], W=[y.r], out=y[:, :, :], in0=y[:, :, :],
              in1=st_[:, 24:32].unsqueeze(2).to_broadcast([128, 8, 64]), op=ALU.mult)
            I('dve', 'tensor_tensor', R=[y.r, cst], W=[y.r], out=yfl, in0=yfl, in1=GNG[:, :], op=ALU.mult)
            I('pool', 'tensor_tensor', R=[y.r, cst], W=[y.r], out=yfl, in0=yfl, in1=GNB[:, :], op=ALU.add)
            I('dve', 'tensor_tensor', R=[y.r, gt.r], W=[oc.r], out=oc[:, :], in0=yfl, in1=gt[:, :], op=ALU.mult)
            dma('sp', CAT_d[r0:r0 + 128, 0:512], oc[:, :], R=[oc.r], W=[CATR[tt]])
        A.release(m)

    def phase_na_prep(j):
        m = A.mark()
        QGB = A.alloc([128, 64], F32, 'naqg')
        KGB = A.alloc([128, 64], F32, 'nakg')
        cst = Res('naconst')
        dma('sp', QGB[:, :], AP(na_q_g, j * 64, [[0, 128], [1, 64]]), W=[cst])
        dma('sp', KGB[:, :], AP(na_k_g, j * 64, [[0, 128], [1, 64]]), W=[cst])
        uqs = [A.alloc([128, 3, 8, 64], F32, 'nu%d' % k) for k in range(2)]
        sq = A.alloc([128, 8, 64], F32, 'nsq')
        st_ = A.alloc([128, 32], F32, 'nst')
        qkb = [A.alloc([128, 8, 64], BF16, 'nqb%d' % k) for k in range(2)]
        qkT = [A.alloc([64, 8, 128], BF16, 'nqT%d' % k) for k in range(2)]
        vas = [A.alloc([128, 8, 65], BF16, 'nva%d' % k) for k in range(2)]
        for k in range(2):
            I('dve', 'memset', W=[vas[k].r], ap=vas[k][:, :, :], constant=1.0)
        for tt in range(NTILE):
            uq, va = uqs[tt % 2], vas[tt % 2]
            r0 = tt * 128
            dma('sp', uq[:, :, :, :].rearrange('p a b c -> p (a b c)'), U_d[r0:r0 + 128, RW_IN:E_OD], R=[UR[tt]], W=[uq.r])
            for which, (gb, dst_d, dres) in enumerate(((QGB, QT_d, QTR), (KGB, KT_d, KTR))):
                x3 = uq[:, which, :, :]
                qb, qT, pt = qkb[which], qkT[which], PT[which]
                I('dve', 'tensor_tensor', R=[uq.r], W=[sq.r], out=sq[:, :, :], in0=x3, in1=x3, op=ALU.mult)
                I('dve', 'tensor_reduce', R=[sq.r], W=[st_.r], out=st_[:, 0:8], in_=sq[:, :, :], axis=AX.X, op=ALU.add)
                rms_rstd(st_[:, 0:8], 64, st_[:, 8:16], [st_.r], [st_.r])
                I('dve', 'tensor_tensor', R=[uq.r, st_.r], W=[sq.r], out=sq[:, :, :], in0=x3,
                  in1=st_[:, 8:16].unsqueeze(2).to_broadcast([128, 8, 64]), op=ALU.mult)
                I('dve', 'tensor_tensor', R=[sq.r, cst], W=[qb.r], out=qb[:, :, :], in0=sq[:, :, :],
                  in1=gb[:, :].unsqueeze(1).to_broadcast([128, 8, 64]), op=ALU.mult)
                for h in range(8):
                    tr(pt[0:64, h * 128:(h + 1) * 128], qb[:, h, :], ident_b[:, :], [qb.r, ident_b.r], [pt.r])
                I('act', 'copy', R=[pt.r], W=[qT.r], out=qT[:, :, :].rearrange('p a b -> p (a b)'), in_=pt[0:64, :])
                dma('sp', AP(dst_d, r0, [[NT, 64], [96 * NT, 8], [1, 128]]), qT[:, :, :], R=[qT.r], W=[dres])
            I('pool', 'tensor_copy', R=[uq.r], W=[va.r], out=va[:, :, 0:64], in_=uq[:, 2, :, :])
            dma('sp', VA_d[r0:r0 + 128, :], va[:, :, :].rearrange('p a b -> p (a b)'), R=[va.r], W=[VAR])
        A.release(m)

    def phase_natten(j):
        m = A.mark()
        scale = 64 ** -0.5
        kTs = [A.alloc([64, NT], BF16, 'nkT%d' % k) for k in range(2)]
        qTs = [A.alloc([64, NT], BF16, 'nqT_%d' % k) for k in range(2)]
        vhs = [A.alloc([128, NTILE, 65], BF16, 'nvh%d' % k) for k in range(2)]
        v64s = [A.alloc([128, 31, 65], BF16, 'nv64%d' % k) for k in range(2)]
        nbs = [A.alloc([128, 8, 256], F32, 'nab%d' % k) for k in range(2)]
        sbs = [A.alloc([128, 256], F32, 'nsb%d' % k) for k in range(2)]
        pps = [A.alloc([128, 384], BF16, 'npp%d' % k) for k in range(2)]
        oas = [A.alloc([64, 64, 64], BF16, 'noa%d' % k) for k in range(2)]
        oac = [A.alloc([128, 2, 64], BF16, 'noc%d' % k) for k in range(2)]
        rc = A.alloc([128, 4], F32, 'nrc')
        it = 0
        for h in range(8):
            kT, qT, vh, v64, nb, oa, oc = kTs[h % 2], qTs[h % 2], vhs[h % 2], v64s[h % 2], nbs[h % 2], oas[h % 2], oac[h % 2]
            dma('sp', kT[:, :], KT_d[h, 0:64, :], R=[KTR], W=[kT.r])
            dma('sp', qT[:, :], QT_d[h, 0:64, :], R=[QTR], W=[qT.r])
            dma('sp', vh[:, :, :], AP(VA_d, h * 65, [[520, 128], [128 * 520, NTILE], [1, 65]]), R=[VAR], W=[vh.r])
            dma('sp', v64[:, :, :], AP(VA_d, 64 * 520 + h * 65, [[520, 128], [128 * 520, 31], [1, 65]]), R=[VAR], W=[v64.r])
            dma('sp', nb[:, :, :], nab_d[j, h, :, :, :], W=[nb.r])
            def na_pv(r, rs, pp, po):
                for kt in range(4):
                    vt = vh[:, rs // 2 + kt, :] if rs % 2 == 0 else v64[:, (rs - 1) // 2 + kt, :]
                    mm(po[0:64, 0:65], pp[:, kt * 64:(kt + 1) * 64], vt, kt == 0, False, [pp.r, vh.r, v64.r], [po.r])
                for c in range(2):
                    mm(po[0:64, 0:65], pp[:, 256 + c * 64:256 + (c + 1) * 64], vh[:, NLT + c, :], False, c == 1, [pp.r, vh.r], [po.r])
                I('dve', 'reciprocal', R=[po.r], W=[rc.r], out=rc[0:64, (r % 2):(r % 2) + 1], in_=po[0:64, 64:65])
                I('dve', 'tensor_scalar', R=[po.r, rc.r], W=[oa.r], out=oa[:, r, :], in0=po[0:64, 0:64], scalar1=rc[0:64, (r % 2):(r % 2) + 1],
                  scalar2=None, op0=ALU.mult)
            prev = None
            for r in range(64):
                rs = min(max(r - 4, 0), 56)
                delta = r - rs
                kb = rs * 64
                ps, po = PB[it % 2], PB[2 + it % 2]
                sb, pp = sbs[it % 2], pps[it % 2]
                it += 1
                qs = qT[:, r * 64:(r + 1) * 64]
                for kt in range(4):
                    mm(ps[:, kt * 64:(kt + 1) * 64], kT[:, kb + kt * 128:kb + (kt + 1) * 128], qs, True, True, [kT.r, qT.r], [ps.r])
                for c in range(2):
                    mm(ps[:, 256 + c * 64:256 + (c + 1) * 64], kT[:, T + c * 128:T + (c + 1) * 128], qs, True, True, [kT.r, qT.r], [ps.r])
                I('dve', 'scalar_tensor_tensor', R=[ps.r, nb.r], W=[sb.r], out=sb[:, :], in0=ps[:, 0:256], scalar=scale, in1=nb[:, delta, :],
                  op0=ALU.mult, op1=ALU.add)
                I('act', 'activation', R=[sb.r], W=[pp.r], out=pp[:, 0:256], in_=sb[:, :], func=AF.Exp)
                I('act', 'activation', R=[ps.r], W=[pp.r], out=pp[:, 256:384], in_=ps[:, 256:384], func=AF.Exp, scale=scale)
                if prev is not None:
                    na_pv(*prev)
                prev = (r, rs, pp, po)
            na_pv(*prev)
            dma('sp', AP(CAT_d, 512 + h * 64, [[D, 64], [64 * D, 64], [1, 64]]), oa[:, :, :], R=[oa.r], W=CATR[0:NLT])
            pp = pps[it % 2]
            for c in range(2):
                ps = PB[c]
                mm(ps[:, 0:256], kT[:, T + c * 128:T + (c + 1) * 128], qT[:, T:NT], True, True, [kT.r, qT.r], [ps.r])
                ppc = pps[c]
                I('act', 'activation', R=[ps.r], W=[ppc.r], out=ppc[:, 0:256], in_=ps[:, 0:256], func=AF.Exp, scale=scale)
                for q2 in range(2):
                    mm(PB[4 + q2][:, 0:65], ppc[:, q2 * 128:(q2 + 1) * 128], vh[:, NLT + c, :], c == 0, c == 1, [ppc.r, vh.r], [PB[4 + q2].r])
            for q2 in range(2):
                I('dve', 'reciprocal', R=[PB[4 + q2].r], W=[rc.r], out=rc[:, 2 + q2:3 + q2], in_=PB[4 + q2][:, 64:65])
                I('dve', 'tensor_scalar', R=[PB[4 + q2].r, rc.r], W=[oc.r], out=oc[:, q2, :], in0=PB[4 + q2][:, 0:64],
                  scalar1=rc[:, 2 + q2:3 + q2], scalar2=None, op0=ALU.mult)
            dma('sp', AP(CAT_d, T * D + 512 + h * 64, [[D, 128], [128 * D, 2], [1, 64]]), oc[:, :, :], R=[oc.r], W=CATR[NLT:NTILE])
        A.release(m)

    for i in range(nlayers):
        j = i // 2
        last = i == 3
        if i % 2 == 0:
            phase_inproj(i, ev_w_in, j, E_EV)
            phase_mla_prep(j)
            phase_attention(96, 96 ** -0.5)
            phase_conv(j)
            w_out_t = ev_w_out
        else:
            phase_inproj(i, od_w_in, j, E_OD)
            if CHUNKED:
                phase_rwkv_prep2(j)
                phase_rwkv_chunk()
                phase_rwkv_post2(j)
            else:
                phase_rwkv_prep(j)
                phase_rwkv_scan()
                phase_rwkv_post(j)
            phase_na_prep(j)
            phase_natten(j)
            w_out_t = od_w_out
        m = A.mark()
        GT = A.alloc([128, 5, 16], F32, 'gt')
        IXG = A.alloc([128, 5, 16], I32, 'ixg')
        IXS = A.alloc([128, 5, 16], I32, 'ixs')
        m2 = A.mark()
        AFFT = A.alloc([16, NT], F32, 'afft')
        phase_outproj(i, w_out_t, j, AFFT)
        phase_topk(AFFT, GT, IXG, IXS, not last)
        A.release(m2)
        phase_moe(i, GT, IXG, IXS, not last)
        A.release(m)

    S.barrier()
    S.emit()
    st.close()
    return nc


def _consts(inputs):
    GRID_W = 64
    t = np.arange(T)
    row = (t // GRID_W).astype(np.float32)
    col = (t % GRID_W).astype(np.float32)
    inv = (10000.0 ** (-np.arange(0, 16, 2, dtype=np.float32) / 16)).astype(np.float32)
    ar = row[:, None] * inv
    ac = col[:, None] * inv
    ang = np.concatenate([ar, ar, ac, ac], -1).astype(np.float32)
    sign = np.array([-1.0] * 8 + [1.0] * 8 + [-1.0] * 8 + [1.0] * 8, np.float32)
    rope = np.concatenate([np.cos(ang), np.sin(ang) * sign], -1).astype(np.float32)
    ident = np.eye(128, dtype=np.float32)
    flip = np.ascontiguousarray(ident[::-1])
    iota = np.tile(np.arange(256, dtype=np.float32)[None, :], (128, 1))
    rpb = np.asarray(inputs['na_rpb'], np.float32)
    nab = np.full((2, 8, 8, 4, 128, 64), -200.0, np.float32)
    c = np.arange(64)
    cs = np.clip(c - 8, 0, 48)
    for delta in range(8):
        for kt in range(4):
            for kk in range(128):
                ii = kt * 2 + kk // 64
                jj = kk % 64
                drow = ii - delta + 7
                valid = (jj >= cs) & (jj < cs + 16)
                dcol = jj - c + 15
                qs = c[valid]
                nab[:, :, delta, kt, kk, qs] = rpb[:, :, drow, dcol[valid]]
    nab = np.ascontiguousarray(nab.transpose(0, 1, 4, 2, 3, 5).reshape(2, 8, 128, 8, 256))
    ii = np.arange(128)
    tri = (ii[:, None] <= ii[None, :]).astype(np.float32)
    blk = np.ones((128, 128), np.float32)
    i6 = np.arange(128)
    lt, gt, le = (i6[:, None] < i6[None, :]), (i6[:, None] > i6[None, :]), (i6[:, None] <= i6[None, :])
    sameb = (i6[:, None] // 16) == (i6[None, :] // 16)
    msk = np.stack([lt & sameb, gt & sameb, le, lt & ~sameb, gt & ~sameb, lt], 1).astype(np.float32)
    return dict(ident=ident, flip=flip, rope=rope, iota=iota, nab=nab, tri=tri, blk=blk, msk=np.ascontiguousarray(msk))


_WNAMES = ['ada_w', 'ada_b', 'norm1_g', 'norm2_g', 'ev_w_in', 'ev_w_out', 'mla_q_norm', 'mla_w_uq', 'mla_kv_norm',
           'mla_w_ukv', 'mla_q_g', 'mla_k_g', 'cv_dw_w', 'cv_dw_b', 'cv_ln_g', 'cv_ln_b', 'od_w_in', 'od_w_out',
           'rw_shift_w', 'rw_w0', 'rw_w2', 'rw_a0', 'rw_a2', 'rw_g2', 'rw_k_k', 'rw_k_a', 'rw_r_k', 'rw_gn_g', 'rw_gn_b',
           'na_q_g', 'na_k_g', 'moe_router', 'moe_w1', 'moe_w3', 'moe_w2']


def make_in_maps(inputs, cores):
    cst = _consts(inputs)
    shared = {k: np.ascontiguousarray(np.asarray(inputs[k], np.float32)) for k in _WNAMES}
    shared.update(cst)
    x = np.asarray(inputs['x'], np.float32)
    c = np.asarray(inputs['c'], np.float32)
    ctx = np.asarray(inputs['ctx'], np.float32)
    c_ctx = np.asarray(inputs['c_ctx'], np.float32)
    maps = []
    for b in cores:
        mp = dict(shared)
        mp['x'] = np.ascontiguousarray(x[b])
        mp['ctx'] = np.ascontiguousarray(ctx[b])
        mp['cc'] = np.ascontiguousarray(np.stack([c[b], c_ctx], 0))
        maps.append(mp)
    return maps


def kernel(**inputs):
    nc = build()
    maps = make_in_maps(inputs, list(range(8)))
    res = run_bass_kernel_spmd(nc, maps, core_ids=list(range(8)))
    return np.stack([np.asarray(r['out'], np.float32) for r in res.results], 0)
```

```python
import numpy as np
from contextlib import ExitStack
import concourse.bass as bass
import concourse.mybir as mybir
from concourse.bass_utils import run_bass_kernel_spmd

F32 = mybir.dt.float32
BF16 = mybir.dt.bfloat16
I32 = mybir.dt.int32
U32 = mybir.dt.uint32
ALU = mybir.AluOpType
AF = mybir.ActivationFunctionType
AX = mybir.AxisListType

ENGS = ('pe', 'dve', 'act', 'pool', 'sp')
DMA_RING = 6

D = 1024
T = 4096
L = 256
NT = T + L
NTILE = NT // 128
NLT = T // 128
EPS = 1e-6
E_EV = 1696
E_OD = 3456
RW_IN = 1920
CHUNKED = True


class Res:
    __slots__ = ('lw', 'rd', 'name')

    def __init__(self, name=''):
        self.lw = None
        self.rd = {}
        self.name = name


class Sched:
    def __init__(self, nc, stack):
        self.nc = nc
        self.th = {e: [] for e in ENGS}
        self.cnt = {e: 0 for e in ENGS}
        self.sem = {e: stack.enter_context(nc.semaphore('s_' + e)) for e in ENGS}
        self.ring = {e: [stack.enter_context(nc.semaphore('d_%s%d' % (e, i))) for i in range(DMA_RING)]
                     for e in ('sp', 'pool', 'act')}
        self.dn = {e: 0 for e in ('sp', 'pool', 'act')}
        self.seen = {e: {} for e in ENGS}

    def res(self, name=''):
        return Res(name)

    def _deps(self, reads, writes):
        evs = []
        for r in reads:
            if r.lw is not None:
                evs.append(r.lw)
        for w in writes:
            if w.lw is not None:
                evs.append(w.lw)
            evs.extend(w.rd.items())
        return evs

    def _waits(self, eng, evs):
        seen = self.seen[eng]
        need = {}
        pes = self.sem['pe']
        for s, v in evs:
            if eng == 'pe' and s is pes:
                continue
            if seen.get(s, 0) < v and need.get(s, 0) < v:
                need[s] = v
        for s, v in need.items():
            seen[s] = v
            self.th[eng].append(lambda E, s=s, v=v: E.wait_ge(s, v))

    def _commit(self, ev, reads, writes):
        s, v = ev
        for r in reads:
            if r.rd.get(s, 0) < v:
                r.rd[s] = v
        for w in writes:
            w.lw = ev
            w.rd = {}

    def op(self, eng, fn, reads=(), writes=()):
        self._waits(eng, self._deps(reads, writes))
        self.cnt[eng] += 1
        v = self.cnt[eng]
        s = self.sem[eng]
        self.th[eng].append(lambda E, fn=fn, s=s: fn(E).then_inc(s, 1))
        self._commit((s, v), reads, writes)

    def dma(self, eng, fn, reads=(), writes=()):
        j = self.dn[eng]
        self.dn[eng] += 1
        s = self.ring[eng][j % DMA_RING]
        evs = self._deps(reads, writes)
        if j >= DMA_RING:
            evs.append((s, 16 * (j // DMA_RING)))
        self._waits(eng, evs)
        self.th[eng].append(lambda E, fn=fn, s=s: fn(E).then_inc(s, 16))
        self._commit((s, 16 * (j // DMA_RING + 1)), reads, writes)

    def all_events(self):
        evs = [(self.sem[e], self.cnt[e]) for e in ENGS if self.cnt[e] > 0]
        for q, ring in self.ring.items():
            n = self.dn[q]
            for k, s in enumerate(ring):
                if n > k:
                    evs.append((s, 16 * ((n - k + DMA_RING - 1) // DMA_RING)))
        return evs

    def barrier(self):
        evs = self.all_events()
        for e in ENGS:
            self._waits(e, evs)

    def emit(self):
        nc = self.nc
        th = self.th
        with nc.Block() as block:
            @block.tensor
            def _(e):
                for t in th['pe']:
                    t(e)

            @block.vector
            def _(e):
                for t in th['dve']:
                    t(e)

            @block.scalar
            def _(e):
                for t in th['act']:
                    t(e)

            @block.gpsimd
            def _(e):
                for t in th['pool']:
                    t(e)

            @block.sync
            def _(e):
                for t in th['sp']:
                    t(e)


class Buf:
    __slots__ = ('ap', 'r')

    def __init__(self, ap, r):
        self.ap = ap
        self.r = r

    def __getitem__(self, k):
        return self.ap[k]


def _dsz(dt):
    return 2 if dt == BF16 else 4


class Arena:
    def __init__(self, nc, S, nwords):
        self.t = nc.alloc_sbuf_tensor('arena', [128, nwords], F32)
        self.S = S
        self.n = nwords
        self.top = 0

    def alloc(self, shape, dt=F32, name=''):
        free = int(np.prod(shape[1:]))
        nw = (free * _dsz(dt) + 31) // 32 * 8
        off = self.top
        self.top += nw
        assert self.top <= self.n, ('arena overflow', name, self.top, self.n)
        v = self.t[:, off:off + nw]
        if dt != F32:
            v = v.bitcast(dt)
        v = v[0:shape[0], 0:free]
        if len(shape) == 3:
            v = v.rearrange('p (a b) -> p a b', a=shape[1])
        elif len(shape) == 4:
            v = v.rearrange('p (a b c) -> p a b c', a=shape[1], b=shape[2])
        elif len(shape) == 5:
            v = v.rearrange('p (a b c d) -> p a b c d', a=shape[1], b=shape[2], c=shape[3])
        return Buf(v, Res(name))

    def mark(self):
        return self.top

    def release(self, m):
        self.S.barrier()
        self.top = m


def build(nlayers=4, dbg=()):
    nc = bass.Bass("TRN2", target_bir_lowering=False)
    st = ExitStack()
    S = Sched(nc, st)
    A = Arena(nc, S, 51200)

    def din(name, shape, dt=F32):
        return nc.dram_tensor(name, list(shape), dt, kind="ExternalInput")

    def dscr(name, shape, dt=F32):
        return nc.dram_tensor(name, list(shape), dt, kind=("ExternalOutput" if name in dbg else "Internal"))

    x_d = din('x', [T, D])
    ctx_d = din('ctx', [L, D])
    cc_d = din('cc', [2, D])
    ada_w = din('ada_w', [4, D, 6 * D])
    ada_b = din('ada_b', [4, 6 * D])
    norm1_g = din('norm1_g', [4, D])
    norm2_g = din('norm2_g', [4, D])
    ev_w_in = din('ev_w_in', [2, D, E_EV])
    ev_w_out = din('ev_w_out', [2, D, D])
    mla_q_norm = din('mla_q_norm', [2, 384])
    mla_w_uq = din('mla_w_uq', [2, 384, 768])
    mla_kv_norm = din('mla_kv_norm', [2, 256])
    mla_w_ukv = din('mla_w_ukv', [2, 256, 1024])
    mla_q_g = din('mla_q_g', [2, 96])
    mla_k_g = din('mla_k_g', [2, 96])
    cv_dw_w = din('cv_dw_w', [2, 31, 512])
    cv_dw_b = din('cv_dw_b', [2, 512])
    cv_ln_g = din('cv_ln_g', [2, 512])
    cv_ln_b = din('cv_ln_b', [2, 512])
    od_w_in = din('od_w_in', [2, D, E_OD])
    od_w_out = din('od_w_out', [2, D, D])
    rw_shift_w = din('rw_shift_w', [2, 3, RW_IN])
    rw_w0 = din('rw_w0', [2, 2, 512])
    rw_w2 = din('rw_w2', [2, 2, 64, 512])
    rw_a0 = din('rw_a0', [2, 2, 512])
    rw_a2 = din('rw_a2', [2, 2, 64, 512])
    rw_g2 = din('rw_g2', [2, 128, 512])
    rw_k_k = din('rw_k_k', [2, 512])
    rw_k_a = din('rw_k_a', [2, 512])
    rw_r_k = din('rw_r_k', [2, 512])
    rw_gn_g = din('rw_gn_g', [2, 512])
    rw_gn_b = din('rw_gn_b', [2, 512])
    na_q_g = din('na_q_g', [2, 64])
    na_k_g = din('na_k_g', [2, 64])
    nab_d = din('nab', [2, 8, 128, 8, 256])
    moe_router = din('moe_router', [4, D, 16])
    moe_w1 = din('moe_w1', [4, 16, D, D])
    moe_w3 = din('moe_w3', [4, 16, D, D])
    moe_w2 = din('moe_w2', [4, 16, D, D])
    ident_d = din('ident', [128, 128])
    flip_d = din('flip', [128, 128])
    rope_d = din('rope', [T, 64])
    iota_d = din('iota', [128, 256])
    tri_d = din('tri', [128, 128])
    blk_d = din('blk', [128, 128])
    msk_d = din('msk', [128, 6, 128])

    out_d = nc.dram_tensor('out', [T, D], F32, kind="ExternalOutput")
    xc_d = dscr('xc', [L, D])
    U_d = dscr('U', [NT, E_OD])
    CAT_d = dscr('CAT', [NT, D], BF16)
    H2_d = dscr('H2', [NT, D], BF16)
    QT_d = dscr('QT', [8, 96, NT], BF16)
    KT_d = dscr('KT', [8, 96, NT], BF16)
    VA_d = dscr('VA', [NT, 8 * 65], BF16)
    HGT_d = dscr('HGT', [512, NT])
    HCT_d = dscr('HCT', [512, NT])
    AFF_d = dscr('AFF', [16, NT])
    OPS_d = dscr('OPS', [2, NT, 2560])
    TM_d = dscr('TM', [2, 4, NT, 512], BF16)
    PLB_d = dscr('PLB', [2, NT, 512])
    FT_d = dscr('FT', [2, 4, 64, 8, NT], BF16)
    YS_d = dscr('YS', [2, NT, 512])
    VT_d = dscr('VT', [2, 512, NT])
    YT_d = dscr('YT', [2, 512, NT])
    BON_d = dscr('BON', [NT, 512])
    GG_d = dscr('GG', [NT, 512])

    def AP(t, off, pat):
        return bass.AP(t, off, [list(p) for p in pat])

    def I(eng, method, R=(), W=(), **kw):
        S.op(eng, lambda E, m=method, kw=kw: getattr(E, m)(**kw), R, W)

    def dma(eng, out, in_, R=(), W=(), **kw):
        S.dma(eng, lambda E, out=out, in_=in_, kw=kw: E.dma_start(out=out, in_=in_, **kw), R, W)

    def mm(out, lhsT, rhs, start, stop, R, W):
        S.op('pe', lambda E: E.matmul(out, lhsT, rhs, start=start, stop=stop), R, W)

    def tr(out, in_, ident, R, W):
        S.op('pe', lambda E: E.transpose(out, in_, ident), R, W)

    def xrows(tt):
        if tt < NLT:
            return out_d[tt * 128:(tt + 1) * 128, :]
        return xc_d[(tt - NLT) * 128:(tt - NLT + 1) * 128, :]

    def psum(name, shape, dt=F32):
        return Buf(nc.alloc_psum_tensor(name, list(shape), dt)[:], Res(name))

    PBALL = nc.alloc_psum_tensor('pball', [128, 4096], F32)
    PB = [Buf(PBALL[:, i * 512:(i + 1) * 512], Res('pb%d' % i)) for i in range(8)]
    PT = [Buf(PBALL[:, (6 + i) * 512:(7 + i) * 512].bitcast(BF16), PB[6 + i].r) for i in range(2)]

    XR = [Res('x%d' % t) for t in range(NTILE)]
    UR = [Res('u%d' % t) for t in range(NTILE)]
    CATR = [Res('cat%d' % t) for t in range(NTILE)]
    H2R = [Res('h2%d' % t) for t in range(NTILE)]
    QTR, KTR, VAR, HGTR, HCTR, AFFR = Res('qt'), Res('kt'), Res('va'), Res('hgt'), Res('hct'), Res('aff')
    OPSR, VTR, YTR, BONR, GGR = Res('ops'), Res('vt'), Res('yt'), Res('bon'), Res('gg')
    TMR, PLBR, FTR, YSR = Res('tm'), Res('plb'), Res('ft'), Res('ys')

    ident_f = A.alloc([128, 128], F32, 'ident_f')
    ident_b = A.alloc([128, 128], BF16, 'ident_b')
    flip_f = A.alloc([128, 128], F32, 'flip_f')
    SCB = A.alloc([128, 2, 8, 128], F32, 'scb')
    MODB = A.alloc([128, 8, 1024], F32, 'modb')
    dma('sp', ident_f[:, :], ident_d[:, :], W=[ident_f.r])
    dma('sp', flip_f[:, :], flip_d[:, :], W=[flip_f.r])
    I('dve', 'tensor_copy', R=[ident_f.r], W=[ident_b.r], out=ident_b[:, :], in_=ident_f[:, :])

    for tt in range(NTILE):
        src = x_d[tt * 128:(tt + 1) * 128, :] if tt < NLT else ctx_d[(tt - NLT) * 128:(tt - NLT + 1) * 128, :]
        dma('sp', xrows(tt), src, W=[XR[tt]])

    m0 = A.mark()
    ccol = A.alloc([128, 2, 8], F32, 'ccol')
    dma('sp', ccol[:, :, :], AP(cc_d, 0, [[1, 128], [D, 2], [128, 8]]), W=[ccol.r], allow_slow_non_contiguous=True)
    I('act', 'activation', R=[ccol.r], W=[ccol.r], out=ccol[:, :, :], in_=ccol[:, :, :], func=AF.Silu)
    for w in range(2):
        for kc in range(8):
            I('dve', 'tensor_copy', R=[ccol.r], W=[SCB.r], out=SCB[:, w, kc, :],
              in_=ccol[:, w, kc:kc + 1].to_broadcast([128, 128]))
    A.release(m0)

    def bc_load(eng, dst, t, off, n, R=()):
        dma(eng, dst, AP(t, off, [[0, 128], [1, n]]), R=R, W=[])

    def mod_pass(i, jobs):
        m = A.mark()
        biasb = A.alloc([128, 1024], F32, 'biasb')
        wst = [A.alloc([128, 8, 256], F32, 'wst%d' % k) for k in range(2)]
        k = 0
        for (j, sl, sc_) in jobs:
            dma('sp', biasb[:, :], AP(ada_b, i * 6 * D + j * D, [[0, 128], [1, D]]), W=[biasb.r])
            for n in range(4):
                wb = wst[k % 2]
                dma('sp', wb[:, :, :], AP(ada_w, i * D * 6 * D + j * D + n * 256, [[6 * D, 128], [128 * 6 * D, 8], [1, 256]]),
                    W=[wb.r])
                for w, slot in ((0, sl), (1, sc_)):
                    pb = PB[k % 2 * 2 + w]
                    for kc in range(8):
                        mm(pb[:, 0:256], SCB[:, w, kc, :], wb[:, kc, :], kc == 0, kc == 7, [SCB.r, wb.r], [pb.r])
                    I('dve', 'tensor_tensor', R=[pb.r, biasb.r], W=[MODB.r], out=MODB[:, slot, n * 256:(n + 1) * 256],
                      in0=pb[:, 0:256], in1=biasb[:, n * 256:(n + 1) * 256], op=ALU.add)
                k += 1
        A.release(m)

    def fold_gain(gain_t, i, slots):
        m = A.mark()
        gb = A.alloc([128, 1024], F32, 'gainb')
        dma('sp', gb[:, :], AP(gain_t, i * D, [[0, 128], [1, D]]), W=[gb.r])
        for s in slots:
            I('dve', 'scalar_tensor_tensor', R=[MODB.r, gb.r], W=[MODB.r], out=MODB[:, s, :], in0=MODB[:, s, :],
              scalar=1.0, in1=gb[:, :], op0=ALU.add, op1=ALU.mult)
        A.release(m)

    def rms_rstd(ss_ap, n, rs_ap, R, W, ecol=0):
        I('act', 'activation', R=R, W=R, out=ss_ap, in_=ss_ap, func=AF.Sqrt, scale=1.0 / n, bias=epsb[:, ecol:ecol + 1])
        I('dve', 'reciprocal', R=R, W=W, out=rs_ap, in_=ss_ap)

    epsb = A.alloc([128, 4], F32, 'epsb')
    I('dve', 'memset', W=[epsb.r], ap=epsb[:, 0:1], constant=EPS)
    I('dve', 'memset', W=[epsb.r], ap=epsb[:, 1:2], constant=64e-5)
    I('dve', 'memset', W=[epsb.r], ap=epsb[:, 2:3], constant=1e-12)

    def load_w_bf16(dst, wt, base, K, N, R=()):
        nch = (N + 1023) // 1024
        cw = N // nch
        assert cw * nch == N
        for kc in range(K // 128):
            for c in range(nch):
                dma('pool', dst[:, kc, c * cw:(c + 1) * cw], AP(wt, base + kc * 128 * N + c * cw, [[N, 128], [1, cw]]), R=R, W=[dst.r])

    def norm_mod_tile(xt, G_ap, SH_ap, hf, hout, ssb, R_mod):
        I('act', 'activation', R=[xt.r], W=[hf.r, ssb.r], out=hf[:, :], in_=xt[:, :], func=AF.Square, accum_out=ssb[:, 0:1])
        rms_rstd(ssb[:, 0:1], D, ssb[:, 1:2], [ssb.r], [ssb.r])
        I('dve', 'scalar_tensor_tensor', R=[xt.r, ssb.r] + R_mod, W=[hf.r], out=hf[:, :], in0=xt[:, :],
          scalar=ssb[:, 1:2], in1=G_ap, op0=ALU.mult, op1=ALU.mult)
        I('pool', 'tensor_tensor', R=[hf.r] + R_mod, W=[hout.r], out=hout[:, :], in0=hf[:, :], in1=SH_ap, op=ALU.add)

    def transpose8(src, dstT, pt, ident, n=8):
        for kc in range(n):
            tr(pt[:, kc * 128:(kc + 1) * 128], src[:, kc * 128:(kc + 1) * 128], ident[:, :], [src.r, ident.r], [pt.r])
        I('act', 'copy', R=[pt.r], W=[dstT.r], out=dstT[:, 0:n * 128], in_=pt[:, 0:n * 128])

    def phase_inproj(i, w_in_t, j, E):
        mod_pass(i, [(0, 0, 2), (1, 1, 3)])
        fold_gain(norm1_g, i, [1, 3])
        m = A.mark()
        WIN = A.alloc([128, 8, E], BF16, 'win')
        load_w_bf16(WIN, w_in_t, j * D * E, D, E)
        xts = [A.alloc([128, 1024], F32, 'xt%d' % k) for k in range(2)]
        hfs = [A.alloc([128, 1024], F32, 'hf%d' % k) for k in range(2)]
        hbs = [A.alloc([128, 1024], BF16, 'hb%d' % k) for k in range(2)]
        hTs = [A.alloc([128, 1024], BF16, 'hT%d' % k) for k in range(2)]
        ssbs = [A.alloc([128, 2], F32, 'ssb%d' % k) for k in range(2)]
        uts = [A.alloc([128, E], F32, 'ut%d' % k) for k in range(2)]
        chunks = [(n0, min(E, n0 + 512)) for n0 in range(0, E, 512)]
        for tt in range(NTILE):
            xt = xts[tt % 2]
            ut = uts[tt % 2]
            hf, hb, hT, ssb = hfs[tt % 2], hbs[tt % 2], hTs[tt % 2], ssbs[tt % 2]
            lat = tt < NLT
            dma('sp', xt[:, :], xrows(tt), R=[XR[tt]], W=[xt.r])
            norm_mod_tile(xt, MODB[:, 1 if lat else 3, :], MODB[:, 0 if lat else 2, :], hf, hb, ssb, [MODB.r])
            transpose8(hb, hT, PT[tt % 2], ident_b)
            for ci, (n0, n1) in enumerate(chunks):
                pb = PB[ci % 4]
                for kc in range(8):
                    mm(pb[:, 0:n1 - n0], hT[:, kc * 128:(kc + 1) * 128], WIN[:, kc, n0:n1], kc == 0, kc == 7,
                       [hT.r, WIN.r], [pb.r])
                I('act' if ci % 2 else 'dve', 'tensor_copy' if ci % 2 == 0 else 'copy', R=[pb.r], W=[ut.r],
                  out=ut[:, n0:n1], in_=pb[:, 0:n1 - n0])
            dma('sp', U_d[tt * 128:(tt + 1) * 128, 0:E], ut[:, :], R=[ut.r], W=[UR[tt]])
        A.release(m)

    def phase_mla_prep(j):
        m = A.mark()
        WUQ = A.alloc([128, 3, 768], BF16, 'wuq')
        WUKV = A.alloc([128, 2, 1024], BF16, 'wukv')
        load_w_bf16(WUQ, mla_w_uq, j * 384 * 768, 384, 768)
        load_w_bf16(WUKV, mla_w_ukv, j * 256 * 1024, 256, 1024)
        QNB = A.alloc([128, 384], F32, 'qnb')
        KVNB = A.alloc([128, 256], F32, 'kvnb')
        QGB = A.alloc([128, 96], F32, 'qgb')
        KGB = A.alloc([128, 96], F32, 'kgb')
        cst = Res('mlaconst')
        for dst, t, off, n in ((QNB, mla_q_norm, j * 384, 384), (KVNB, mla_kv_norm, j * 256, 256),
                               (QGB, mla_q_g, j * 96, 96), (KGB, mla_k_g, j * 96, 96)):
            dma('sp', dst[:, :], AP(t, off, [[0, 128], [1, n]]), W=[cst])
        uqs = [A.alloc([128, 672], F32, 'uq%d' % k) for k in range(2)]
        rps = [A.alloc([128, 64], F32, 'rp%d' % k) for k in range(2)]
        junk = A.alloc([128, 768], F32, 'junk')
        st_ = A.alloc([128, 32], F32, 'stats')
        cn = A.alloc([128, 640], BF16, 'cn')
        cT = A.alloc([128, 640], BF16, 'cT')
        qf = A.alloc([128, 8, 96], F32, 'qf')
        kvf = A.alloc([128, 8, 128], F32, 'kvf')
        kn = A.alloc([128, 8, 96], F32, 'kn')
        t1 = A.alloc([128, 8, 32], F32, 't1')
        t2 = A.alloc([128, 8, 32], F32, 't2')
        qkb = [A.alloc([128, 8, 96], BF16, 'qkb%d' % k) for k in range(2)]
        qkT = [A.alloc([96, 8, 128], BF16, 'qkT%d' % k) for k in range(2)]
        vas = [A.alloc([128, 8, 65], BF16, 'va%d' % k) for k in range(2)]
        for k in range(2):
            I('dve', 'memset', W=[vas[k].r], ap=vas[k][:, :, :], constant=1.0)

        def head_norm(src3, n, ssq, rq):
            I('dve', 'tensor_tensor', R=[qf.r, kvf.r], W=[junk.r], out=junk[:, 0:8 * n].rearrange('p (a b) -> p a b', a=8),
              in0=src3, in1=src3, op=ALU.mult)
            I('dve', 'tensor_reduce', R=[junk.r], W=[st_.r], out=ssq,
              in_=junk[:, 0:8 * n].rearrange('p (a b) -> p a b', a=8), axis=AX.X, op=ALU.add)

        def rope(x3, rp, xr):
            cosb = rp[:, 0:32].unsqueeze(1).to_broadcast([128, 8, 32])
            I('dve', 'tensor_tensor', R=[xr, rp.r], W=[t1.r], out=t1[:, :, :], in0=x3, in1=cosb, op=ALU.mult)
            x4 = x3.rearrange('p h (a b) -> p h a b', a=4)
            t4 = t2[:, :, :].rearrange('p h (a b) -> p h a b', a=4)
            s4 = rp[:, 32:64].rearrange('p (a b) -> p a b', a=4)
            for a_dst, a_src in ((0, 1), (1, 0), (2, 3), (3, 2)):
                I('dve', 'tensor_tensor', R=[xr, rp.r], W=[t2.r], out=t4[:, :, a_dst, :], in0=x4[:, :, a_src, :],
                  in1=s4[:, a_dst, :].unsqueeze(1).to_broadcast([128, 8, 8]), op=ALU.mult)
            I('dve', 'tensor_tensor', R=[t1.r, t2.r], W=[xr], out=x3, in0=t1[:, :, :], in1=t2[:, :, :], op=ALU.add)

        for tt in range(NTILE):
            lat = tt < NLT
            uq = uqs[tt % 2]
            rp = rps[tt % 2]
            va = vas[tt % 2]
            dma('sp', uq[:, :], U_d[tt * 128:(tt + 1) * 128, 0:672], R=[UR[tt]], W=[uq.r])
            if lat:
                dma('sp', rp[:, :], rope_d[tt * 128:(tt + 1) * 128, :], W=[rp.r])
            I('act', 'activation', R=[uq.r], W=[junk.r, st_.r], out=junk[:, 0:384], in_=uq[:, 0:384], func=AF.Square,
              accum_out=st_[:, 0:1])
            I('act', 'activation', R=[uq.r], W=[junk.r, st_.r], out=junk[:, 0:256], in_=uq[:, 384:640], func=AF.Square,
              accum_out=st_[:, 1:2])
            I('act', 'activation', R=[uq.r], W=[junk.r, st_.r], out=junk[:, 0:32], in_=uq[:, 640:672], func=AF.Square,
              accum_out=st_[:, 2:3])
            rms_rstd(st_[:, 0:1], 384, st_[:, 4:5], [st_.r], [st_.r])
            rms_rstd(st_[:, 1:2], 256, st_[:, 5:6], [st_.r], [st_.r])
            I('dve', 'scalar_tensor_tensor', R=[uq.r, st_.r, cst], W=[cn.r], out=cn[:, 0:384], in0=uq[:, 0:384],
              scalar=st_[:, 4:5], in1=QNB[:, :], op0=ALU.mult, op1=ALU.mult)
            I('dve', 'scalar_tensor_tensor', R=[uq.r, st_.r, cst], W=[cn.r], out=cn[:, 384:640], in0=uq[:, 384:640],
              scalar=st_[:, 5:6], in1=KVNB[:, :], op0=ALU.mult, op1=ALU.mult)
            transpose8(cn, cT, PT[tt % 2], ident_b, n=5)
            for ci, (n0, n1) in enumerate(((0, 512), (512, 768))):
                pb = PB[ci]
                for kc in range(3):
                    mm(pb[:, 0:n1 - n0], cT[:, kc * 128:(kc + 1) * 128], WUQ[:, kc, n0:n1], kc == 0, kc == 2, [cT.r, WUQ.r], [pb.r])
                I('act', 'copy', R=[pb.r], W=[qf.r], out=qf[:, :, :].rearrange('p a b -> p (a b)')[:, n0:n1], in_=pb[:, 0:n1 - n0])
            for ci in range(2):
                pb = PB[2 + ci]
                for kc in range(2):
                    mm(pb[:, :], cT[:, (3 + kc) * 128:(4 + kc) * 128], WUKV[:, kc, ci * 512:(ci + 1) * 512], kc == 0, kc == 1,
                       [cT.r, WUKV.r], [pb.r])
                I('act', 'copy', R=[pb.r], W=[kvf.r], out=kvf[:, :, :].rearrange('p a b -> p (a b)')[:, ci * 512:(ci + 1) * 512],
                  in_=pb[:, :])
            head_norm(qf[:, :, :], 96, st_[:, 8:16], None)
            rms_rstd(st_[:, 8:16], 96, st_[:, 16:24], [st_.r], [st_.r])
            I('dve', 'tensor_tensor', R=[qf.r, st_.r], W=[qf.r], out=qf[:, :, :], in0=qf[:, :, :],
              in1=st_[:, 16:24].unsqueeze(2).to_broadcast([128, 8, 96]), op=ALU.mult)
            I('dve', 'tensor_tensor', R=[qf.r, cst], W=[qf.r], out=qf[:, :, :], in0=qf[:, :, :],
              in1=QGB[:, :].unsqueeze(1).to_broadcast([128, 8, 96]), op=ALU.mult)
            head_norm(kvf[:, :, 0:64], 64, st_[:, 8:16], None)
            I('dve', 'tensor_scalar', R=[st_.r], W=[st_.r], out=st_[:, 8:16], in0=st_[:, 8:16], scalar1=st_[:, 2:3], scalar2=None,
              op0=ALU.add)
            rms_rstd(st_[:, 8:16], 96, st_[:, 24:32], [st_.r], [st_.r])
            I('dve', 'tensor_tensor', R=[kvf.r, st_.r], W=[kn.r], out=kn[:, :, 0:64], in0=kvf[:, :, 0:64],
              in1=st_[:, 24:32].unsqueeze(2).to_broadcast([128, 8, 64]), op=ALU.mult)
            I('dve', 'tensor_tensor', R=[uq.r, st_.r], W=[kn.r], out=kn[:, :, 64:96],
              in0=uq[:, 640:672].unsqueeze(1).to_broadcast([128, 8, 32]),
              in1=st_[:, 24:32].unsqueeze(2).to_broadcast([128, 8, 32]), op=ALU.mult)
            I('dve', 'tensor_tensor', R=[kn.r, cst], W=[kn.r], out=kn[:, :, :], in0=kn[:, :, :],
              in1=KGB[:, :].unsqueeze(1).to_broadcast([128, 8, 96]), op=ALU.mult)
            if lat:
                rope(qf[:, :, 64:96], rp, qf.r)
                rope(kn[:, :, 64:96], rp, kn.r)
            I('pool', 'tensor_copy', R=[kvf.r], W=[va.r], out=va[:, :, 0:64], in_=kvf[:, :, 64:128])
            dma('sp', VA_d[tt * 128:(tt + 1) * 128, :], va[:, :, :].rearrange('p a b -> p (a b)'), R=[va.r], W=[VAR])
            for which, (src, dst_d, dres) in enumerate(((qf, QT_d, QTR), (kn, KT_d, KTR))):
                qb = qkb[which]
                qT = qkT[which]
                pt = PT[which]
                I('pool', 'tensor_copy', R=[src.r], W=[qb.r], out=qb[:, :, :], in_=src[:, :, :])
                for h in range(8):
                    tr(pt[0:96, h * 128:(h + 1) * 128], qb[:, h, :], ident_b[:, :], [qb.r, ident_b.r], [pt.r])
                I('act', 'copy', R=[pt.r], W=[qT.r], out=qT[:, :, :].rearrange('p a b -> p (a b)'), in_=pt[0:96, :])
                dma('sp', AP(dst_d, tt * 128, [[NT, 96], [96 * NT, 8], [1, 128]]), qT[:, :, :], R=[qT.r], W=[dres])
        A.release(m)

    def phase_attention(dh, scale):
        m = A.mark()
        kTs = [A.alloc([dh, NT], BF16, 'kT%d' % k) for k in range(2)]
        qTs = [A.alloc([dh, NT], BF16, 'qT%d' % k) for k in range(2)]
        vhs = [A.alloc([128, NTILE, 65], BF16, 'vh%d' % k) for k in range(2)]
        pts = [A.alloc([128, 512], BF16, 'pT%d' % k) for k in range(3)]
        oat = [A.alloc([128, NTILE, 64], BF16, 'oat%d' % k) for k in range(2)]
        rc = A.alloc([128, 4], F32, 'rc')
        PO = [PB[2], PB[3], PB[4], PB[5]]
        ei = 0
        for h in range(8):
            kT, qT, vh, oa = kTs[h % 2], qTs[h % 2], vhs[h % 2], oat[h % 2]
            dma('sp', kT[:, :], KT_d[h, 0:dh, :], R=[KTR], W=[kT.r])
            dma('sp', qT[:, :], QT_d[h, 0:dh, :], R=[QTR], W=[qT.r])
            dma('sp', vh[:, :, :], AP(VA_d, h * 65, [[520, 128], [128 * 520, NTILE], [1, 65]]), R=[VAR], W=[vh.r])
            blocks = [(qb * 512, 512, list(range(NTILE))) for qb in range(8)] + [(T, 256, [NLT, NLT + 1])]
            for (q0, nq, kts) in blocks:
                nqs = nq // 128
                def pv(pT, kt, ki):
                    for qs in range(nqs):
                        mm(PO[qs][:, 0:65], pT[:, qs * 128:(qs + 1) * 128], vh[:, kt, :], ki == 0, ki == len(kts) - 1,
                           [pT.r, vh.r], [PO[qs].r])
                prev = None
                for ki, kt in enumerate(kts):
                    pb = PB[ei % 2]
                    pT = pts[ei % 3]
                    ei += 1
                    mm(pb[:, 0:nq], kT[:, kt * 128:(kt + 1) * 128], qT[:, q0:q0 + nq], True, True, [kT.r, qT.r], [pb.r])
                    I('act', 'activation', R=[pb.r], W=[pT.r], out=pT[:, 0:nq], in_=pb[:, 0:nq], func=AF.Exp, scale=scale)
                    if prev is not None:
                        pv(*prev)
                    prev = (pT, kt, ki)
                pv(*prev)
                for qs in range(nqs):
                    I('dve', 'reciprocal', R=[PO[qs].r], W=[rc.r], out=rc[:, qs:qs + 1], in_=PO[qs][:, 64:65])
                    I('dve', 'tensor_scalar', R=[PO[qs].r, rc.r], W=[oa.r], out=oa[:, q0 // 128 + qs, :], in0=PO[qs][:, 0:64],
                      scalar1=rc[:, qs:qs + 1], scalar2=None, op0=ALU.mult)
            for tt in range(NTILE):
                pass
            dma('sp', AP(CAT_d, h * 64, [[D, 128], [128 * D, NTILE], [1, 64]]), oa[:, :, :], R=[oa.r], W=CATR)
        A.release(m)

    def phase_conv(j):
        m = A.mark()
        wraw = A.alloc([32, 512], F32, 'wraw')
        DW = A.alloc([128, 4, 32], F32, 'dw')
        I('dve', 'memset', W=[wraw.r], ap=wraw[:, :], constant=0.0)
        dma('sp', wraw[0:31, :], cv_dw_w[j, :, :], W=[wraw.r])
        dma('sp', wraw[31:32, :], cv_dw_b[j:j + 1, :], W=[wraw.r])
        for ct in range(4):
            tr(PB[0][:, ct * 32:(ct + 1) * 32], wraw[:, ct * 128:(ct + 1) * 128], ident_f[0:32, 0:32], [wraw.r, ident_f.r], [PB[0].r])
        I('act', 'copy', R=[PB[0].r], W=[DW.r], out=DW[:, :, :].rearrange('p a b -> p (a b)'), in_=PB[0][:, 0:128])
        LNG = A.alloc([128, 512], F32, 'lng')
        LNB = A.alloc([128, 512], F32, 'lnb')
        cst = Res('cvconst')
        dma('sp', LNG[:, :], AP(cv_ln_g, j * 512, [[0, 128], [1, 512]]), W=[cst])
        dma('sp', LNB[:, :], AP(cv_ln_b, j * 512, [[0, 128], [1, 512]]), W=[cst])
        m2 = A.mark()
        ugs = [A.alloc([128, 1024], F32, 'ug%d' % k) for k in range(2)]
        sg = A.alloc([128, 512], F32, 'sg')
        hgs = [A.alloc([128, 4, 128], F32, 'hg%d' % k) for k in range(2)]
        for tt in range(NTILE):
            ug = ugs[tt % 2]
            hg = hgs[tt % 2]
            pb = PB[1 + tt % 2]
            dma('sp', ug[:, :], U_d[tt * 128:(tt + 1) * 128, 672:1696], R=[UR[tt]], W=[ug.r])
            I('act', 'activation', R=[ug.r], W=[sg.r], out=sg[:, :], in_=ug[:, 512:1024], func=AF.Sigmoid)
            I('dve', 'tensor_tensor', R=[ug.r, sg.r], W=[sg.r], out=sg[:, :], in0=ug[:, 0:512], in1=sg[:, :], op=ALU.mult)
            for ct in range(4):
                tr(pb[:, ct * 128:(ct + 1) * 128], sg[:, ct * 128:(ct + 1) * 128], ident_f[:, :], [sg.r, ident_f.r], [pb.r])
            I('act', 'copy', R=[pb.r], W=[hg.r], out=hg[:, :, :].rearrange('p a b -> p (a b)'), in_=pb[:, :])
            dma('sp', AP(HGT_d, tt * 128, [[NT, 128], [128 * NT, 4], [1, 128]]), hg[:, :, :], R=[hg.r], W=[HGTR])
        A.release(m2)
        m2 = A.mark()
        cv = A.alloc([128, T + 32], F32, 'cv')
        acc = A.alloc([128, T], F32, 'acc')
        for ct in range(4):
            for (t0, n) in ((0, T), (T, L)):
                I('pool', 'memset', W=[cv.r], ap=cv[:, 0:16], constant=0.0)
                I('pool', 'memset', W=[cv.r], ap=cv[:, 15 + n:15 + n + 16], constant=0.0)
                dma('sp', cv[:, 15:15 + n], HGT_d[ct * 128:(ct + 1) * 128, t0:t0 + n], R=[HGTR], W=[cv.r])
                I('dve', 'tensor_scalar', R=[cv.r, DW.r], W=[acc.r], out=acc[:, 0:n], in0=cv[:, 0:n], scalar1=DW[:, ct, 0:1],
                  scalar2=DW[:, ct, 31:32], op0=ALU.mult, op1=ALU.add)
                for k in range(1, 31):
                    I('dve', 'scalar_tensor_tensor', R=[cv.r, DW.r, acc.r], W=[acc.r], out=acc[:, 0:n], in0=cv[:, k:k + n],
                      scalar=DW[:, ct, k:k + 1], in1=acc[:, 0:n], op0=ALU.mult, op1=ALU.add)
                dma('sp', HCT_d[ct * 128:(ct + 1) * 128, t0:t0 + n], acc[:, 0:n], R=[acc.r], W=[HCTR])
        A.release(m2)
        m2 = A.mark()
        hcs = [A.alloc([128, 4, 128], F32, 'hc%d' % k) for k in range(2)]
        xc = A.alloc([128, 512], F32, 'xc_')
        jk = A.alloc([128, 512], F32, 'jk')
        stt = A.alloc([128, 4], F32, 'stt')
        ocs = [A.alloc([128, 512], BF16, 'oc%d' % k) for k in range(2)]
        for tt in range(NTILE):
            hc = hcs[tt % 2]
            oc = ocs[tt % 2]
            pb = PB[1 + tt % 2]
            dma('sp', hc[:, :, :], AP(HCT_d, tt * 128, [[NT, 128], [128 * NT, 4], [1, 128]]), R=[HCTR], W=[hc.r])
            for ct in range(4):
                tr(pb[:, ct * 128:(ct + 1) * 128], hc[:, ct, :], ident_f[:, :], [hc.r, ident_f.r], [pb.r])
            I('dve', 'tensor_reduce', R=[pb.r], W=[stt.r], out=stt[:, 0:1], in_=pb[:, :], axis=AX.X, op=ALU.add)
            I('dve', 'tensor_scalar', R=[stt.r], W=[stt.r], out=stt[:, 1:2], in0=stt[:, 0:1], scalar1=-1.0 / 512, scalar2=None,
              op0=ALU.mult)
            I('dve', 'tensor_scalar', R=[pb.r, stt.r], W=[xc.r], out=xc[:, :], in0=pb[:, :], scalar1=stt[:, 1:2], scalar2=None,
              op0=ALU.add)
            I('act', 'activation', R=[xc.r], W=[jk.r, stt.r], out=jk[:, :], in_=xc[:, :], func=AF.Square, accum_out=stt[:, 2:3])
            rms_rstd(stt[:, 2:3], 512, stt[:, 3:4], [stt.r], [stt.r])
            I('dve', 'scalar_tensor_tensor', R=[xc.r, stt.r, cst], W=[xc.r], out=xc[:, :], in0=xc[:, :], scalar=stt[:, 3:4],
              in1=LNG[:, :], op0=ALU.mult, op1=ALU.mult)
            I('pool', 'tensor_tensor', R=[xc.r, cst], W=[xc.r], out=xc[:, :], in0=xc[:, :], in1=LNB[:, :], op=ALU.add)
            I('act', 'activation', R=[xc.r], W=[oc.r], out=oc[:, :], in_=xc[:, :], func=AF.Silu)
            dma('sp', CAT_d[tt * 128:(tt + 1) * 128, 512:1024], oc[:, :], R=[oc.r], W=[CATR[tt]])
        A.release(m2)
        A.release(m)

    def phase_outproj(i, w_out_t, j, AFFT):
        mod_pass(i, [(2, 0, 1), (3, 2, 3), (4, 4, 5), (5, 6, 7)])
        fold_gain(norm2_g, i, [4, 5])
        m = A.mark()
        WOUT = A.alloc([128, 8, D], BF16, 'wout')
        load_w_bf16(WOUT, w_out_t, j * D * D, D, D)
        ROUT = A.alloc([128, 8, 16], F32, 'rout')
        dma('sp', ROUT[:, :, :], AP(moe_router, i * D * 16, [[16, 128], [128 * 16, 8], [1, 16]]), W=[ROUT.r])
        cbs = [A.alloc([128, 1024], BF16, 'cb%d' % k) for k in range(2)]
        cTs = [A.alloc([128, 1024], BF16, 'cT5%d' % k) for k in range(2)]
        xts = [A.alloc([128, 1024], F32, 'xt5%d' % k) for k in range(2)]
        hfs = [A.alloc([128, 1024], F32, 'hf5%d' % k) for k in range(2)]
        h2fs = [A.alloc([128, 1024], F32, 'h2f%d' % k) for k in range(2)]
        h2bs = [A.alloc([128, 1024], BF16, 'h2b%d' % k) for k in range(2)]
        h2Ts = [A.alloc([128, 1024], F32, 'h2T%d' % k) for k in range(2)]
        ssbs = [A.alloc([128, 2], F32, 'ssb5%d' % k) for k in range(2)]
        sms = [A.alloc([128, 40], F32, 'sm%d' % k) for k in range(2)]
        for tt in range(NTILE):
            lat = tt < NLT
            w = 0 if lat else 1
            cb, xt, h2b = cbs[tt % 2], xts[tt % 2], h2bs[tt % 2]
            cT, hf, h2f, h2T, ssb, sm = cTs[tt % 2], hfs[tt % 2], h2fs[tt % 2], h2Ts[tt % 2], ssbs[tt % 2], sms[tt % 2]
            dma('sp', cb[:, :], CAT_d[tt * 128:(tt + 1) * 128, :], R=[CATR[tt]], W=[cb.r])
            dma('sp', xt[:, :], xrows(tt), R=[XR[tt]], W=[xt.r])
            transpose8(cb, cT, PT[tt % 2], ident_b)
            for ci in range(2):
                pb = PB[ci]
                for kc in range(8):
                    mm(pb[:, :], cT[:, kc * 128:(kc + 1) * 128], WOUT[:, kc, ci * 512:(ci + 1) * 512], kc == 0, kc == 7,
                       [cT.r, WOUT.r], [pb.r])
                I('dve', 'tensor_tensor', R=[pb.r, MODB.r], W=[hf.r], out=hf[:, ci * 512:(ci + 1) * 512], in0=pb[:, :],
                  in1=MODB[:, 0 + w, ci * 512:(ci + 1) * 512], op=ALU.mult)
            I('pool', 'tensor_tensor', R=[hf.r, xt.r], W=[xt.r], out=xt[:, :], in0=xt[:, :], in1=hf[:, :], op=ALU.add)
            dma('sp', xrows(tt), xt[:, :], R=[xt.r], W=[XR[tt]])
            norm_mod_tile(xt, MODB[:, 4 + w, :], MODB[:, 2 + w, :], hf, h2f, ssb, [MODB.r])
            I('act', 'copy', R=[h2f.r], W=[h2b.r], out=h2b[:, :], in_=h2f[:, :])
            dma('sp', H2_d[tt * 128:(tt + 1) * 128, :], h2b[:, :], R=[h2b.r], W=[H2R[tt]])
            for half in range(2):
                pb = PB[2 + half]
                for kc in range(4):
                    tr(pb[:, kc * 128:(kc + 1) * 128], h2f[:, (half * 4 + kc) * 128:(half * 4 + kc + 1) * 128], ident_f[:, :],
                       [h2f.r, ident_f.r], [pb.r])
                I('act' if half else 'dve', 'copy' if half else 'tensor_copy', R=[pb.r], W=[h2T.r],
                  out=h2T[:, half * 512:(half + 1) * 512], in_=pb[:, :])
            pl = PB[4]
            for kc in range(8):
                mm(pl[:, 0:16], h2T[:, kc * 128:(kc + 1) * 128], ROUT[:, kc, :], kc == 0, kc == 7, [h2T.r, ROUT.r], [pl.r])
            I('dve', 'tensor_reduce', R=[pl.r], W=[sm.r], out=sm[:, 0:1], in_=pl[:, 0:16], axis=AX.X, op=ALU.max)
            I('dve', 'tensor_scalar', R=[sm.r], W=[sm.r], out=sm[:, 1:2], in0=sm[:, 0:1], scalar1=-1.0, scalar2=None, op0=ALU.mult)
            I('act', 'activation', R=[pl.r, sm.r], W=[sm.r], out=sm[:, 8:24], in_=pl[:, 0:16], func=AF.Exp, bias=sm[:, 1:2],
              accum_out=sm[:, 2:3])
            I('dve', 'reciprocal', R=[sm.r], W=[sm.r], out=sm[:, 3:4], in_=sm[:, 2:3])
            I('dve', 'tensor_scalar', R=[sm.r], W=[sm.r], out=sm[:, 24:40], in0=sm[:, 8:24], scalar1=sm[:, 3:4], scalar2=None,
              op0=ALU.mult)
            pa = PB[5]
            tr(pa[0:16, 0:128], sm[:, 24:40], ident_f[:, :], [sm.r, ident_f.r], [pa.r])
            I('act', 'copy', R=[pa.r], W=[AFFT.r], out=AFFT[:, tt * 128:(tt + 1) * 128], in_=pa[0:16, 0:128])
        if 'AFF' in dbg:
            dma('sp', AFF_d[:, :], AFFT[:, :], R=[AFFT.r], W=[AFFR])
        A.release(m)

    def phase_topk(AFFT, GT, IXG, IXS, with_ctx):
        m = A.mark()
        NR = 64
        MX = A.alloc([16, 512 + 32], F32, 'mx')
        IDX = A.alloc([16, 512 + 32], U32, 'idx')
        IDF = A.alloc([16, 512 + 32], F32, 'idf')
        WK = A.alloc([16, T], F32, 'wk')
        I('dve', 'tensor_copy', R=[AFFT.r], W=[WK.r], out=WK[:, :], in_=AFFT[:, 0:T])
        for r in range(NR):
            sl = slice(r * 8, (r + 1) * 8)
            I('dve', 'max', R=[WK.r], W=[MX.r], out=MX[:, sl], in_=WK[:, :])
            I('dve', 'max_index', R=[WK.r, MX.r], W=[IDX.r], out=IDX[:, sl], in_max=MX[:, sl], in_values=WK[:, :])
            if r < NR - 1:
                I('dve', 'match_replace', R=[WK.r, MX.r], W=[WK.r], out=WK[:, :], in_to_replace=MX[:, sl], in_values=WK[:, :],
                  imm_value=-1.0)
        nslot = 4
        if with_ctx:
            nslot = 5
            I('dve', 'tensor_copy', R=[AFFT.r], W=[WK.r], out=WK[:, 0:L], in_=AFFT[:, T:NT])
            for r in range(4):
                sl = slice(512 + r * 8, 512 + (r + 1) * 8)
                I('dve', 'max', R=[WK.r], W=[MX.r], out=MX[:, sl], in_=WK[:, 0:L])
                I('dve', 'max_index', R=[WK.r, MX.r], W=[IDX.r], out=IDX[:, sl], in_max=MX[:, sl], in_values=WK[:, 0:L])
                if r < 3:
                    I('dve', 'match_replace', R=[WK.r, MX.r], W=[WK.r], out=WK[:, 0:L], in_to_replace=MX[:, sl],
                      in_values=WK[:, 0:L], imm_value=-1.0)
        I('dve', 'tensor_copy', R=[IDX.r], W=[IDF.r], out=IDF[:, :], in_=IDX[:, :])
        IXF = A.alloc([128, 5, 16], F32, 'ixf')
        I('dve', 'memset', W=[GT.r], ap=GT[:, :, :], constant=0.0)
        I('dve', 'memset', W=[IXF.r], ap=IXF[:, :, :], constant=0.0)
        for s in range(nslot):
            n = 128 if s < 4 else 32
            pa = PB[s % 2]
            tr(pa[0:n, 0:16], MX[:, s * 128:s * 128 + n], ident_f[0:16, 0:16], [MX.r, ident_f.r], [pa.r])
            tr(pa[0:n, 16:32], IDF[:, s * 128:s * 128 + n], ident_f[0:16, 0:16], [IDF.r, ident_f.r], [pa.r])
            I('act', 'copy', R=[pa.r], W=[GT.r], out=GT[0:n, s, :], in_=pa[0:n, 0:16])
            I('act', 'copy', R=[pa.r], W=[IXF.r], out=IXF[0:n, s, :], in_=pa[0:n, 16:32])
        I('dve', 'tensor_copy', R=[IXF.r], W=[IXS.r], out=IXS[:, :, :], in_=IXF[:, :, :])
        if with_ctx:
            I('dve', 'tensor_scalar', R=[IXF.r], W=[IXF.r], out=IXF[:, 4, :], in0=IXF[:, 4, :], scalar1=float(T), scalar2=None,
              op0=ALU.add)
        I('dve', 'tensor_copy', R=[IXF.r], W=[IXG.r], out=IXG[:, :, :], in_=IXF[:, :, :])
        A.release(m)

    def phase_moe(i, GT, IXG, IXS, with_ctx):
        m = A.mark()
        nslot = 5 if with_ctx else 4
        NC_ = 512 + (32 if with_ctx else 0)
        WBs = [[A.alloc([128, 8, D], BF16, 'w%d_%d' % (k, q)) for q in range(3)] for k in range(2)]
        xss = [[A.alloc([128, 1024], BF16, 'xs%d_%d' % (k, s_)) for s_ in range(nslot)] for k in range(2)]
        XST = A.alloc([128, 8, 544], BF16, 'xst')
        HIDT = A.alloc([128, 8, 544], BF16, 'hidt')
        s1 = [A.alloc([128, 544], F32, 's1_%d' % k) for k in range(2)]
        yss = [A.alloc([128, 1024], F32, 'ys%d' % k) for k in range(2)]
        gi = 0

        def load_weights(e):
            W1, W3, W2 = WBs[e % 2]
            for wt, dst in ((moe_w1, W1), (moe_w3, W3), (moe_w2, W2)):
                load_w_bf16(dst, wt, (i * 16 + e) * D * D, D, D)

        def issue_gathers(e):
            for s in range(nslot):
                n = 128 if s < 4 else 32
                xs = xss[e % 2][s]
                S.dma('pool', lambda E, xs=xs, s=s, e=e, n=n: E.indirect_dma_start(
                    out=xs[0:n, :], out_offset=None, in_=H2_d[:, :],
                    in_offset=bass.IndirectOffsetOnAxis(ap=IXG[0:n, s, e:e + 1], axis=0)),
                    reads=H2R + [IXG.r], writes=[xs.r])

        load_weights(0)
        issue_gathers(0)
        for e in range(16):
            W1, W3, W2 = WBs[e % 2]
            for s in range(nslot):
                n = 128 if s < 4 else 32
                xs = xss[e % 2][s]
                pt = PT[gi % 2]
                gi += 1
                for kc in range(8):
                    tr(pt[:, kc * 128:kc * 128 + n], xs[0:n, kc * 128:(kc + 1) * 128], ident_b[0:n, 0:n], [xs.r, ident_b.r], [pt.r])
                I('act', 'copy', R=[pt.r], W=[XST.r], out=XST[:, :, s * 128:s * 128 + n],
                  in_=pt[:, :].rearrange('p (a b) -> p a b', a=8)[:, :, 0:n])
            if e + 1 < 16:
                load_weights(e + 1)
                issue_gathers(e + 1)
            for fc in range(8):
                cols = [(0, 512)] + ([(512, 544)] if with_ctx else [])
                pbs = {}
                for wi, Wm in enumerate((W1, W3)):
                    for ci, (c0, c1) in enumerate(cols):
                        pb = PB[(fc % 2) * 2 + wi] if ci == 0 else PB[4 + wi]
                        pbs[(wi, ci)] = pb
                        for kc in range(8):
                            mm(pb[:, 0:c1 - c0], Wm[:, kc, fc * 128:(fc + 1) * 128], XST[:, kc, c0:c1], kc == 0, kc == 7,
                               [Wm.r, XST.r], [pb.r])
                sb = s1[fc % 2]
                for ci, (c0, c1) in enumerate(cols):
                    I('act', 'activation', R=[pbs[(0, ci)].r], W=[sb.r], out=sb[:, c0:c1], in_=pbs[(0, ci)][:, 0:c1 - c0], func=AF.Silu)
                    I('dve', 'tensor_tensor', R=[sb.r, pbs[(1, ci)].r], W=[HIDT.r], out=HIDT[:, fc, c0:c1], in0=sb[:, c0:c1],
                      in1=pbs[(1, ci)][:, 0:c1 - c0], op=ALU.mult)
            for s in range(nslot):
                n = 128 if s < 4 else 32
                ys = yss[s % 2]
                for ci in range(2):
                    pb = PB[4 + ci]
                    for fc in range(8):
                        mm(pb[0:n, :], HIDT[:, fc, s * 128:s * 128 + n], W2[:, fc, ci * 512:(ci + 1) * 512], fc == 0, fc == 7,
                           [HIDT.r, W2.r], [pb.r])
                    I('dve', 'scalar_tensor_tensor', R=[pb.r, GT.r, MODB.r], W=[ys.r], out=ys[0:n, ci * 512:(ci + 1) * 512],
                      in0=pb[0:n, :], scalar=GT[0:n, s, e:e + 1], in1=MODB[0:n, 6 + (0 if s < 4 else 1), ci * 512:(ci + 1) * 512],
                      op0=ALU.mult, op1=ALU.mult)
                tgt = out_d if s < 4 else xc_d
                xres = XR[0:NLT] if s < 4 else XR[NLT:NTILE]
                S.dma('pool', lambda E, ys=ys, s=s, e=e, n=n, tgt=tgt: E.indirect_dma_start(
                    out=tgt[:, :], out_offset=bass.IndirectOffsetOnAxis(ap=IXS[0:n, s, e:e + 1], axis=0),
                    in_=ys[0:n, :], in_offset=None, compute_op=ALU.add),
                    reads=[ys.r, IXS.r], writes=xres)
        A.release(m)


    def fstep(tt):
        return L + tt * 128 if tt < NLT else (tt - NLT) * 128

    def bstep(tt):
        return L + (NLT - 1 - tt) * 128 if tt < NLT else (1 - (tt - NLT)) * 128

    def phase_rwkv_prep(j):
        m = A.mark()
        SW = A.alloc([128, 3, RW_IN], F32, 'sw')
        cst = Res('rwconst')
        for k in range(3):
            dma('sp', SW[:, k, :], AP(rw_shift_w, (j * 3 + k) * RW_IN, [[0, 128], [1, RW_IN]]), W=[cst])
        W0B = A.alloc([128, 2, 512], F32, 'w0b')
        A0B = A.alloc([128, 2, 512], F32, 'a0b')
        KKB = A.alloc([128, 512], F32, 'kkb')
        KAB = A.alloc([128, 512], F32, 'kab')
        RKB = A.alloc([128, 512], F32, 'rkb')
        dma('sp', W0B[:, :, :].rearrange('p a b -> p (a b)'), AP(rw_w0, j * 1024, [[0, 128], [1, 1024]]), W=[cst])
        dma('sp', A0B[:, :, :].rearrange('p a b -> p (a b)'), AP(rw_a0, j * 1024, [[0, 128], [1, 1024]]), W=[cst])
        dma('sp', KKB[:, :], AP(rw_k_k, j * 512, [[0, 128], [1, 512]]), W=[cst])
        dma('sp', KAB[:, :], AP(rw_k_a, j * 512, [[0, 128], [1, 512]]), W=[cst])
        dma('sp', RKB[:, :], AP(rw_r_k, j * 512, [[0, 128], [1, 512]]), W=[cst])
        W2 = A.alloc([128, 512], BF16, 'w2')
        A2 = A.alloc([128, 512], BF16, 'a2')
        G2 = A.alloc([128, 512], BF16, 'g2')
        dma('pool', W2[:, :], AP(rw_w2, j * 128 * 512, [[512, 128], [1, 512]]), W=[W2.r])
        dma('pool', A2[:, :], AP(rw_a2, j * 128 * 512, [[512, 128], [1, 512]]), W=[A2.r])
        dma('pool', G2[:, :], AP(rw_g2, j * 128 * 512, [[512, 128], [1, 512]]), W=[G2.r])
        um = A.alloc([128, RW_IN], F32, 'um')
        u0 = A.alloc([128, RW_IN], F32, 'u0')
        up = A.alloc([128, RW_IN], F32, 'up')
        L3 = A.alloc([128, 384], BF16, 'l3')
        L3T = A.alloc([128, 384], BF16, 'l3t')
        OPST = A.alloc([128, 2, 5, 512], F32, 'opst')
        OPF = A.alloc([128, 5, 512], F32, 'opf')
        kk = A.alloc([128, 8, 64], F32, 'kk')
        tA = A.alloc([128, 512], F32, 'tA')
        tB = A.alloc([128, 512], F32, 'tB')
        av = [A.alloc([128, 512], F32, 'av%d' % d) for d in range(2)]
        rr = A.alloc([128, 512], F32, 'rr')
        st_ = A.alloc([128, 32], F32, 'rst')
        gg = A.alloc([128, 512], F32, 'ggt')
        bon = A.alloc([128, 8, 64], F32, 'bon')
        vf = A.alloc([128, 512], F32, 'vf')
        vts = [A.alloc([128, 4, 128], F32, 'vts%d' % d) for d in range(2)]
        for tt in range(NTILE):
            first = tt in (0, NLT)
            lastt = tt in (NLT - 1, NTILE - 1)
            r0 = tt * 128
            dma('sp', u0[:, :], U_d[r0:r0 + 128, 0:RW_IN], R=[UR[tt]], W=[u0.r])
            if first:
                I('pool', 'memset', W=[um.r], ap=um[:, :], constant=0.0)
                dma('sp', um[1:128, :], U_d[r0:r0 + 127, 0:RW_IN], R=[UR[tt]], W=[um.r])
            else:
                dma('sp', um[:, :], U_d[r0 - 1:r0 + 127, 0:RW_IN], R=[UR[tt], UR[tt - 1]], W=[um.r])
            if lastt:
                I('pool', 'memset', W=[up.r], ap=up[:, :], constant=0.0)
                dma('sp', up[0:127, :], U_d[r0 + 1:r0 + 128, 0:RW_IN], R=[UR[tt]], W=[up.r])
            else:
                dma('sp', up[:, :], U_d[r0 + 1:r0 + 129, 0:RW_IN], R=[UR[tt], UR[tt + 1]], W=[up.r])
            I('dve', 'tensor_tensor', R=[u0.r, cst], W=[u0.r], out=u0[:, :], in0=u0[:, :], in1=SW[:, 1, :], op=ALU.mult)
            I('pool', 'tensor_tensor', R=[um.r, cst], W=[um.r], out=um[:, :], in0=um[:, :], in1=SW[:, 0, :], op=ALU.mult)
            I('pool', 'tensor_tensor', R=[up.r, cst], W=[up.r], out=up[:, :], in0=up[:, :], in1=SW[:, 2, :], op=ALU.mult)
            I('dve', 'tensor_tensor', R=[u0.r, um.r], W=[u0.r], out=u0[:, :], in0=u0[:, :], in1=um[:, :], op=ALU.add)
            I('dve', 'tensor_tensor', R=[u0.r, up.r], W=[u0.r], out=u0[:, :], in0=u0[:, :], in1=up[:, :], op=ALU.add)
            r_ap, k_ap, v_ap = u0[:, 0:512], u0[:, 512:1024], u0[:, 1024:1536]
            I('act', 'activation', R=[u0.r], W=[L3.r], out=L3[:, 0:128], in_=u0[:, 1536:1664], func=AF.Tanh)
            I('act', 'activation', R=[u0.r], W=[L3.r], out=L3[:, 256:384], in_=u0[:, 1792:1920], func=AF.Sigmoid)
            I('act', 'copy', R=[u0.r], W=[L3.r], out=L3[:, 128:256], in_=u0[:, 1664:1792])
            transpose8(L3, L3T, PT[tt % 2], ident_b, n=3)
            I('pool', 'tensor_copy', R=[u0.r], W=[OPST.r], out=OPST[:, 0, 0, :], in_=r_ap)
            I('pool', 'tensor_copy', R=[u0.r], W=[OPF.r], out=OPF[:, 0, :], in_=r_ap)
            I('dve', 'tensor_tensor', R=[u0.r, cst], W=[kk.r], out=kk[:, :, :].rearrange('p a b -> p (a b)'), in0=k_ap, in1=KKB[:, :],
              op=ALU.mult)
            I('dve', 'tensor_tensor', R=[kk.r], W=[tA.r], out=tA[:, :].rearrange('p (a b) -> p a b', a=8), in0=kk[:, :, :],
              in1=kk[:, :, :], op=ALU.mult)
            I('dve', 'tensor_reduce', R=[tA.r], W=[st_.r], out=st_[:, 0:8], in_=tA[:, :].rearrange('p (a b) -> p a b', a=8),
              axis=AX.X, op=ALU.add)
            rms_rstd(st_[:, 0:8], 1.0, st_[:, 8:16], [st_.r], [st_.r], ecol=2)
            I('dve', 'tensor_tensor', R=[kk.r, st_.r], W=[kk.r], out=kk[:, :, :], in0=kk[:, :, :],
              in1=st_[:, 8:16].unsqueeze(2).to_broadcast([128, 8, 64]), op=ALU.mult)
            kkf = kk[:, :, :].rearrange('p a b -> p (a b)')
            I('pool', 'tensor_scalar', R=[kk.r], W=[OPST.r], out=OPST[:, 0, 3, :], in0=kkf, scalar1=-1.0, scalar2=None, op0=ALU.mult)
            I('pool', 'tensor_scalar', R=[kk.r], W=[OPF.r], out=OPF[:, 3, :], in0=kkf, scalar1=-1.0, scalar2=None, op0=ALU.mult)
            I('pool', 'tensor_tensor', R=[u0.r, cst], W=[rr.r], out=rr[:, :], in0=r_ap, in1=RKB[:, :], op=ALU.mult)
            for d in range(2):
                dst = OPST[:, 0, :, :] if d == 0 else OPF[:, :, :]
                dres = OPST.r if d == 0 else OPF.r
                pw, pa_ = PB[0 + d], PB[2 + d]
                mm(pw[:, :], L3T[d * 64:(d + 1) * 64, 0:128], W2[d * 64:(d + 1) * 64, :], True, True, [L3T.r, W2.r], [pw.r])
                mm(pa_[:, :], L3T[d * 64:(d + 1) * 64, 128:256], A2[d * 64:(d + 1) * 64, :], True, True, [L3T.r, A2.r], [pa_.r])
                I('dve', 'tensor_tensor', R=[pw.r, cst], W=[tA.r], out=tA[:, :], in0=pw[:, :], in1=W0B[:, d, :], op=ALU.add)
                I('act', 'activation', R=[tA.r], W=[tA.r], out=tA[:, :], in_=tA[:, :], func=AF.Sigmoid)
                I('act', 'activation', R=[tA.r], W=[dres], out=dst[:, 1, :], in_=tA[:, :], func=AF.Exp, scale=-float(np.exp(-0.5)))
                a_t = av[d]
                I('dve', 'tensor_tensor', R=[pa_.r, cst], W=[a_t.r], out=a_t[:, :], in0=pa_[:, :], in1=A0B[:, d, :], op=ALU.add)
                I('act', 'activation', R=[a_t.r], W=[a_t.r], out=a_t[:, :], in_=a_t[:, :], func=AF.Sigmoid)
                I('dve', 'scalar_tensor_tensor', R=[a_t.r, cst], W=[tB.r], out=tB[:, :], in0=a_t[:, :], scalar=-1.0, in1=KAB[:, :],
                  op0=ALU.add, op1=ALU.mult)
                I('dve', 'scalar_tensor_tensor', R=[tB.r, u0.r], W=[dres], out=dst[:, 2, :], in0=tB[:, :], scalar=1.0, in1=k_ap,
                  op0=ALU.add, op1=ALU.mult)
                I('pool', 'tensor_tensor', R=[kk.r, a_t.r], W=[dres], out=dst[:, 4, :], in0=kkf, in1=a_t[:, :], op=ALU.mult)
                I('dve', 'tensor_tensor', R=[rr.r, dres], W=[tB.r], out=tB[:, :], in0=rr[:, :], in1=dst[:, 2, :], op=ALU.mult)
                I('dve', 'tensor_reduce', R=[tB.r], W=[st_.r], out=st_[:, 16 + d * 8:24 + d * 8],
                  in_=tB[:, :].rearrange('p (a b) -> p a b', a=8), axis=AX.X, op=ALU.add)
            I('dve', 'tensor_tensor', R=[st_.r], W=[st_.r], out=st_[:, 16:24], in0=st_[:, 16:24], in1=st_[:, 24:32], op=ALU.add)
            I('dve', 'tensor_tensor', R=[st_.r, u0.r], W=[bon.r], out=bon[:, :, :], in0=v_ap.rearrange('p (a b) -> p a b', a=8),
              in1=st_[:, 16:24].unsqueeze(2).to_broadcast([128, 8, 64]), op=ALU.mult)
            dma('sp', BON_d[r0:r0 + 128, :], bon[:, :, :].rearrange('p a b -> p (a b)'), R=[bon.r], W=[BONR])
            pg = PB[4]
            mm(pg[:, :], L3T[:, 256:384], G2[:, :], True, True, [L3T.r, G2.r], [pg.r])
            I('act', 'copy', R=[pg.r], W=[gg.r], out=gg[:, :], in_=pg[:, :])
            dma('sp', GG_d[r0:r0 + 128, :], gg[:, :], R=[gg.r], W=[GGR])
            for q in range(5):
                pf = PB[q % 2]
                mm(pf[:, :], flip_f[:, :], OPF[:, q, :], True, True, [flip_f.r, OPF.r], [pf.r])
                I('act' if q % 2 else 'dve', 'copy' if q % 2 else 'tensor_copy', R=[pf.r], W=[OPST.r], out=OPST[:, 1, q, :], in_=pf[:, :])
            I('pool', 'tensor_copy', R=[u0.r], W=[vf.r], out=vf[:, :], in_=v_ap)
            for d in range(2):
                pv = PB[2 + d]
                for g in range(4):
                    mm(pv[:, g * 128:(g + 1) * 128], vf[:, g * 128:(g + 1) * 128], (ident_f if d == 0 else flip_f)[:, :], True, True,
                       [vf.r, ident_f.r, flip_f.r], [pv.r])
                I('act', 'copy', R=[pv.r], W=[vts[d].r], out=vts[d][:, :, :].rearrange('p a b -> p (a b)'), in_=pv[:, :])
                s0 = fstep(tt) if d == 0 else bstep(tt)
                dma('sp', AP(VT_d, d * 512 * NT + s0, [[NT, 128], [128 * NT, 4], [1, 128]]), vts[d][:, :, :], R=[vts[d].r], W=[VTR])
                dma('sp', OPS_d[d, s0:s0 + 128, :], OPST[:, d, :, :].rearrange('p a b -> p (a b)'), R=[OPST.r], W=[OPSR])
        A.release(m)

    def phase_rwkv_scan():
        m = A.mark()
        NS = 4
        St = A.alloc([128, 2, 4, 64], F32, 'state')
        tmp = A.alloc([128, 2, 4, 64], F32, 'stmp')
        tk = [A.alloc([128, 2, 4, 64], F32, 'stk%d' % k) for k in range(2)]
        sa = A.alloc([128, 2, 4], F32, 'sa')
        BCT = [A.alloc([128, 2, NS, 20, 64], F32, 'bct%d' % k) for k in range(2)]
        VB = [A.alloc([128, 2, 4, 128], F32, 'vb%d' % k) for k in range(2)]
        YB = [A.alloc([128, 2, 4, 128], F32, 'yb%d' % k) for k in range(2)]
        I('dve', 'memset', W=[St.r], ap=St[:, :, :, :], constant=0.0)
        bc3 = lambda ap: ap.unsqueeze(3).to_broadcast([128, 2, 4, 64])
        for s in range(NT):
            blk = s // 128
            vb, yb = VB[blk % 2], YB[blk % 2]
            if s % 128 == 0:
                for d in range(2):
                    dma('sp', vb[:, d, :, :], AP(VT_d, d * 512 * NT + s, [[NT, 128], [128 * NT, 4], [1, 128]]), R=[VTR], W=[vb.r])
            bct = BCT[(s // NS) % 2]
            if s % NS == 0:
                for d in range(2):
                    for h2 in range(2):
                        dma('sp', bct[h2 * 64:(h2 + 1) * 64, d, :, :, :],
                            AP(OPS_d, (d * NT + s) * 2560 + h2 * 64, [[0, 64], [2560, NS], [128, 20], [1, 64]]), R=[OPSR], W=[bct.r])
            sl = s % NS
            op = lambda q: bct[:, :, sl, q * 4:(q + 1) * 4, :]
            c = s % 128
            t2 = tk[s % 2]
            I('pool', 'tensor_tensor', R=[bct.r, vb.r], W=[t2.r], out=t2[:, :, :, :], in0=op(2), in1=bc3(vb[:, :, :, c]), op=ALU.mult)
            I('dve', 'tensor_tensor', R=[St.r, bct.r], W=[tmp.r], out=tmp[:, :, :, :], in0=St[:, :, :, :], in1=op(3), op=ALU.mult)
            I('dve', 'tensor_reduce', R=[tmp.r], W=[sa.r], out=sa[:, :, :], in_=tmp[:, :, :, :], axis=AX.X, op=ALU.add)
            I('dve', 'tensor_tensor', R=[St.r, bct.r], W=[St.r], out=St[:, :, :, :], in0=St[:, :, :, :], in1=op(1), op=ALU.mult)
            I('dve', 'tensor_tensor', R=[sa.r, bct.r], W=[tmp.r], out=tmp[:, :, :, :], in0=op(4), in1=bc3(sa[:, :, :]), op=ALU.mult)
            I('dve', 'tensor_tensor', R=[St.r, tmp.r], W=[St.r], out=St[:, :, :, :], in0=St[:, :, :, :], in1=tmp[:, :, :, :], op=ALU.add)
            I('dve', 'tensor_tensor', R=[St.r, t2.r], W=[St.r], out=St[:, :, :, :], in0=St[:, :, :, :], in1=t2[:, :, :, :], op=ALU.add)
            I('dve', 'tensor_tensor', R=[St.r, bct.r], W=[tmp.r], out=tmp[:, :, :, :], in0=St[:, :, :, :], in1=op(0), op=ALU.mult)
            I('dve', 'tensor_reduce', R=[tmp.r], W=[yb.r], out=yb[:, :, :, c], in_=tmp[:, :, :, :], axis=AX.X, op=ALU.add)
            if c == 127:
                for d in range(2):
                    dma('sp', AP(YT_d, d * 512 * NT + s - 127, [[NT, 128], [128 * NT, 4], [1, 128]]), yb[:, d, :, :], R=[yb.r], W=[YTR])
        A.release(m)

    def phase_rwkv_post(j):
        m = A.mark()
        GNG = A.alloc([128, 512], F32, 'gng')
        GNB = A.alloc([128, 512], F32, 'gnb')
        cst = Res('gnconst')
        dma('sp', GNG[:, :], AP(rw_gn_g, j * 512, [[0, 128], [1, 512]]), W=[cst])
        dma('sp', GNB[:, :], AP(rw_gn_b, j * 512, [[0, 128], [1, 512]]), W=[cst])
        yfs = [A.alloc([128, 4, 128], F32, 'yf%d' % k) for k in range(2)]
        ybs = [A.alloc([128, 4, 128], F32, 'yb_%d' % k) for k in range(2)]
        zb = A.alloc([128, 512], F32, 'zb')
        bons = [A.alloc([128, 512], F32, 'bon%d' % k) for k in range(2)]
        ggs = [A.alloc([128, 512], F32, 'gg%d' % k) for k in range(2)]
        y = A.alloc([128, 8, 64], F32, 'ysum')
        sq = A.alloc([128, 8, 64], F32, 'ysq')
        st_ = A.alloc([128, 32], F32, 'gst')
        ocs = [A.alloc([128, 512], BF16, 'orw%d' % k) for k in range(2)]
        for tt in range(NTILE):
            yf, yb, bo, gt, oc = yfs[tt % 2], ybs[tt % 2], bons[tt % 2], ggs[tt % 2], ocs[tt % 2]
            r0 = tt * 128
            dma('sp', yf[:, :, :], AP(YT_d, fstep(tt), [[NT, 128], [128 * NT, 4], [1, 128]]), R=[YTR], W=[yf.r])
            dma('sp', yb[:, :, :], AP(YT_d, 512 * NT + bstep(tt), [[NT, 128], [128 * NT, 4], [1, 128]]), R=[YTR], W=[yb.r])
            dma('sp', bo[:, :], BON_d[r0:r0 + 128, :], R=[BONR], W=[bo.r])
            dma('sp', gt[:, :], GG_d[r0:r0 + 128, :], R=[GGR], W=[gt.r])
            pz, py = PB[tt % 2], PB[2 + tt % 2]
            for g in range(4):
                mm(pz[:, g * 128:(g + 1) * 128], yb[:, g, :], ident_f[:, :], True, True, [yb.r, ident_f.r], [pz.r])
            I('act', 'copy', R=[pz.r], W=[zb.r], out=zb[:, :], in_=pz[:, :])
            mm(py[:, :], flip_f[:, :], zb[:, :], True, False, [flip_f.r, zb.r], [py.r])
            for g in range(4):
                mm(py[:, g * 128:(g + 1) * 128], yf[:, g, :], ident_f[:, :], False, g == 3, [yf.r, ident_f.r], [py.r])
            yfl = y[:, :, :].rearrange('p a b -> p (a b)')
            I('dve', 'tensor_tensor', R=[py.r, bo.r], W=[y.r], out=yfl, in0=py[:, :], in1=bo[:, :], op=ALU.add)
            I('dve', 'tensor_reduce', R=[y.r], W=[st_.r], out=st_[:, 0:8], in_=y[:, :, :], axis=AX.X, op=ALU.add)
            I('dve', 'tensor_scalar', R=[st_.r], W=[st_.r], out=st_[:, 8:16], in0=st_[:, 0:8], scalar1=-1.0 / 64, scalar2=None, op0=ALU.mult)
            I('dve', 'tensor_tensor', R=[y.r, st_.r], W=[y.r], out=y[:, :, :], in0=y[:, :, :],
              in1=st_[:, 8:16].unsqueeze(2).to_broadcast([128, 8, 64]), op=ALU.add)
            I('pool', 'tensor_tensor', R=[y.r], W=[sq.r], out=sq[:, :, :], in0=y[:, :, :], in1=y[:, :, :], op=ALU.mult)
            I('dve', 'tensor_reduce', R=[sq.r], W=[st_.r], out=st_[:, 16:24], in_=sq[:, :, :], axis=AX.X, op=ALU.add)
            rms_rstd(st_[:, 16:24], 64, st_[:, 24:32], [st_.r], [st_.r], ecol=1)
            I('dve', 'tensor_tensor', R=[y.r, st_.r], W=[y.r], out=y[:, :, :], in0=y[:, :, :],
              in1=st_[:, 24:32].unsqueeze(2).to_broadcast([128, 8, 64]), op=ALU.mult)
            I('dve', 'tensor_tensor', R=[y.r, cst], W=[y.r], out=yfl, in0=yfl, in1=GNG[:, :], op=ALU.mult)
            I('pool', 'tensor_tensor', R=[y.r, cst], W=[y.r], out=yfl, in0=yfl, in1=GNB[:, :], op=ALU.add)
            I('dve', 'tensor_tensor', R=[y.r, gt.r], W=[oc.r], out=oc[:, :], in0=yfl, in1=gt[:, :], op=ALU.mult)
            dma('sp', CAT_d[r0:r0 + 128, 0:512], oc[:, :], R=[oc.r], W=[CATR[tt]])
        A.release(m)


    def phase_rwkv_prep2(j):
        m = A.mark()
        SW = A.alloc([128, 3, RW_IN], F32, 'sw')
        cst = Res('rwconst')
        for k in range(3):
            dma('sp', SW[:, k, :], AP(rw_shift_w, (j * 3 + k) * RW_IN, [[0, 128], [1, RW_IN]]), W=[cst])
        W0B = A.alloc([128, 2, 512], F32, 'w0b')
        A0B = A.alloc([128, 2, 512], F32, 'a0b')
        KKB = A.alloc([128, 512], F32, 'kkb')
        KAB = A.alloc([128, 512], F32, 'kab')
        RKB = A.alloc([128, 512], F32, 'rkb')
        TRI = A.alloc([128, 128], F32, 'tri')
        BLK = A.alloc([128, 128], F32, 'blk')
        dma('sp', W0B[:, :, :].rearrange('p a b -> p (a b)'), AP(rw_w0, j * 1024, [[0, 128], [1, 1024]]), W=[cst])
        dma('sp', A0B[:, :, :].rearrange('p a b -> p (a b)'), AP(rw_a0, j * 1024, [[0, 128], [1, 1024]]), W=[cst])
        dma('sp', KKB[:, :], AP(rw_k_k, j * 512, [[0, 128], [1, 512]]), W=[cst])
        dma('sp', KAB[:, :], AP(rw_k_a, j * 512, [[0, 128], [1, 512]]), W=[cst])
        dma('sp', RKB[:, :], AP(rw_r_k, j * 512, [[0, 128], [1, 512]]), W=[cst])
        dma('sp', TRI[:, :], tri_d[:, :], W=[cst])
        dma('sp', BLK[:, :], blk_d[:, :], W=[cst])
        W2 = A.alloc([128, 512], BF16, 'w2')
        A2 = A.alloc([128, 512], BF16, 'a2')
        G2 = A.alloc([128, 512], BF16, 'g2')
        dma('pool', W2[:, :], AP(rw_w2, j * 128 * 512, [[512, 128], [1, 512]]), W=[W2.r])
        dma('pool', A2[:, :], AP(rw_a2, j * 128 * 512, [[512, 128], [1, 512]]), W=[A2.r])
        dma('pool', G2[:, :], AP(rw_g2, j * 128 * 512, [[512, 128], [1, 512]]), W=[G2.r])
        um = A.alloc([128, RW_IN], F32, 'um')
        u0 = A.alloc([128, RW_IN], F32, 'u0')
        up = A.alloc([128, RW_IN], F32, 'up')
        L3 = A.alloc([128, 384], BF16, 'l3')
        L3T = A.alloc([128, 384], BF16, 'l3t')
        OPST = A.alloc([128, 2, 6, 512], F32, 'opst')
        OPF = A.alloc([128, 6, 512], F32, 'opf')
        kk = A.alloc([128, 8, 64], F32, 'kk')
        tA = A.alloc([128, 512], F32, 'tA')
        tB = A.alloc([128, 512], F32, 'tB')
        av = [A.alloc([128, 512], F32, 'av%d' % d) for d in range(2)]
        rr = A.alloc([128, 512], F32, 'rr')
        st_ = A.alloc([128, 32], F32, 'rst')
        gg = A.alloc([128, 512], F32, 'ggt')
        bon = A.alloc([128, 8, 64], F32, 'bon')
        CUM = A.alloc([128, 512], F32, 'cum')
        EE = [A.alloc([128, 512], F32, 'ee%d' % k) for k in range(4)]
        PLBt = A.alloc([128, 512], F32, 'plbt')
        TMb = [A.alloc([128, 512], BF16, 'tmb%d' % k) for k in range(7)]
        FTs = [A.alloc([64, 8, 128], BF16, 'fts%d' % k) for k in range(2)]
        fti = 0
        for tt in range(NTILE):
            first = tt in (0, NLT)
            lastt = tt in (NLT - 1, NTILE - 1)
            r0 = tt * 128
            dma('sp', u0[:, :], U_d[r0:r0 + 128, 0:RW_IN], R=[UR[tt]], W=[u0.r])
            if first:
                I('pool', 'memset', W=[um.r], ap=um[:, :], constant=0.0)
                dma('sp', um[1:128, :], U_d[r0:r0 + 127, 0:RW_IN], R=[UR[tt]], W=[um.r])
            else:
                dma('sp', um[:, :], U_d[r0 - 1:r0 + 127, 0:RW_IN], R=[UR[tt], UR[tt - 1]], W=[um.r])
            if lastt:
                I('pool', 'memset', W=[up.r], ap=up[:, :], constant=0.0)
                dma('sp', up[0:127, :], U_d[r0 + 1:r0 + 128, 0:RW_IN], R=[UR[tt]], W=[up.r])
            else:
                dma('sp', up[:, :], U_d[r0 + 1:r0 + 129, 0:RW_IN], R=[UR[tt], UR[tt + 1]], W=[up.r])
            I('dve', 'tensor_tensor', R=[u0.r, cst], W=[u0.r], out=u0[:, :], in0=u0[:, :], in1=SW[:, 1, :], op=ALU.mult)
            I('pool', 'tensor_tensor', R=[um.r, cst], W=[um.r], out=um[:, :], in0=um[:, :], in1=SW[:, 0, :], op=ALU.mult)
            I('pool', 'tensor_tensor', R=[up.r, cst], W=[up.r], out=up[:, :], in0=up[:, :], in1=SW[:, 2, :], op=ALU.mult)
            I('dve', 'tensor_tensor', R=[u0.r, um.r], W=[u0.r], out=u0[:, :], in0=u0[:, :], in1=um[:, :], op=ALU.add)
            I('dve', 'tensor_tensor', R=[u0.r, up.r], W=[u0.r], out=u0[:, :], in0=u0[:, :], in1=up[:, :], op=ALU.add)
            r_ap, k_ap, v_ap = u0[:, 0:512], u0[:, 512:1024], u0[:, 1024:1536]
            I('act', 'activation', R=[u0.r], W=[L3.r], out=L3[:, 0:128], in_=u0[:, 1536:1664], func=AF.Tanh)
            I('act', 'activation', R=[u0.r], W=[L3.r], out=L3[:, 256:384], in_=u0[:, 1792:1920], func=AF.Sigmoid)
            I('act', 'copy', R=[u0.r], W=[L3.r], out=L3[:, 128:256], in_=u0[:, 1664:1792])
            transpose8(L3, L3T, PT[tt % 2], ident_b, n=3)
            I('act', 'copy', R=[u0.r], W=[OPST.r], out=OPST[:, 0, 0, :], in_=r_ap)
            I('act', 'copy', R=[u0.r], W=[OPF.r], out=OPF[:, 0, :], in_=r_ap)
            I('act', 'copy', R=[u0.r], W=[OPST.r], out=OPST[:, 0, 5, :], in_=v_ap)
            I('dve', 'tensor_copy', R=[u0.r], W=[OPF.r], out=OPF[:, 5, :], in_=v_ap)
            I('dve', 'tensor_tensor', R=[u0.r, cst], W=[kk.r], out=kk[:, :, :].rearrange('p a b -> p (a b)'), in0=k_ap, in1=KKB[:, :],
              op=ALU.mult)
            I('dve', 'tensor_tensor', R=[kk.r], W=[tA.r], out=tA[:, :].rearrange('p (a b) -> p a b', a=8), in0=kk[:, :, :],
              in1=kk[:, :, :], op=ALU.mult)
            I('dve', 'tensor_reduce', R=[tA.r], W=[st_.r], out=st_[:, 0:8], in_=tA[:, :].rearrange('p (a b) -> p a b', a=8),
              axis=AX.X, op=ALU.add)
            rms_rstd(st_[:, 0:8], 1.0, st_[:, 8:16], [st_.r], [st_.r], ecol=2)
            I('dve', 'tensor_tensor', R=[kk.r, st_.r], W=[kk.r], out=kk[:, :, :], in0=kk[:, :, :],
              in1=st_[:, 8:16].unsqueeze(2).to_broadcast([128, 8, 64]), op=ALU.mult)
            kkf = kk[:, :, :].rearrange('p a b -> p (a b)')
            I('act', 'mul', R=[kk.r], W=[OPST.r], out=OPST[:, 0, 3, :], in_=kkf, mul=-1.0)
            I('act', 'mul', R=[kk.r], W=[OPF.r], out=OPF[:, 3, :], in_=kkf, mul=-1.0)
            I('pool', 'tensor_tensor', R=[u0.r, cst], W=[rr.r], out=rr[:, :], in0=r_ap, in1=RKB[:, :], op=ALU.mult)
            for d in range(2):
                dst = OPST[:, 0, :, :] if d == 0 else OPF[:, :, :]
                dres = OPST.r if d == 0 else OPF.r
                pw, pa_ = PB[0 + d], PB[2 + d]
                mm(pw[:, :], L3T[d * 64:(d + 1) * 64, 0:128], W2[d * 64:(d + 1) * 64, :], True, True, [L3T.r, W2.r], [pw.r])
                mm(pa_[:, :], L3T[d * 64:(d + 1) * 64, 128:256], A2[d * 64:(d + 1) * 64, :], True, True, [L3T.r, A2.r], [pa_.r])
                I('dve', 'tensor_tensor', R=[pw.r, cst], W=[tA.r], out=tA[:, :], in0=pw[:, :], in1=W0B[:, d, :], op=ALU.add)
                I('act', 'activation', R=[tA.r], W=[tA.r], out=tA[:, :], in_=tA[:, :], func=AF.Sigmoid)
                I('act', 'mul', R=[tA.r], W=[dres], out=dst[:, 1, :], in_=tA[:, :], mul=-float(np.exp(-0.5)))
                a_t = av[d]
                I('dve', 'tensor_tensor', R=[pa_.r, cst], W=[a_t.r], out=a_t[:, :], in0=pa_[:, :], in1=A0B[:, d, :], op=ALU.add)
                I('act', 'activation', R=[a_t.r], W=[a_t.r], out=a_t[:, :], in_=a_t[:, :], func=AF.Sigmoid)
                I('dve', 'scalar_tensor_tensor', R=[a_t.r, cst], W=[tB.r], out=tB[:, :], in0=a_t[:, :], scalar=-1.0, in1=KAB[:, :],
                  op0=ALU.add, op1=ALU.mult)
                I('dve', 'scalar_tensor_tensor', R=[tB.r, u0.r], W=[dres], out=dst[:, 2, :], in0=tB[:, :], scalar=1.0, in1=k_ap,
                  op0=ALU.add, op1=ALU.mult)
                I('pool', 'tensor_tensor', R=[kk.r, a_t.r], W=[dres], out=dst[:, 4, :], in0=kkf, in1=a_t[:, :], op=ALU.mult)
                I('dve', 'tensor_tensor', R=[rr.r, dres], W=[tB.r], out=tB[:, :], in0=rr[:, :], in1=dst[:, 2, :], op=ALU.mult)
                I('dve', 'tensor_reduce', R=[tB.r], W=[st_.r], out=st_[:, 16 + d * 8:24 + d * 8],
                  in_=tB[:, :].rearrange('p (a b) -> p a b', a=8), axis=AX.X, op=ALU.add)
            I('dve', 'tensor_tensor', R=[st_.r], W=[st_.r], out=st_[:, 16:24], in0=st_[:, 16:24], in1=st_[:, 24:32], op=ALU.add)
            I('dve', 'tensor_tensor', R=[st_.r, u0.r], W=[bon.r], out=bon[:, :, :], in0=v_ap.rearrange('p (a b) -> p a b', a=8),
              in1=st_[:, 16:24].unsqueeze(2).to_broadcast([128, 8, 64]), op=ALU.mult)
            dma('sp', BON_d[r0:r0 + 128, :], bon[:, :, :].rearrange('p a b -> p (a b)'), R=[bon.r], W=[BONR])
            pg = PB[4]
            mm(pg[:, :], L3T[:, 256:384], G2[:, :], True, True, [L3T.r, G2.r], [pg.r])
            I('act', 'copy', R=[pg.r], W=[gg.r], out=gg[:, :], in_=pg[:, :])
            dma('sp', GG_d[r0:r0 + 128, :], gg[:, :], R=[gg.r], W=[GGR])
            for q in range(6):
                pf = PB[q % 2]
                mm(pf[:, :], flip_f[:, :], OPF[:, q, :], True, True, [flip_f.r, OPF.r], [pf.r])
                I('act' if q % 2 else 'dve', 'copy' if q % 2 else 'tensor_copy', R=[pf.r], W=[OPST.r], out=OPST[:, 1, q, :], in_=pf[:, :])
            for d in range(2):
                s0 = fstep(tt) if d == 0 else bstep(tt)
                X = lambda q: OPST[:, d, q, :]
                pc, pl = PB[2 + d], PB[4 + d]
                mm(pc[:, :], TRI[:, :], X(1), True, True, [cst, OPST.r], [pc.r])
                mm(pl[:, :], BLK[:, :], X(1), True, True, [cst, OPST.r], [pl.r])
                I('act', 'copy', R=[pc.r], W=[CUM.r], out=CUM[:, :], in_=pc[:, :])
                I('act', 'activation', R=[CUM.r], W=[EE[0].r], out=EE[0][:, :], in_=CUM[:, :], func=AF.Exp)
                I('act', 'activation', R=[CUM.r], W=[EE[1].r], out=EE[1][:, :], in_=CUM[:, :], func=AF.Exp, scale=-1.0)
                I('pool', 'tensor_tensor', R=[CUM.r, OPST.r], W=[EE[2].r], out=EE[2][:, :], in0=CUM[:, :], in1=X(1), op=ALU.subtract)
                I('act', 'activation', R=[EE[2].r], W=[EE[2].r], out=EE[2][:, :], in_=EE[2][:, :], func=AF.Exp)
                I('dve', 'tensor_tensor', R=[pl.r, CUM.r], W=[EE[3].r], out=EE[3][:, :], in0=pl[:, :], in1=CUM[:, :], op=ALU.subtract)
                I('act', 'activation', R=[EE[3].r], W=[EE[3].r], out=EE[3][:, :], in_=EE[3][:, :], func=AF.Exp)
                I('act', 'activation', R=[pl.r], W=[PLBt.r], out=PLBt[:, :], in_=pl[:, :], func=AF.Exp)
                prods = ((0, 3, 2, 'dve'), (1, 0, 0, 'pool'), (2, 4, 1, 'dve'), (3, 2, 1, 'pool'), (4, 4, 3, 'dve'), (5, 2, 3, 'pool'))
                for (ti, q, e, eng) in prods:
                    I(eng, 'tensor_tensor', R=[OPST.r, EE[e].r], W=[TMb[ti].r], out=TMb[ti][:, :], in0=X(q), in1=EE[e][:, :], op=ALU.mult)
                I('act', 'copy', R=[OPST.r], W=[TMb[6].r], out=TMb[6][:, :], in_=X(5))
                for oi, ti in enumerate((0, 4, 5, 6)):
                    dma('sp', TM_d[d, oi, s0:s0 + 128, :], TMb[ti][:, :], R=[TMb[ti].r], W=[TMR])
                dma('sp', PLB_d[d, s0:s0 + 128, :], PLBt[:, :], R=[PLBt.r], W=[PLBR])
                for oi, ti in enumerate((0, 2, 3, 1)):
                    ft = FTs[fti % 2]
                    pt = PT[fti % 2]
                    fti += 1
                    for h in range(8):
                        tr(pt[0:64, h * 128:(h + 1) * 128], TMb[ti][:, h * 64:(h + 1) * 64], ident_b[:, :], [TMb[ti].r, ident_b.r], [pt.r])
                    I('act' if oi % 2 else 'dve', 'copy' if oi % 2 else 'tensor_copy', R=[pt.r], W=[ft.r],
                      out=ft[:, :, :].rearrange('p a b -> p (a b)'), in_=pt[0:64, :])
                    dma('sp', AP(FT_d, ((d * 4 + oi) * 64) * 8 * NT + s0, [[8 * NT, 64], [NT, 8], [1, 128]]), ft[:, :, :], R=[ft.r], W=[FTR])
        A.release(m)

    def phase_rwkv_chunk():
        m = A.mark()
        MSK = A.alloc([128, 6, 128], F32, 'msk')
        dma('sp', MSK[:, :, :], msk_d[:, :, :], W=[MSK.r])
        H = A.alloc([64, 2, 8, 64], F32, 'hstate')
        I('dve', 'memset', W=[H.r], ap=H[:, :, :, :], constant=0.0)
        HR = [Res('h0'), Res('h1')]
        NL = 7
        bufs = {}
        for d in range(2):
            bufs[d] = dict(
                tm=[A.alloc([128, 4, 8, 64], BF16, 'ctm%d_%d' % (d, k)) for k in range(2)],
                pl=[A.alloc([64, 512], F32, 'cpl%d_%d' % (d, k)) for k in range(2)],
                ft=[A.alloc([64, 4, 8, 128], BF16, 'cft%d_%d' % (d, k)) for k in range(2)],
                Z=[A.alloc([128, 8, 128], BF16, 'cz%d_%d' % (d, k)) for k in range(2)],
                N=[A.alloc([128, 8, 128], BF16, 'cn%d_%d' % (d, k)) for k in range(2)],
                P=[A.alloc([128, 8, 128], BF16, 'cp%d_%d' % (d, k)) for k in range(2)],
                PT=[A.alloc([128, 8, 128], BF16, 'cpt%d_%d' % (d, k)) for k in range(2)],
                Zp=A.alloc([128, 8, 128], BF16, 'czp%d' % d),
                MT=A.alloc([128, 8, 128], BF16, 'cmt%d' % d),
                QbT=A.alloc([128, 8, 128], BF16, 'cqb%d' % d),
                QkT=A.alloc([128, 8, 128], BF16, 'cqk%d' % d),
                Xf=A.alloc([128, 8, 128], F32, 'cxf%d' % d),
                Xb=A.alloc([128, 8, 128], BF16, 'cxb%d' % d),
                FT=A.alloc([64, 8, 64], F32, 'cF%d' % d),
                CP=A.alloc([64, 8, 64], F32, 'cC%d' % d),
                RfT=A.alloc([64, 8, 128], F32, 'cR%d' % d),
                Y0=A.alloc([128, 8, 64], F32, 'cy0%d' % d),
                Yo=[A.alloc([128, 8, 64], F32, 'cyo%d_%d' % (d, k)) for k in range(2)],
                tmpd=A.alloc([64, 8, 64], F32, 'ctd%d' % d),
            )
        idb = ident_f[0:64, 0:64].unsqueeze(1).to_broadcast([64, 8, 64])

        def stages(d, u):
            B = bufs[d]
            tm, plk, ft = B['tm'][u % 2], B['pl'][u % 2], B['ft'][u % 2]
            s0 = u * 128
            PXa, PXb, PGa, PGb = PB[4 * d], PB[4 * d + 1], PB[4 * d + 2], PB[4 * d + 3]
            PXr, PGr = [PXa.r, PXb.r], [PGa.r, PGb.r]
            PX = PBALL[:, (4 * d) * 512:(4 * d + 2) * 512].rearrange('p (a b) -> p a b', a=8)
            PG = PBALL[:, (4 * d + 2) * 512:(4 * d + 4) * 512].rearrange('p (a b) -> p a b', a=8)
            PGs = PBALL[0:64, (4 * d + 2) * 512:(4 * d + 3) * 512].rearrange('p (a b) -> p a b', a=8)
            PGr1 = [PGa.r]
            AtK, BpK, KpK, VmK = (tm[:, q, :, :] for q in range(4))
            AtT, BtT, KtT, RtT = (ft[:, q, :, :] for q in range(4))
            Z, N_ = B['Z'], B['N']
            MT, QbT, QkT, Xf, Xb = B['MT'], B['QbT'], B['QkT'], B['Xf'], B['Xb']
            FT, CP, RfT, Y0, tmpd = B['FT'], B['CP'], B['RfT'], B['Y0'], B['tmpd']
            Yo = B['Yo'][u % 2]
            out = []

            def load():
                dma('sp', tm[:, :, :, :].rearrange('p q a b -> p q (a b)'),
                    AP(TM_d, d * 4 * NT * 512 + s0 * 512, [[512, 128], [NT * 512, 4], [1, 512]]), R=[TMR], W=[tm.r])
                dma('sp', plk[:, :], PLB_d[d, s0:s0 + 64, :], R=[PLBR], W=[plk.r])
                for q in range(4):
                    dma('sp', ft[:, q, :, :], AP(FT_d, ((d * 4 + q) * 64) * 8 * NT + s0, [[8 * NT, 64], [NT, 8], [1, 128]]),
                        R=[FTR], W=[ft.r])
            out.append(load)

            def gram(lhs, rhs, mi, dst, eng):
                def f():
                    for h in range(8):
                        mm(PG[:, h, :], lhs[:, h, :], rhs[:, h, :], True, True, [ft.r], [PGa.r if h < 4 else PGb.r])
                    I(eng, 'tensor_tensor', R=PGr + [MSK.r], W=[dst.r], out=dst[:, :, :], in0=PG,
                      in1=MSK[:, mi, :].unsqueeze(1).to_broadcast([128, 8, 128]), op=ALU.mult)
                return f
            P_, PTt, Zp = B['P'], B['PT'], B['Zp']

            def gram2(lhs, rhs, m1, d1, m2, d2):
                def f():
                    for h in range(8):
                        mm(PG[:, h, :], lhs[:, h, :], rhs[:, h, :], True, True, [ft.r], [PGa.r if h < 4 else PGb.r])
                    I('dve', 'tensor_tensor', R=PGr + [MSK.r], W=[d1.r], out=d1[:, :, :], in0=PG,
                      in1=MSK[:, m1, :].unsqueeze(1).to_broadcast([128, 8, 128]), op=ALU.mult)
                    I('dve', 'tensor_tensor', R=PGr + [MSK.r], W=[d2.r], out=d2[:, :, :], in0=PG,
                      in1=MSK[:, m2, :].unsqueeze(1).to_broadcast([128, 8, 128]), op=ALU.mult)
                return f
            out.append(gram2(BtT, AtT, 0, Z[0], 3, PTt[0]))
            out.append(gram2(AtT, BtT, 1, N_[0], 4, P_[0]))
            out.append(gram(KtT, AtT, 5, MT, 'dve'))
            out.append(gram(BtT, RtT, 2, QbT, 'dve'))
            out.append(gram(KtT, RtT, 2, QkT, 'dve'))

            def mv():
                for h in range(8):
                    mm(PX[:, h, 0:64], MT[:, h, :], VmK[:, h, :], True, True, [MT.r, tm.r], [PXa.r if h < 4 else PXb.r])
                I('dve', 'tensor_copy', R=PXr, W=[Xf.r], out=Xf[:, :, 64:128], in_=PX[:, :, 0:64])
                I('act', 'copy', R=PXr, W=[Xb.r], out=Xb[:, :, 64:128], in_=PX[:, :, 0:64])
                I('pool', 'tensor_copy', R=[tm.r], W=[Xf.r], out=Xf[:, :, 0:64], in_=AtK)
                I('pool', 'tensor_copy', R=[tm.r], W=[Xb.r], out=Xb[:, :, 0:64], in_=AtK)
            out.append(mv)
            idb128 = ident_f[:, :].unsqueeze(1).to_broadcast([128, 8, 128])

            def xapply(L_):
                def f():
                    for h in range(8):
                        mm(PX[:, h, :], L_[:, h, :], Xb[:, h, :], True, True, [L_.r, Xb.r], [PXa.r if h < 4 else PXb.r])
                    I('dve', 'tensor_tensor', R=PXr + [Xf.r], W=[Xf.r], out=Xf[:, :, :], in0=PX, in1=Xf[:, :, :], op=ALU.add)
                    I('act', 'copy', R=[Xf.r], W=[Xb.r], out=Xb[:, :, :], in_=Xf[:, :, :])
                return f

            def mmset(ps, psr, lhs, rhs, dst, eng):
                def f():
                    for h in range(8):
                        mm(ps[:, h, :], lhs[:, h, :], rhs[:, h, :], True, True, [lhs.r, rhs.r], [psr[0] if h < 4 else psr[1]])
                    I(eng, 'copy' if eng == 'act' else 'tensor_copy', R=psr, W=[dst.r], out=dst[:, :, :], in_=ps)
                return f

            def mkzp(Zc):
                def f():
                    I('pool', 'tensor_tensor', R=[Zc.r, ident_f.r], W=[Zp.r], out=Zp[:, :, :], in0=Zc[:, :, :], in1=idb128, op=ALU.add)
                return f
            ND_LEV, NP_LEV = 4, 3
            for i in range(ND_LEV):
                Zc, Nc, Pc, PTc = Z[i % 2], N_[i % 2], P_[i % 2], PTt[i % 2]
                Zn, Nn, Pn, PTn = Z[(i + 1) % 2], N_[(i + 1) % 2], P_[(i + 1) % 2], PTt[(i + 1) % 2]
                out.append(mkzp(Zc))
                out.append(xapply(Zc))
                out.append(mmset(PG, PGr, Zp, Pc, Pn, 'act'))
                out.append(mmset(PX, PXr, Pc, Zp, PTn, 'dve'))
                if i < ND_LEV - 1:
                    out.append(mmset(PG, PGr, Nc, Zc, Zn, 'act'))
                    out.append(mmset(PX, PXr, Zc, Nc, Nn, 'dve'))
            for jl in range(NP_LEV):
                k0 = ND_LEV + jl
                Pc, PTc = P_[k0 % 2], PTt[k0 % 2]
                Pn, PTn = P_[(k0 + 1) % 2], PTt[(k0 + 1) % 2]
                out.append(xapply(PTc))
                if jl < NP_LEV - 1:
                    out.append(mmset(PG, PGr, Pc, PTc, PTn, 'act'))
                    out.append(mmset(PX, PXr, PTc, Pc, Pn, 'dve'))

            def trans():
                for h in range(8):
                    mm(PGs[:, h, :], Xb[:, h, 0:64], BpK[:, h, :], True, True, [Xb.r, tm.r], PGr1)
                I('pool', 'tensor_tensor', R=[plk.r, ident_f.r], W=[tmpd.r], out=tmpd[:, :, :],
                  in0=plk[:, :].rearrange('p (a b) -> p a b', a=8), in1=idb, op=ALU.mult)
                I('dve', 'tensor_tensor', R=PGr1 + [tmpd.r], W=[FT.r], out=FT[:, :, :], in0=PGs, in1=tmpd[:, :, :], op=ALU.add)
            out.append(trans)

            def cprime():
                for h in range(8):
                    mm(PGs[:, h, :], BpK[:, h, :], Xb[:, h, 64:128], True, False, [Xb.r, tm.r], PGr1)
                    mm(PGs[:, h, :], KpK[:, h, :], VmK[:, h, :], False, True, [tm.r], PGr1)
                I('act', 'copy', R=PGr1, W=[CP.r], out=CP[:, :, :], in_=PGs)
            out.append(cprime)

            def w1t():
                PGw = PBALL[0:64, (4 * d + 2) * 512:(4 * d + 4) * 512].rearrange('p (a b) -> p a b', a=8)
                for h in range(8):
                    mm(PGw[:, h, :], Xb[:, h, 0:64], QbT[:, h, :], True, True, [Xb.r, QbT.r], [PGa.r if h < 4 else PGb.r])
                I('dve', 'tensor_tensor', R=PGr + [ft.r], W=[RfT.r], out=RfT[:, :, :], in0=PGw, in1=RtT, op=ALU.add)
            out.append(w1t)

            def y0():
                PGy = PBALL[:, (4 * d + 2) * 512:(4 * d + 3) * 512].rearrange('p (a b) -> p a b', a=8)
                for h in range(8):
                    mm(PGy[:, h, :], QbT[:, h, :], Xb[:, h, 64:128], True, False, [Xb.r, QbT.r], PGr1)
                    mm(PGy[:, h, :], QkT[:, h, :], VmK[:, h, :], False, True, [QkT.r, tm.r], PGr1)
                I('act', 'copy', R=PGr1, W=[Y0.r], out=Y0[:, :, :], in_=PGy)
            out.append(y0)

            def final():
                PGy = PBALL[:, (4 * d + 2) * 512:(4 * d + 3) * 512].rearrange('p (a b) -> p a b', a=8)
                for h in range(8):
                    mm(PGy[:, h, :], RfT[:, h, :], H[:, d, h, :], True, True, [RfT.r, HR[d]], PGr1)
                I('dve', 'tensor_tensor', R=PGr1 + [Y0.r], W=[Yo.r], out=Yo[:, :, :], in0=PGy, in1=Y0[:, :, :], op=ALU.add)
                dma('sp', YS_d[d, s0:s0 + 128, :], Yo[:, :, :].rearrange('p a b -> p (a b)'), R=[Yo.r], W=[YSR])
            out.append(final)

            def chain():
                PGc = PBALL[0:64, (4 * d + 3) * 512:(4 * d + 4) * 512].rearrange('p (a b) -> p a b', a=8)
                for h in range(8):
                    mm(PGc[:, h, :], FT[:, h, :], H[:, d, h, :], True, True, [FT.r, HR[d]], [PGb.r])
                I('dve', 'tensor_tensor', R=[PGb.r, CP.r], W=[HR[d]], out=H[:, d, :, :], in0=PGc, in1=CP[:, :, :], op=ALU.add)
            out.append(chain)
            return out

        for u in range(NTILE):
            sts = [stages(d, u) for d in range(2)]
            for k in range(len(sts[0])):
                for d in range(2):
                    sts[d][k]()
        A.release(m)

    def phase_rwkv_post2(j):
        m = A.mark()
        GNG = A.alloc([128, 512], F32, 'gng')
        GNB = A.alloc([128, 512], F32, 'gnb')
        cst = Res('gnconst')
        dma('sp', GNG[:, :], AP(rw_gn_g, j * 512, [[0, 128], [1, 512]]), W=[cst])
        dma('sp', GNB[:, :], AP(rw_gn_b, j * 512, [[0, 128], [1, 512]]), W=[cst])
        yfs = [A.alloc([128, 512], F32, 'yf%d' % k) for k in range(2)]
        ybs = [A.alloc([128, 512], F32, 'yb_%d' % k) for k in range(2)]
        bons = [A.alloc([128, 512], F32, 'bon%d' % k) for k in range(2)]
        ggs = [A.alloc([128, 512], F32, 'gg%d' % k) for k in range(2)]
        y = A.alloc([128, 8, 64], F32, 'ysum')
        sq = A.alloc([128, 8, 64], F32, 'ysq')
        st_ = A.alloc([128, 32], F32, 'gst')
        ocs = [A.alloc([128, 512], BF16, 'orw%d' % k) for k in range(2)]
        for tt in range(NTILE):
            yf, yb, bo, gt, oc = yfs[tt % 2], ybs[tt % 2], bons[tt % 2], ggs[tt % 2], ocs[tt % 2]
            r0 = tt * 128
            dma('sp', yf[:, :], YS_d[0, fstep(tt):fstep(tt) + 128, :], R=[YSR], W=[yf.r])
            dma('sp', yb[:, :], YS_d[1, bstep(tt):bstep(tt) + 128, :], R=[YSR], W=[yb.r])
            dma('sp', bo[:, :], BON_d[r0:r0 + 128, :], R=[BONR], W=[bo.r])
            dma('sp', gt[:, :], GG_d[r0:r0 + 128, :], R=[GGR], W=[gt.r])
            py = PB[tt % 2]
            mm(py[:, :], flip_f[:, :], yb[:, :], True, True, [flip_f.r, yb.r], [py.r])
            yfl = y[:, :, :].rearrange('p a b -> p (a b)')
            I('pool', 'tensor_tensor', R=[yf.r, bo.r], W=[yf.r], out=yf[:, :], in0=yf[:, :], in1=bo[:, :], op=ALU.add)
            I('dve', 'tensor_tensor', R=[py.r, yf.r], W=[y.r], out=yfl, in0=py[:, :], in1=yf[:, :], op=ALU.add)
            I('dve', 'tensor_reduce', R=[y.r], W=[st_.r], out=st_[:, 0:8], in_=y[:, :, :], axis=AX.X, op=ALU.add)
            I('dve', 'tensor_scalar', R=[st_.r], W=[st_.r], out=st_[:, 8:16], in0=st_[:, 0:8], scalar1=-1.0 / 64, scalar2=None, op0=ALU.mult)
            I('dve', 'tensor_tensor', R=[y.r, st_.r], W=[y.r], out=y[:, :, :], in0=y[:, :, :],
              in1=st_[:, 8:16].unsqueeze(2).to_broadcast([128, 8, 64]), op=ALU.add)
            I('pool', 'tensor_tensor', R=[y.r], W=[sq.r], out=sq[:, :, :], in0=y[:, :, :], in1=y[:, :, :], op=ALU.mult)
            I('dve', 'tensor_reduce', R=[sq.r], W=[st_.r], out=st_[:, 16:24], in_=sq[:, :, :], axis=AX.X, op=ALU.add)
            rms_rstd(st_[:, 16:24], 64, st_[:, 24:32], [st_.r], [st_.r], ecol=1)
            I('dve', 'tensor_tensor', R=[y.r, st_.r], W=[y.r], out=y[:, :, :], in0=y[:, :, :],
              in1=st_[:, 24:32].unsqueeze(2).to_broadcast([128, 8, 64]), op=ALU.mult)
            I('dve', 'tensor_tensor', R=[y.r, cst], W=[y.r], out=yfl, in0=yfl, in1=GNG[:, :], op=ALU.mult)
            I('pool', 'tensor_tensor', R=[y.r, cst], W=[y.r], out=yfl, in0=yfl, in1=GNB[:, :], op=ALU.add)
            I('dve', 'tensor_tensor', R=[y.r, gt.r], W=[oc.r], out=oc[:, :], in0=yfl, in1=gt[:, :], op=ALU.mult)
            dma('sp', CAT_d[r0:r0 + 128, 0:512], oc[:, :], R=[oc.r], W=[CATR[tt]])
        A.release(m)

    def phase_na_prep(j):
        m = A.mark()
        QGB = A.alloc([128, 64], F32, 'naqg')
        KGB = A.alloc([128, 64], F32, 'nakg')
        cst = Res('naconst')
        dma('sp', QGB[:, :], AP(na_q_g, j * 64, [[0, 128], [1, 64]]), W=[cst])
        dma('sp', KGB[:, :], AP(na_k_g, j * 64, [[0, 128], [1, 64]]), W=[cst])
        uqs = [A.alloc([128, 3, 8, 64], F32, 'nu%d' % k) for k in range(2)]
        sq = A.alloc([128, 8, 64], F32, 'nsq')
        st_ = A.alloc([128, 32], F32, 'nst')
        qkb = [A.alloc([128, 8, 64], BF16, 'nqb%d' % k) for k in range(2)]
        qkT = [A.alloc([64, 8, 128], BF16, 'nqT%d' % k) for k in range(2)]
        vas = [A.alloc([128, 8, 65], BF16, 'nva%d' % k) for k in range(2)]
        for k in range(2):
            I('dve', 'memset', W=[vas[k].r], ap=vas[k][:, :, :], constant=1.0)
        for tt in range(NTILE):
            uq, va = uqs[tt % 2], vas[tt % 2]
            r0 = tt * 128
            dma('sp', uq[:, :, :, :].rearrange('p a b c -> p (a b c)'), U_d[r0:r0 + 128, RW_IN:E_OD], R=[UR[tt]], W=[uq.r])
            for which, (gb, dst_d, dres) in enumerate(((QGB, QT_d, QTR), (KGB, KT_d, KTR))):
                x3 = uq[:, which, :, :]
                qb, qT, pt = qkb[which], qkT[which], PT[which]
                I('dve', 'tensor_tensor', R=[uq.r], W=[sq.r], out=sq[:, :, :], in0=x3, in1=x3, op=ALU.mult)
                I('dve', 'tensor_reduce', R=[sq.r], W=[st_.r], out=st_[:, 0:8], in_=sq[:, :, :], axis=AX.X, op=ALU.add)
                rms_rstd(st_[:, 0:8], 64, st_[:, 8:16], [st_.r], [st_.r])
                I('dve', 'tensor_tensor', R=[uq.r, st_.r], W=[sq.r], out=sq[:, :, :], in0=x3,
                  in1=st_[:, 8:16].unsqueeze(2).to_broadcast([128, 8, 64]), op=ALU.mult)
                I('dve', 'tensor_tensor', R=[sq.r, cst], W=[qb.r], out=qb[:, :, :], in0=sq[:, :, :],
                  in1=gb[:, :].unsqueeze(1).to_broadcast([128, 8, 64]), op=ALU.mult)
                for h in range(8):
                    tr(pt[0:64, h * 128:(h + 1) * 128], qb[:, h, :], ident_b[:, :], [qb.r, ident_b.r], [pt.r])
                I('act', 'copy', R=[pt.r], W=[qT.r], out=qT[:, :, :].rearrange('p a b -> p (a b)'), in_=pt[0:64, :])
                dma('sp', AP(dst_d, r0, [[NT, 64], [96 * NT, 8], [1, 128]]), qT[:, :, :], R=[qT.r], W=[dres])
            I('pool', 'tensor_copy', R=[uq.r], W=[va.r], out=va[:, :, 0:64], in_=uq[:, 2, :, :])
            dma('sp', VA_d[r0:r0 + 128, :], va[:, :, :].rearrange('p a b -> p (a b)'), R=[va.r], W=[VAR])
        A.release(m)

    def phase_natten(j):
        m = A.mark()
        scale = 64 ** -0.5
        kTs = [A.alloc([64, NT], BF16, 'nkT%d' % k) for k in range(2)]
        qTs = [A.alloc([64, NT], BF16, 'nqT_%d' % k) for k in range(2)]
        vhs = [A.alloc([128, NTILE, 65], BF16, 'nvh%d' % k) for k in range(2)]
        v64s = [A.alloc([128, 31, 65], BF16, 'nv64%d' % k) for k in range(2)]
        nbs = [A.alloc([128, 8, 256], F32, 'nab%d' % k) for k in range(2)]
        sbs = [A.alloc([128, 256], F32, 'nsb%d' % k) for k in range(2)]
        pps = [A.alloc([128, 384], BF16, 'npp%d' % k) for k in range(2)]
        oas = [A.alloc([64, 64, 64], BF16, 'noa%d' % k) for k in range(2)]
        oac = [A.alloc([128, 2, 64], BF16, 'noc%d' % k) for k in range(2)]
        rc = A.alloc([128, 4], F32, 'nrc')
        it = 0
        for h in range(8):
            kT, qT, vh, v64, nb, oa, oc = kTs[h % 2], qTs[h % 2], vhs[h % 2], v64s[h % 2], nbs[h % 2], oas[h % 2], oac[h % 2]
            dma('sp', kT[:, :], KT_d[h, 0:64, :], R=[KTR], W=[kT.r])
            dma('sp', qT[:, :], QT_d[h, 0:64, :], R=[QTR], W=[qT.r])
            dma('sp', vh[:, :, :], AP(VA_d, h * 65, [[520, 128], [128 * 520, NTILE], [1, 65]]), R=[VAR], W=[vh.r])
            dma('sp', v64[:, :, :], AP(VA_d, 64 * 520 + h * 65, [[520, 128], [128 * 520, 31], [1, 65]]), R=[VAR], W=[v64.r])
            dma('sp', nb[:, :, :], nab_d[j, h, :, :, :], W=[nb.r])
            def na_pv(r, rs, pp, po):
                for kt in range(4):
                    vt = vh[:, rs // 2 + kt, :] if rs % 2 == 0 else v64[:, (rs - 1) // 2 + kt, :]
                    mm(po[0:64, 0:65], pp[:, kt * 64:(kt + 1) * 64], vt, kt == 0, False, [pp.r, vh.r, v64.r], [po.r])
                for c in range(2):
                    mm(po[0:64, 0:65], pp[:, 256 + c * 64:256 + (c + 1) * 64], vh[:, NLT + c, :], False, c == 1, [pp.r, vh.r], [po.r])
                I('dve', 'reciprocal', R=[po.r], W=[rc.r], out=rc[0:64, (r % 2):(r % 2) + 1], in_=po[0:64, 64:65])
                I('dve', 'tensor_scalar', R=[po.r, rc.r], W=[oa.r], out=oa[:, r, :], in0=po[0:64, 0:64], scalar1=rc[0:64, (r % 2):(r % 2) + 1],
                  scalar2=None, op0=ALU.mult)
            prev = None
            for r in range(64):
                rs = min(max(r - 4, 0), 56)
                delta = r - rs
                kb = rs * 64
                ps, po = PB[it % 2], PB[2 + it % 2]
                sb, pp = sbs[it % 2], pps[it % 2]
                it += 1
                qs = qT[:, r * 64:(r + 1) * 64]
                for kt in range(4):
                    mm(ps[:, kt * 64:(kt + 1) * 64], kT[:, kb + kt * 128:kb + (kt + 1) * 128], qs, True, True, [kT.r, qT.r], [ps.r])
                for c in range(2):
                    mm(ps[:, 256 + c * 64:256 + (c + 1) * 64], kT[:, T + c * 128:T + (c + 1) * 128], qs, True, True, [kT.r, qT.r], [ps.r])
                I('dve', 'scalar_tensor_tensor', R=[ps.r, nb.r], W=[sb.r], out=sb[:, :], in0=ps[:, 0:256], scalar=scale, in1=nb[:, delta, :],
                  op0=ALU.mult, op1=ALU.add)
                I('act', 'activation', R=[sb.r], W=[pp.r], out=pp[:, 0:256], in_=sb[:, :], func=AF.Exp)
                I('act', 'activation', R=[ps.r], W=[pp.r], out=pp[:, 256:384], in_=ps[:, 256:384], func=AF.Exp, scale=scale)
                if prev is not None:
                    na_pv(*prev)
                prev = (r, rs, pp, po)
            na_pv(*prev)
            dma('sp', AP(CAT_d, 512 + h * 64, [[D, 64], [64 * D, 64], [1, 64]]), oa[:, :, :], R=[oa.r], W=CATR[0:NLT])
            pp = pps[it % 2]
            for c in range(2):
                ps = PB[c]
                mm(ps[:, 0:256], kT[:, T + c * 128:T + (c + 1) * 128], qT[:, T:NT], True, True, [kT.r, qT.r], [ps.r])
                ppc = pps[c]
                I('act', 'activation', R=[ps.r], W=[ppc.r], out=ppc[:, 0:256], in_=ps[:, 0:256], func=AF.Exp, scale=scale)
                for q2 in range(2):
                    mm(PB[4 + q2][:, 0:65], ppc[:, q2 * 128:(q2 + 1) * 128], vh[:, NLT + c, :], c == 0, c == 1, [ppc.r, vh.r], [PB[4 + q2].r])
            for q2 in range(2):
                I('dve', 'reciprocal', R=[PB[4 + q2].r], W=[rc.r], out=rc[:, 2 + q2:3 + q2], in_=PB[4 + q2][:, 64:65])
                I('dve', 'tensor_scalar', R=[PB[4 + q2].r, rc.r], W=[oc.r], out=oc[:, q2, :], in0=PB[4 + q2][:, 0:64],
                  scalar1=rc[:, 2 + q2:3 + q2], scalar2=None, op0=ALU.mult)
            dma('sp', AP(CAT_d, T * D + 512 + h * 64, [[D, 128], [128 * D, 2], [1, 64]]), oc[:, :, :], R=[oc.r], W=CATR[NLT:NTILE])
        A.release(m)

    for i in range(nlayers):
        j = i // 2
        last = i == 3
        if i % 2 == 0:
            phase_inproj(i, ev_w_in, j, E_EV)
            phase_mla_prep(j)
            phase_attention(96, 96 ** -0.5)
            phase_conv(j)
            w_out_t = ev_w_out
        else:
            phase_inproj(i, od_w_in, j, E_OD)
            if CHUNKED:
                phase_rwkv_prep2(j)
                phase_rwkv_chunk()
                phase_rwkv_post2(j)
            else:
                phase_rwkv_prep(j)
                phase_rwkv_scan()
                phase_rwkv_post(j)
            phase_na_prep(j)
            phase_natten(j)
            w_out_t = od_w_out
        m = A.mark()
        GT = A.alloc([128, 5, 16], F32, 'gt')
        IXG = A.alloc([128, 5, 16], I32, 'ixg')
        IXS = A.alloc([128, 5, 16], I32, 'ixs')
        m2 = A.mark()
        AFFT = A.alloc([16, NT], F32, 'afft')
        phase_outproj(i, w_out_t, j, AFFT)
        phase_topk(AFFT, GT, IXG, IXS, not last)
        A.release(m2)
        phase_moe(i, GT, IXG, IXS, not last)
        A.release(m)

    S.barrier()
    S.emit()
    st.close()
    return nc


def _consts(inputs):
    GRID_W = 64
    t = np.arange(T)
    row = (t // GRID_W).astype(np.float32)
    col = (t % GRID_W).astype(np.float32)
    inv = (10000.0 ** (-np.arange(0, 16, 2, dtype=np.float32) / 16)).astype(np.float32)
    ar = row[:, None] * inv
    ac = col[:, None] * inv
    ang = np.concatenate([ar, ar, ac, ac], -1).astype(np.float32)
    sign = np.array([-1.0] * 8 + [1.0] * 8 + [-1.0] * 8 + [1.0] * 8, np.float32)
    rope = np.concatenate([np.cos(ang), np.sin(ang) * sign], -1).astype(np.float32)
    ident = np.eye(128, dtype=np.float32)
    flip = np.ascontiguousarray(ident[::-1])
    iota = np.tile(np.arange(256, dtype=np.float32)[None, :], (128, 1))
    rpb = np.asarray(inputs['na_rpb'], np.float32)
    nab = np.full((2, 8, 8, 4, 128, 64), -200.0, np.float32)
    c = np.arange(64)
    cs = np.clip(c - 8, 0, 48)
    for delta in range(8):
        for kt in range(4):
            for kk in range(128):
                ii = kt * 2 + kk // 64
                jj = kk % 64
                drow = ii - delta + 7
                valid = (jj >= cs) & (jj < cs + 16)
                dcol = jj - c + 15
                qs = c[valid]
                nab[:, :, delta, kt, kk, qs] = rpb[:, :, drow, dcol[valid]]
    nab = np.ascontiguousarray(nab.transpose(0, 1, 4, 2, 3, 5).reshape(2, 8, 128, 8, 256))
    ii = np.arange(128)
    tri = (ii[:, None] <= ii[None, :]).astype(np.float32)
    blk = np.ones((128, 128), np.float32)
    i6 = np.arange(128)
    lt, gt, le = (i6[:, None] < i6[None, :]), (i6[:, None] > i6[None, :]), (i6[:, None] <= i6[None, :])
    sameb = (i6[:, None] // 16) == (i6[None, :] // 16)
    msk = np.stack([lt & sameb, gt & sameb, le, lt & ~sameb, gt & ~sameb, lt], 1).astype(np.float32)
    return dict(ident=ident, flip=flip, rope=rope, iota=iota, nab=nab, tri=tri, blk=blk, msk=np.ascontiguousarray(msk))


_WNAMES = ['ada_w', 'ada_b', 'norm1_g', 'norm2_g', 'ev_w_in', 'ev_w_out', 'mla_q_norm', 'mla_w_uq', 'mla_kv_norm',
           'mla_w_ukv', 'mla_q_g', 'mla_k_g', 'cv_dw_w', 'cv_dw_b', 'cv_ln_g', 'cv_ln_b', 'od_w_in', 'od_w_out',
           'rw_shift_w', 'rw_w0', 'rw_w2', 'rw_a0', 'rw_a2', 'rw_g2', 'rw_k_k', 'rw_k_a', 'rw_r_k', 'rw_gn_g', 'rw_gn_b',
           'na_q_g', 'na_k_g', 'moe_router', 'moe_w1', 'moe_w3', 'moe_w2']


def make_in_maps(inputs, cores):
    cst = _consts(inputs)
    shared = {k: np.ascontiguousarray(np.asarray(inputs[k], np.float32)) for k in _WNAMES}
    shared.update(cst)
    x = np.asarray(inputs['x'], np.float32)
    c = np.asarray(inputs['c'], np.float32)
    ctx = np.asarray(inputs['ctx'], np.float32)
    c_ctx = np.asarray(inputs['c_ctx'], np.float32)
    maps = []
    for b in cores:
        mp = dict(shared)
        mp['x'] = np.ascontiguousarray(x[b])
        mp['ctx'] = np.ascontiguousarray(ctx[b])
        mp['cc'] = np.ascontiguousarray(np.stack([c[b], c_ctx], 0))
        maps.append(mp)
    return maps


def kernel(**inputs):
    nc = build()
    maps = make_in_maps(inputs, list(range(8)))
    res = run_bass_kernel_spmd(nc, maps, core_ids=list(range(8)))
    return np.stack([np.asarray(r['out'], np.float32) for r in res.results], 0)
```
